# Optimizing a Trainium2 kernel written in Bass

```python
import jax, jax.numpy as jnp
from jax import lax
import numpy as np

D_MODEL = 2048
BATCH = 2
SEQ = 4096
DEPTH = 2

N_MIX_HEADS = 12
HEAD_DIM = 128
MIX_WIDTH = N_MIX_HEADS * HEAD_DIM
MEM_HEADS = 4
MEM_HEAD_DIM = 128
MEM_WIDTH = MEM_HEADS * MEM_HEAD_DIM
N_MEM = 256
CONV_WIDTH = 3
Q_LORA = 512
KV_LORA = 256
QK_NOPE = 128
QK_ROPE = 64
QK_HEAD = QK_NOPE + QK_ROPE
V_HEAD = 128
ROPE_THETA = 10000.0
Q_BLOCK = 128
D_FF = 7168
N_EXPERTS = 8
TOP_K = 2
EPS = 1e-6
N_A = DEPTH // 2
N_B = DEPTH - N_A
N_DENSE = (DEPTH + 1) // 2
N_MOE = DEPTH // 2

kernel_name = "yoco_shortconv_mla_memxattn_moe"


def rms_norm(x, g):
    xf = x.astype(jnp.float32)
    y = xf * lax.rsqrt(jnp.mean(xf * xf, axis=-1, keepdims=True) + EPS)
    return (y * g.astype(jnp.float32)).astype(x.dtype)


def rope_tables(positions):
    inv_freq = 1.0 / (ROPE_THETA ** (jnp.arange(0, QK_ROPE, 2, dtype=jnp.float32) / QK_ROPE))
    ang = positions.astype(jnp.float32)[..., None] * inv_freq
    return jnp.cos(ang)[:, :, None, :], jnp.sin(ang)[:, :, None, :]


def apply_rope(x, cos, sin):
    x1, x2 = jnp.split(x.astype(jnp.float32), 2, axis=-1)
    out = jnp.concatenate([x1 * cos - x2 * sin, x2 * cos + x1 * sin], axis=-1)
    return out.astype(x.dtype)


def swiglu(h, w_gate, w_up, w_down):
    return (jax.nn.silu(h @ w_gate) * (h @ w_up)) @ w_down


def short_conv_mixer(u_a, conv_w):
    xin, gate_b, gate_c = jnp.split(u_a, 3, axis=-1)
    z = gate_c * xin
    y = lax.conv_general_dilated(
        z, conv_w[:, None, :].astype(z.dtype), window_strides=(1,),
        padding=[(CONV_WIDTH - 1, 0)],
        dimension_numbers=('NWC', 'WIO', 'NWC'),
        feature_group_count=MIX_WIDTH)
    return gate_b * y


def memory_attention(u_m, mem, g_mem, w_mem_kv, g_mq, g_mk):
    b, s, _ = u_m.shape
    m = mem.shape[1]
    q = rms_norm(u_m.reshape(b, s, MEM_HEADS, MEM_HEAD_DIM), g_mq)
    kv = rms_norm(mem, g_mem) @ w_mem_kv
    k, v = jnp.split(kv, 2, axis=-1)
    k = rms_norm(k.reshape(b, m, MEM_HEADS, MEM_HEAD_DIM), g_mk)
    v = v.reshape(b, m, MEM_HEADS, MEM_HEAD_DIM)
    sc = jnp.einsum('bshd,bmhd->bhsm', q, k).astype(jnp.float32) * (MEM_HEAD_DIM ** -0.5)
    p = jax.nn.softmax(sc, axis=-1).astype(v.dtype)
    return jnp.einsum('bhsm,bmhd->bshd', p, v).reshape(b, s, MEM_WIDTH)


def shared_mla_kv(x, g_kv, w_kv_a, g_kv_a, w_kv_b, g_kn, cos, sin):
    b, s, _ = x.shape
    kv_a = rms_norm(x, g_kv) @ w_kv_a
    c_kv = rms_norm(kv_a[..., :KV_LORA], g_kv_a)
    k_pe = kv_a[..., KV_LORA:]
    kv = (c_kv @ w_kv_b).reshape(b, s, N_MIX_HEADS, QK_NOPE + V_HEAD)
    k_nope, v = kv[..., :QK_NOPE], kv[..., QK_NOPE:]
    k_pe = jnp.broadcast_to(k_pe[:, :, None, :], (b, s, N_MIX_HEADS, QK_ROPE))
    k = rms_norm(jnp.concatenate([k_nope, k_pe], axis=-1), g_kn)
    k = jnp.concatenate([k[..., :QK_NOPE], apply_rope(k[..., QK_NOPE:], cos, sin)], axis=-1)
    return k, v


def mla_query(u_q, g_q_a, w_q_b, g_qn, cos, sin):
    b, s, _ = u_q.shape
    c_q = rms_norm(u_q, g_q_a)
    q = (c_q @ w_q_b).reshape(b, s, N_MIX_HEADS, QK_HEAD)
    q = rms_norm(q, g_qn)
    return jnp.concatenate([q[..., :QK_NOPE], apply_rope(q[..., QK_NOPE:], cos, sin)], axis=-1)


def causal_block_attention(q, k, v):
    b, s, h, dq = q.shape
    nb = s // Q_BLOCK
    qb = q.reshape(b, nb, Q_BLOCK, h, dq).transpose(1, 0, 2, 3, 4)
    k_idx = jnp.arange(s)
    scale = QK_HEAD ** -0.5

    def one_block(args):
        q_blk, i = args
        sc = jnp.einsum('bqhd,bkhd->bhqk', q_blk, k).astype(jnp.float32) * scale
        q_idx = i * Q_BLOCK + jnp.arange(Q_BLOCK)
        sc = jnp.where(k_idx[None, :] <= q_idx[:, None], sc, -jnp.inf)
        p = jax.nn.softmax(sc, axis=-1).astype(v.dtype)
        return jnp.einsum('bhqk,bkhd->bqhd', p, v)

    out = lax.map(one_block, (qb, jnp.arange(nb)))
    return out.transpose(1, 0, 2, 3, 4).reshape(b, s, h * v.shape[-1])


def moe_swiglu(h, w_router, w_gate, w_up, w_down):
    b, s, d = h.shape
    t = h.reshape(b * s, d)
    logits = t.astype(jnp.float32) @ w_router.astype(jnp.float32)
    top_val, top_idx = lax.top_k(logits, TOP_K)
    top_w = jax.nn.softmax(top_val, axis=-1)
    gates = jnp.sum(jax.nn.one_hot(top_idx, N_EXPERTS, dtype=jnp.float32) * top_w[..., None], axis=1)
    gates = gates.astype(t.dtype)
    out = jnp.zeros_like(t)
    for e in range(N_EXPERTS):
        out = out + gates[:, e:e + 1] * swiglu(t, w_gate[e], w_up[e], w_down[e])
    return out.reshape(b, s, d)


def setup_inputs(seed: int = 0) -> dict:
    key = jax.random.key(seed)
    ks = iter(jax.random.split(key, 40))
    f32 = jnp.float32

    def w(shape, fan_in):
        return jax.random.normal(next(ks), shape, f32) * (fan_in ** -0.5)

    def gain(shape):
        return 1.0 + 0.02 * jax.random.normal(next(ks), shape, f32)

    x = jax.random.normal(next(ks), (BATCH, SEQ, D_MODEL), f32)
    mem = jax.random.normal(next(ks), (BATCH, N_MEM, D_MODEL), f32)
    positions = (jnp.arange(SEQ, dtype=jnp.int32)[None, :]
                 + jax.random.randint(next(ks), (BATCH, 1), 0, 1024, dtype=jnp.int32))
    return {
        "x": x,
        "mem": mem,
        "positions": positions,
        "g_mix": gain((DEPTH, D_MODEL)),
        "g_ffn": gain((DEPTH, D_MODEL)),
        "g_mem": gain((DEPTH, D_MODEL)),
        "w_mem_kv": w((DEPTH, D_MODEL, 2 * MEM_WIDTH), D_MODEL),
        "g_mq": gain((DEPTH, MEM_HEAD_DIM)),
        "g_mk": gain((DEPTH, MEM_HEAD_DIM)),
        "w_out": w((DEPTH, MIX_WIDTH + MEM_WIDTH, D_MODEL), MIX_WIDTH + MEM_WIDTH),
        "a_w_in": w((N_A, D_MODEL, 3 * MIX_WIDTH + MEM_WIDTH), D_MODEL),
        "a_conv_w": w((N_A, CONV_WIDTH, MIX_WIDTH), CONV_WIDTH),
        "b_w_in": w((N_B, D_MODEL, Q_LORA + MEM_WIDTH), D_MODEL),
        "b_g_q_a": gain((N_B, Q_LORA)),
        "b_w_q_b": w((N_B, Q_LORA, N_MIX_HEADS * QK_HEAD), Q_LORA),
        "b_g_qn": gain((N_B, QK_HEAD)),
        "g_kv": gain((D_MODEL,)),
        "w_kv_a": w((D_MODEL, KV_LORA + QK_ROPE), D_MODEL),
        "g_kv_a": gain((KV_LORA,)),
        "w_kv_b": w((KV_LORA, N_MIX_HEADS * (QK_NOPE + V_HEAD)), KV_LORA),
        "g_kn": gain((QK_HEAD,)),
        "ffn_w_gate": w((N_DENSE, D_MODEL, D_FF), D_MODEL),
        "ffn_w_up": w((N_DENSE, D_MODEL, D_FF), D_MODEL),
        "ffn_w_down": w((N_DENSE, D_FF, D_MODEL), D_FF),
        "moe_w_router": w((N_MOE, D_MODEL, N_EXPERTS), D_MODEL),
        "moe_w_gate": w((N_MOE, N_EXPERTS, D_MODEL, D_FF), D_MODEL),
        "moe_w_up": w((N_MOE, N_EXPERTS, D_MODEL, D_FF), D_MODEL),
        "moe_w_down": w((N_MOE, N_EXPERTS, D_FF, D_MODEL), D_FF),
    }


def reference(x, mem, positions, g_mix, g_ffn, g_mem, w_mem_kv, g_mq, g_mk, w_out,
              a_w_in, a_conv_w, b_w_in, b_g_q_a, b_w_q_b, b_g_qn,
              g_kv, w_kv_a, g_kv_a, w_kv_b, g_kn,
              ffn_w_gate, ffn_w_up, ffn_w_down,
              moe_w_router, moe_w_gate, moe_w_up, moe_w_down):
    cos, sin = rope_tables(positions)
    k_shared = None
    v_shared = None
    for l in range(DEPTH):
        h = rms_norm(x, g_mix[l])
        if l < N_A:
            u = h @ a_w_in[l]
            mix = short_conv_mixer(u[..., :3 * MIX_WIDTH], a_conv_w[l])
            u_m = u[..., 3 * MIX_WIDTH:]
        else:
            if l == N_A:
                k_shared, v_shared = shared_mla_kv(x, g_kv, w_kv_a, g_kv_a, w_kv_b, g_kn, cos, sin)
            j = l - N_A
            u = h @ b_w_in[j]
            q = mla_query(u[..., :Q_LORA], b_g_q_a[j], b_w_q_b[j], b_g_qn[j], cos, sin)
            mix = causal_block_attention(q, k_shared, v_shared)
            u_m = u[..., Q_LORA:]
        mem_out = memory_attention(u_m, mem, g_mem[l], w_mem_kv[l], g_mq[l], g_mk[l])
        x = x + jnp.concatenate([mix, mem_out], axis=-1) @ w_out[l]
        h = rms_norm(x, g_ffn[l])
        if l % 2 == 0:
            i = l // 2
            x = x + swiglu(h, ffn_w_gate[i], ffn_w_up[i], ffn_w_down[i])
        else:
            i = l // 2
            x = x + moe_swiglu(h, moe_w_router[i], moe_w_gate[i], moe_w_up[i], moe_w_down[i])
    return x
```

```python
import contextlib
import numpy as np
import concourse.bass as bass
import concourse.mybir as mybir
from concourse.bass_utils import run_bass_kernel_spmd

F32 = mybir.dt.float32
BF16 = mybir.dt.bfloat16
I32 = mybir.dt.int32
AF = mybir.ActivationFunctionType
ALU = mybir.AluOpType


class Buf:
    __slots__ = ("name", "writer", "readers", "dsem", "dcount")

    def __init__(self, name):
        self.name = name
        self.writer = None
        self.readers = []
        self.dsem = None
        self.dcount = 0


class Eng:
    def __init__(self, name, h, sem):
        self.name = name
        self.h = h
        self.sem = sem
        self.cnt = 0
        self.known = {}


class KB:
    def __init__(self, nc, stack):
        self.nc = nc
        self.stack = stack
        self.gstack = stack
        self.nsem = 0
        self.E = {}
        for name, h in (("pe", nc.tensor), ("act", nc.scalar), ("dve", nc.vector),
                        ("pool", nc.gpsimd), ("sp", nc.sync)):
            self.E[name] = Eng(name, h, self.new_sem("e_" + name))
        self.nbuf = 0
        self.n_inst = 0

    def new_sem(self, name):
        self.nsem += 1
        return self.gstack.enter_context(self.nc.semaphore(f"{name}_{self.nsem}"))

    def buf(self, name=None):
        self.nbuf += 1
        return Buf(name or f"b{self.nbuf}")

    def sb(self, name, shape, dt):
        self.nbuf += 1
        return self.stack.enter_context(self.nc.sbuf_tensor(f"{name}_{self.nbuf}", list(shape), dt))

    def ps(self, name, shape, dt=F32):
        return self.stack.enter_context(self.nc.psum_tensor(name, list(shape), dt))

    def _wait(self, E, tok):
        sem, val, src = tok
        if src is E and E.name == "pe":
            return
        key = id(sem)
        if E.known.get(key, 0) >= val:
            return
        E.h.wait_ge(sem, val)
        E.known[key] = val

    def _sync(self, E, reads, writes):
        for b in reads:
            if b.writer is not None:
                self._wait(E, b.writer)
        for b in writes:
            if b.writer is not None and b.writer[2] is not E:
                self._wait(E, b.writer)
            for r in b.readers:
                if r[2] is not E:
                    self._wait(E, r)

    def _commit(self, tok, reads, writes):
        for b in reads:
            b.readers.append(tok)
            if len(b.readers) > 64:
                best = {}
                for t in b.readers:
                    k = id(t[0])
                    if k not in best or best[k][1] < t[1]:
                        best[k] = t
                b.readers = list(best.values())
        for b in writes:
            b.writer = tok
            b.readers = []

    def op(self, eng, reads, writes, fn, *a, **kw):
        E = self.E[eng]
        self._sync(E, reads, writes)
        ins = fn(*a, **kw)
        E.cnt += 1
        ins.then_inc(E.sem, 1)
        self.n_inst += 1
        self._commit((E.sem, E.cnt, E), reads, writes)
        return ins

    def mm(self, out_buf, out_ap, pairs, reads, transpose=False):
        E = self.E["pe"]
        self._sync(E, reads, [out_buf])
        n = len(pairs)
        ins = None
        for i, (l, r) in enumerate(pairs):
            ins = self.nc.tensor.matmul(out_ap, l, r, start=(i == 0), stop=(i == n - 1))
            self.n_inst += 1
        E.cnt += 1
        ins.then_inc(E.sem, 1)
        self._commit((E.sem, E.cnt, E), reads, [out_buf])

    def mm_open(self, reads, writes):
        E = self.E["pe"]
        self._sync(E, reads, writes)

    def mm_close(self, ins, reads, writes):
        E = self.E["pe"]
        E.cnt += 1
        ins.then_inc(E.sem, 1)
        self._commit((E.sem, E.cnt, E), reads, writes)

    def dma(self, q, out_ap, in_ap, sbuf_buf, reads, writes, **kw):
        E = self.E[q]
        b = sbuf_buf
        if b.dsem is None:
            b.dsem = self.new_sem("d_" + b.name)
        self._sync(E, reads, writes)
        if b.dcount:
            self._wait(E, (b.dsem, b.dcount, None))
        ins = E.h.dma_start(out=out_ap, in_=in_ap, **kw)
        b.dcount += 16
        ins.then_inc(b.dsem, 16)
        self.n_inst += 1
        tok = (b.dsem, b.dcount, None)
        self._commit(tok, reads, writes)
        return tok

    def wait_tok(self, eng, tok):
        self._wait(self.E[eng], tok)


    def barrier(self):
        toks = [(E.sem, E.cnt, E) for E in self.E.values() if E.cnt > 0]
        toks += [(b.dsem, b.dcount, None) for b in self._dbufs if b.dcount > 0]
        for E in self.E.values():
            for t in toks:
                if t[2] is E:
                    continue
                self._wait(E, t)


D = 2048
SEQ = 4096
NB = 2
NCORE = 8
TOK = 1024
TL = 512
NTL = 2
KC = 16
DFF = 7168
NEXP = 8
EPS = 1e-6
NPC = 168

PC_GMIX0, PC_GFFN0, PC_GKV, PC_GMIX1, PC_GFFN1 = 0, 16, 32, 48, 64
PC_CONV = 80
PC_GMQ0, PC_GMK0, PC_GMQ1, PC_GMK1 = 116, 117, 118, 119
PC_GQA = 120
PC_GKVA = 124
PC_GQN_N, PC_GKN_N = 126, 127
PC_GQN_R, PC_GQN_RS, PC_GKN_R, PC_GKN_RS, PC_SGN, PC_INVF = 128, 129, 130, 131, 132, 133
PC_HALO = 134
PC_KIDX = 136


class Ctx:
    pass


def build(mode):
    nc = bass.Bass("TRN2", target_bir_lowering=False)
    do0 = mode in ("L0", "fused")
    do1 = mode in ("L1", "fused")

    def din(name, shape, dt=F32):
        return nc.dram_tensor(name, list(shape), dt, kind="ExternalInput").ap()

    def dout(name, shape, dt=F32):
        return nc.dram_tensor(name, list(shape), dt, kind="ExternalOutput").ap()

    def dint(name, shape, dt=F32):
        return nc.dram_tensor(name, list(shape), dt, kind="Internal").ap()

    g = Ctx()
    g.nc = nc
    g.xT = din("xT", [D, TOK])
    g.params = din("params", [128, NPC])
    g.ident = din("ident", [128, 128])
    g.sel = din("sel", [8, NEXP * 128])
    g.mem = din("mem", [256, D])
    g.g_mem = din("g_mem", [2, D])
    g.w_mem_kv = din("w_mem_kv", [2, D, 1024])
    g.w_out = din("w_out", [2, D, D])
    if do0:
        g.xhT = din("xhT", [D, 4])
        g.pos = din("pos", [1, TOK], I32)
        g.a_w_in = din("a_w_in", [D, 5120])
        g.w_kv_a = din("w_kv_a", [D, 320])
        g.w_kv_b = din("w_kv_b", [256, 3072])
        g.ffn_gate = din("ffn_gate", [D, DFF])
        g.ffn_up = din("ffn_up", [D, DFF])
        g.ffn_down = din("ffn_down", [DFF, D])
    if do1:
        if not do0:
            g.pos = din("pos", [1, TOK], I32)
        g.qidx = din("qidx", [1, TOK])
        g.b_w_in = din("b_w_in", [D, 1024])
        g.w_q_b = din("w_q_b", [512, 2304])
        g.w_router = din("w_router", [D, NEXP])
        g.moe_gate = din("moe_gate", [NEXP, D, DFF])
        g.moe_up = din("moe_up", [NEXP, D, DFF])
        g.moe_down = din("moe_down", [NEXP, DFF, D])
        g.yT = dout("yT", [D, TOK])
    if mode == "L0":
        g.x1T = dout("x1T", [D, TOK])
        KT_own = dout("KT_own", [2304, TOK], BF16)
        V_own = dout("V_own", [TOK, 1536], BF16)
        g.kt_own = lambda h: KT_own[h * 192:(h + 1) * 192, :]
        g.v_own = lambda h: V_own[:, h * 128:(h + 1) * 128]
    elif mode == "L1":
        KTg = din("KTg", [4 * 2304, TOK], BF16)
        Vg = din("Vg", [4 * TOK, 1536], BF16)
        g.kt_g = lambda h, r: KTg[r * 2304 + h * 192:r * 2304 + (h + 1) * 192, :]
        g.v_g = lambda h, r: Vg[r * TOK:(r + 1) * TOK, h * 128:(h + 1) * 128]
    else:
        g.KT_own_l = [dint(f"KT_own{h}", [192, TOK], BF16) for h in range(12)]
        g.V_own_l = [dint(f"V_own{h}", [TOK, 128], BF16) for h in range(12)]
        g.KTg_l = [dint(f"KTg{h}", [4 * 192, TOK], BF16) for h in range(12)]
        g.Vg_l = [dint(f"Vg{h}", [4 * TOK, 128], BF16) for h in range(12)]
        g.kt_own = lambda h: g.KT_own_l[h]
        g.v_own = lambda h: g.V_own_l[h]
        g.kt_g = lambda h, r: g.KTg_l[h][r * 192:(r + 1) * 192, :]
        g.v_g = lambda h, r: g.Vg_l[h][r * TOK:(r + 1) * TOK, :]

    with contextlib.ExitStack() as st:
        kb = KB(nc, st)
        kb._dbufs = []
        g.kb = kb
        setup_globals(g)
        if do0:
            layer0(g)
            shared_kv(g)
            if mode == "L0":
                store_x(g, g.x1T)
        if mode == "fused":
            exchange_kv(g)
        if do1:
            layer1(g)
            store_x(g, g.yT)
        kb.barrier()
        g.n_inst = kb.n_inst
    return nc, g


def setup_globals(g):
    kb, nc = g.kb, g.nc
    g.x = kb.sb("x", [128, KC, TOK], F32)
    g.xb = [[kb.buf(f"x{kc}_{t}") for t in range(NTL)] for kc in range(KC)]
    g.par = kb.sb("par", [128, NPC], F32)
    g.par_b = kb.buf("par")
    g.idf = kb.sb("idf", [128, 128], F32)
    g.idb = kb.sb("idb", [128, 128], BF16)
    g.ones = kb.sb("ones", [128, 128], BF16)
    g.cst_b = kb.buf("cst")
    g.cs = kb.sb("cs", [64, 2, TOK], F32)
    g.cs_b = kb.buf("cs")
    g.pst = [kb.ps(f"ps{i}", [128, 512]) for i in range(8)]
    g.psb = [kb.buf(f"ps{i}") for i in range(8)]
    g.bank_i = 0
    g.bank_set = list(range(8))
    g.ffn_pending = []
    g.sq = [kb.sb(f"sq{i}", [128, 512], BF16) for i in range(4)]
    g.sq_b = [kb.buf(f"sq{i}") for i in range(4)]
    g.sq_i = 0
    g.rs = [kb.sb(f"rs{i}", [128, 512], F32) for i in range(3)]
    g.rs_b = [kb.buf(f"rs{i}") for i in range(3)]
    g.rs_i = 0
    g.fs = [kb.sb(f"fs{i}", [128, 514], F32) for i in range(3)]
    g.fs_b = [kb.buf(f"fs{i}") for i in range(3)]
    g.fs_i = 0

    xb_all = [b for row in g.xb for b in row]
    xv = g.xT.rearrange("(kc p) n -> p kc n", p=128)
    ldb = kb.buf("xload"); kb._dbufs.append(ldb)
    for t in range(NTL):
        kb.dma("sp", g.x[:, :, t * TL:(t + 1) * TL], xv[:, :, t * TL:(t + 1) * TL], ldb, [],
               [g.xb[kc][t] for kc in range(KC)])
    cb = kb.buf("cload"); kb._dbufs.append(cb)
    kb.dma("sp", g.par[:], g.params, cb, [], [g.par_b])
    kb.dma("sp", g.idf[:], g.ident, cb, [], [g.cst_b])
    kb.op("dve", [g.cst_b], [g.cst_b], nc.vector.tensor_copy, g.idb[:], g.idf[:])
    kb.op("dve", [], [g.cst_b], nc.vector.memset, g.ones[:], 1.0)
    rope_tables(g)


def bank(g):
    bs = g.bank_set
    for _ in range(len(bs)):
        i = bs[g.bank_i % len(bs)]
        g.bank_i += 1
        b = g.psb[i]
        if b.writer is None or b.readers:
            return g.pst[i], b
    raise RuntimeError("no free PSUM bank")


def alloc_ring(g, slot_el, nslot=2):
    kb = g.kb
    g.NSLOT = nslot
    g.SLOT = slot_el
    g.ring_gen = getattr(g, "ring_gen", 0) + 1
    g.wslot = [kb.sb(f"wslot{g.ring_gen}_{i}", [128, slot_el], BF16) for i in range(nslot)]
    g.wslot_b = [kb.buf(f"wslot{g.ring_gen}_{i}") for i in range(nslot)]
    for b in g.wslot_b:
        kb._dbufs.append(b)
    g.slot_i = 0


def scratch(g, kind):
    lst, bl, key = {"sq": (g.sq, g.sq_b, "sq_i"), "rs": (g.rs, g.rs_b, "rs_i"),
                    "fs": (g.fs, g.fs_b, "fs_i")}[kind]
    i = getattr(g, key)
    setattr(g, key, (i + 1) % len(lst))
    return lst[i], bl[i]


def load_w(g, pieces):
    kb = g.kb
    i = g.slot_i
    g.slot_i = (i + 1) % g.NSLOT
    t, b = g.wslot[i], g.wslot_b[i]
    views = []
    off = 0
    E = kb.E["pool"]
    kb._sync(E, [], [b])
    if b.dsem is None:
        b.dsem = kb.new_sem("d_" + b.name)
    if b.dcount:
        kb._wait(E, (b.dsem, b.dcount, None))
    for shape, src in pieces:
        n = int(np.prod(shape[1:]))
        v = t[:shape[0], off:off + n]
        if len(shape) == 3:
            v = v.rearrange("p (a b) -> p a b", a=shape[1])
        elif len(shape) == 4:
            v = v.rearrange("p (a b c) -> p a b c", a=shape[1], b=shape[2])
        ins = g.nc.gpsimd.dma_start(out=v, in_=src, max_dma_last_dim=4096)
        b.dcount += 16
        ins.then_inc(b.dsem, 16)
        kb.n_inst += 1
        views.append(v)
        off += n
    assert off <= g.SLOT
    kb._commit((b.dsem, b.dcount, None), [], [b])
    return views, b


def pcol(g, c, P=128):
    return g.par[:P, c:c + 1]


def rope_tables(g):
    kb, nc = g.kb, g.nc
    with contextlib.ExitStack() as st:
        old = kb.stack
        kb.stack = st
        posi = kb.sb("posi", [64, TOK], I32); pb = kb.buf("posi"); kb._dbufs.append(pb)
        ang = kb.sb("ang", [64, TOK], F32); ab = kb.buf("ang")
        t1 = kb.sb("rt1", [64, TOK], F32); t1b = kb.buf("rt1")
        ki = kb.sb("rki", [64, TOK], I32); kib = kb.buf("rki")
        kf = kb.sb("rkf", [64, TOK], F32); kfb = kb.buf("rkf")
        m = kb.sb("rm", [64, TOK], F32); mb = kb.buf("rm")
        kb.dma("sp", posi[:], g.pos.partition_broadcast(64).rearrange("p a n -> p (a n)"), pb, [], [pb])
        kb.op("dve", [pb], [ab], nc.vector.tensor_copy, ang[:], posi[:])
        kb.op("dve", [ab, g.par_b], [ab], nc.vector.tensor_scalar, out=ang[:], in0=ang[:],
              scalar1=pcol(g, PC_INVF, 64), scalar2=None, op0=ALU.mult)
        TWO_PI = 2.0 * np.pi
        for which, shift in ((1, 0.0), (0, np.pi / 2)):
            kb.op("dve", [ab], [t1b], nc.vector.tensor_scalar, out=t1[:], in0=ang[:],
                  scalar1=float(shift), scalar2=float(1.0 / TWO_PI), op0=ALU.add, op1=ALU.mult)
            kb.op("dve", [t1b], [kib], nc.vector.tensor_copy, ki[:], t1[:])
            kb.op("dve", [kib], [kfb], nc.vector.tensor_copy, kf[:], ki[:])
            kb.op("dve", [kfb, ab], [t1b], nc.vector.scalar_tensor_tensor, out=t1[:], in0=kf[:],
                  scalar=float(-TWO_PI), in1=ang[:], op0=ALU.mult, op1=ALU.add)
            if shift:
                kb.op("dve", [t1b], [t1b], nc.vector.tensor_scalar, out=t1[:], in0=t1[:],
                      scalar1=float(shift), scalar2=None, op0=ALU.add)
            kb.op("dve", [t1b], [mb], nc.vector.tensor_scalar, out=m[:], in0=t1[:],
                  scalar1=float(np.pi), scalar2=float(-TWO_PI), op0=ALU.is_gt, op1=ALU.mult)
            kb.op("dve", [mb, t1b], [t1b], nc.vector.tensor_tensor, out=t1[:], in0=t1[:], in1=m[:], op=ALU.add)
            kb.op("dve", [t1b], [mb], nc.vector.tensor_scalar, out=m[:], in0=t1[:],
                  scalar1=float(-np.pi), scalar2=float(TWO_PI), op0=ALU.is_lt, op1=ALU.mult)
            kb.op("dve", [mb, t1b], [t1b], nc.vector.tensor_tensor, out=t1[:], in0=t1[:], in1=m[:], op=ALU.add)
            kb.op("dve", [t1b], [t1b], nc.vector.tensor_scalar, out=t1[:], in0=t1[:],
                  scalar1=float(np.pi), scalar2=float(-np.pi), op0=ALU.min, op1=ALU.max)
            kb.op("act", [t1b], [g.cs_b], nc.scalar.activation, out=g.cs[:, which, :], in_=t1[:], func=AF.Sin)
        kb.op("dve", [g.cs_b, g.par_b], [g.cs_b], nc.vector.tensor_scalar, out=g.cs[:, 1, :], in0=g.cs[:, 1, :],
              scalar1=pcol(g, PC_SGN, 64), scalar2=None, op0=ALU.mult)
        kb.barrier()
        kb.stack = old


def sumsq_rstd(g, srcs, N, Dn, reads):
    kb, nc = g.kb, g.nc
    pt, pb = bank(g)
    n = len(srcs)
    for i, (ap, P) in enumerate(srcs):
        sq, sqb = scratch(g, "sq")
        kb.op("act", reads, [sqb], nc.scalar.activation, out=sq[:P, :N], in_=ap, func=AF.Square)
        kb.mm_open([sqb, g.cst_b], [pb] if i == 0 else [])
        ins = nc.tensor.matmul(pt[:, :N], g.ones[:P, :], sq[:P, :N], start=(i == 0), stop=(i == n - 1))
        kb.n_inst += 1
        kb.mm_close(ins, [sqb], [pb])
    rs, rsb = scratch(g, "rs")
    kb.op("act", [pb], [rsb], nc.scalar.activation, out=rs[:, :N], in_=pt[:, :N], func=AF.Sqrt,
          scale=float(1.0 / Dn), bias=float(EPS))
    kb.op("dve", [rsb], [rsb], nc.vector.reciprocal, rs[:, :N], rs[:, :N])
    return rs, rsb


def norm_x(g, gcol0, hT, hT_b, tiles=(0, 1), on_rstd=None):
    kb, nc = g.kb, g.nc
    for t in tiles:
        sl = slice(t * TL, (t + 1) * TL)
        rs, rsb = sumsq_rstd(g, [(g.x[:, kc, sl], 128) for kc in range(KC)], TL, D,
                             [g.xb[kc][t] for kc in range(KC)])
        if on_rstd is not None:
            on_rstd(t, rs, rsb)
        for kc in range(KC):
            kb.op("dve", [g.xb[kc][t], rsb, g.par_b], [hT_b[t]], nc.vector.scalar_tensor_tensor,
                  out=hT[:, kc, sl], in0=g.x[:, kc, sl], scalar=pcol(g, gcol0 + kc), in1=rs[:, :TL],
                  op0=ALU.mult, op1=ALU.mult)


def store_x(g, dst):
    kb = g.kb
    dv = dst.rearrange("(kc p) n -> p kc n", p=128)
    sb = kb.buf("xstore"); kb._dbufs.append(sb)
    for t in range(NTL):
        kb.dma("sp", dv[:, :, t * TL:(t + 1) * TL], g.x[:, :, t * TL:(t + 1) * TL], sb,
               [g.xb[kc][t] for kc in range(KC)], [])


def mem_kv(g, l, kmT, kmT_b, vm, vm_b):
    kb, nc = g.kb, g.nc
    with contextlib.ExitStack() as st:
        old = kb.stack
        kb.stack = st
        mt = kb.sb("mem_t", [128, 2, D], F32); mtb = kb.buf("mem_t"); kb._dbufs.append(mtb)
        gb = kb.sb("gmem_bc", [128, D], F32); gbb = kb.buf("gmem_bc"); kb._dbufs.append(gbb)
        mn = kb.sb("mem_n", [128, 2, D], BF16); mnb = kb.buf("mem_n")
        mnT = kb.sb("mem_nT", [128, KC, 256], BF16); mnTb = kb.buf("mem_nT")
        ssq = kb.sb("mem_ss", [128, 2], F32); ssb = kb.buf("mem_ss")
        kb.dma("sp", mt[:], g.mem.rearrange("(mb p) d -> p mb d", p=128), mtb, [], [mtb])
        kb.dma("sp", gb[:], g.g_mem[l:l + 1, :].partition_broadcast(128).rearrange("p a n -> p (a n)"),
               gbb, [], [gbb])
        for mb in range(2):
            kb.op("act", [mtb], [mnb, ssb], nc.scalar.activation, out=mn[:, mb, :], in_=mt[:, mb, :],
                  func=AF.Square, accum_out=ssq[:, mb:mb + 1])
        kb.op("act", [ssb], [ssb], nc.scalar.activation, out=ssq[:], in_=ssq[:], func=AF.Sqrt,
              scale=float(1.0 / D), bias=float(EPS))
        kb.op("dve", [ssb], [ssb], nc.vector.reciprocal, ssq[:], ssq[:])
        for mb in range(2):
            kb.op("dve", [mtb, ssb, gbb], [mnb], nc.vector.scalar_tensor_tensor, out=mn[:, mb, :],
                  in0=mt[:, mb, :], scalar=ssq[:, mb:mb + 1], in1=gb[:], op0=ALU.mult, op1=ALU.mult)
        for mb in range(2):
            for k4 in range(4):
                pt, pb = bank(g)
                ptb = pt[:].bitcast(BF16)
                kb.mm_open([mnb, g.cst_b], [pb])
                ins = None
                for q in range(4):
                    kc = k4 * 4 + q
                    ins = nc.tensor.transpose(ptb[:, q * 128:(q + 1) * 128], mn[:, mb, kc * 128:(kc + 1) * 128],
                                              g.idb[:])
                    kb.n_inst += 1
                kb.mm_close(ins, [mnb], [pb])
                kb.op("dve", [pb], [mnTb], nc.vector.tensor_copy,
                      mnT[:, k4 * 4:(k4 + 1) * 4, mb * 128:(mb + 1) * 128],
                      ptb[:, 0:512].rearrange("p (q n) -> p q n", q=4))
        wv = g.w_mem_kv[l].rearrange("(kc p) m -> p kc m", p=128)
        (wk,), wkb = load_w(g, [((128, KC, 512), wv[:, :, 0:512])])
        (wvv,), wvb = load_w(g, [((128, KC, 512), wv[:, :, 512:1024])])
        gk = PC_GMK0 if l == 0 else PC_GMK1
        for hh in range(4):
            pt, pb = bank(g)
            kb.mm(pb, pt[:, :256], [(wk[:, kc, hh * 128:(hh + 1) * 128], mnT[:, kc, :]) for kc in range(KC)],
                  [wkb, mnTb])
            rs, rsb = sumsq_rstd(g, [(pt[:, :256], 128)], 256, 128, [pb])
            kb.op("dve", [pb, rsb, g.par_b], [kmT_b], nc.vector.scalar_tensor_tensor, out=kmT[:, hh, :],
                  in0=pt[:, :256], scalar=pcol(g, gk), in1=rs[:, :256], op0=ALU.mult, op1=ALU.mult)
        for mb in range(2):
            pt, pb = bank(g)
            kb.mm(pb, pt[:, :], [(mnT[:, kc, mb * 128:(mb + 1) * 128], wvv[:, kc, :]) for kc in range(KC)],
                  [wvb, mnTb])
            kb.op("act", [pb], [vm_b], nc.scalar.copy, vm[:, mb, :], pt[:, :])
        kb.barrier()
        kb.stack = old


def mem_attention(g, wq, wqb, hT, hT_b, gq_col, kmT, kmT_b, vm, vm_b, mixT, mix_b):
    kb, nc = g.kb, g.nc
    sc = float(128 ** -0.5)
    qm = [kb.sb(f"qm{i}", [128, TL], BF16) for i in range(2)]
    qm_b = [kb.buf(f"qm{i}") for i in range(2)]
    qi = 0
    for t in range(NTL):
        sl = slice(t * TL, (t + 1) * TL)
        for hh in range(4):
            pt, pb = bank(g)
            kb.mm(pb, pt[:, :], [(wq[:, kc, hh * 128:(hh + 1) * 128], hT[:, kc, sl]) for kc in range(KC)],
                  [wqb, hT_b[t]])
            rs, rsb = sumsq_rstd(g, [(pt[:, :], 128)], TL, 128, [pb])
            q, qb = qm[qi % 2], qm_b[qi % 2]
            qi += 1
            kb.op("dve", [pb, rsb, g.par_b], [qb], nc.vector.scalar_tensor_tensor, out=q[:],
                  in0=pt[:, :], scalar=pcol(g, gq_col), in1=rs[:], op0=ALU.mult, op1=ALU.mult)
            po, pob = bank(g)
            pd, pdb = bank(g)
            for mb in range(2):
                ps_, psb_ = bank(g)
                kb.mm(psb_, ps_[:, :], [(kmT[:, hh, mb * 128:(mb + 1) * 128], q[:])], [kmT_b, qb])
                e, eb = scratch(g, "sq")
                kb.op("act", [psb_], [eb], nc.scalar.activation, out=e[:], in_=ps_[:, :], func=AF.Exp, scale=sc)
                kb.mm_open([eb, vm_b, g.cst_b], [pob, pdb] if mb == 0 else [])
                nc.tensor.matmul(po[:, :], vm[:, mb, hh * 128:(hh + 1) * 128], e[:], start=(mb == 0), stop=(mb == 1))
                ins = nc.tensor.matmul(pd[:, :], g.ones[:, :], e[:], start=(mb == 0), stop=(mb == 1))
                kb.n_inst += 2
                kb.mm_close(ins, [eb, vm_b], [pob, pdb])
            rd, rdb = scratch(g, "rs")
            kb.op("dve", [pdb], [rdb], nc.vector.reciprocal, rd[:], pd[:, :])
            kb.op("dve", [pob, rdb], [mix_b[t]], nc.vector.tensor_tensor, out=mixT[:, 12 + hh, sl], in0=po[:, :],
                  in1=rd[:], op=ALU.mult)


def out_proj(g, l, mixT, mix_b):
    kb, nc = g.kb, g.nc
    wv = g.w_out[l].rearrange("(kc p) m -> p kc m", p=128)
    for pc in range(4):
        (w,), wb = load_w(g, [((128, KC, 512), wv[:, :, pc * 512:(pc + 1) * 512])])
        for t in range(NTL):
            sl = slice(t * TL, (t + 1) * TL)
            for dq in range(4):
                dc = pc * 4 + dq
                pt, pb = bank(g)
                kb.mm(pb, pt[:, :], [(w[:, kc, dq * 128:(dq + 1) * 128], mixT[:, kc, sl]) for kc in range(KC)],
                      [wb, mix_b[t]])
                kb.op("dve", [pb, g.xb[dc][t]], [g.xb[dc][t]], nc.vector.tensor_tensor, out=g.x[:, dc, sl],
                      in0=g.x[:, dc, sl], in1=pt[:, :], op=ALU.add)


def ffn_drain(g, n=None):
    pend = g.ffn_pending
    k = len(pend) if n is None else min(n, len(pend))
    for _ in range(k):
        pend.pop(0)()


def ffn_group(g, wg, wu, wd, wb, hT, hT_b, gate_bc=None, gate_bcb=None, act=None, act_b=None):
    kb, nc = g.kb, g.nc
    for t in range(NTL):
        sl = slice(t * TL, (t + 1) * TL)
        for m in range(2):
            pg, pgb = bank(g)
            kb.mm(pgb, pg[:, :], [(wg[:, kc, m * 128:(m + 1) * 128], hT[:, kc, sl]) for kc in range(KC)],
                  [wb, hT_b[t]])
            sg, sgb = scratch(g, "sq")
            kb.op("act", [pgb], [sgb], nc.scalar.activation, out=sg[:], in_=pg[:, :], func=AF.Silu)
            ffn_drain(g, 4)
            pu, pub = bank(g)
            kb.mm(pub, pu[:, :], [(wu[:, kc, m * 128:(m + 1) * 128], hT[:, kc, sl]) for kc in range(KC)],
                  [wb, hT_b[t]])
            if gate_bc is None:
                kb.op("dve", [pub, sgb], [act_b[t][m]], nc.vector.tensor_tensor, out=act[t][:, m, :], in0=pu[:, :],
                      in1=sg[:], op=ALU.mult)
            else:
                ug, ugb = scratch(g, "fs")
                kb.op("dve", [pub, gate_bcb[t]], [ugb], nc.vector.tensor_tensor, out=ug[:, :TL], in0=pu[:, :],
                      in1=gate_bc[:, sl], op=ALU.mult)
                kb.op("dve", [ugb, sgb], [act_b[t][m]], nc.vector.tensor_tensor, out=act[t][:, m, :],
                      in0=ug[:, :TL], in1=sg[:], op=ALU.mult)
            ffn_drain(g, 4)
        ffn_drain(g)

        def down(dc, t=t, sl=sl, wd=wd, wb=wb):
            pt, pb = bank(g)
            kb.mm(pb, pt[:, :], [(wd[m][:, dc * 128:(dc + 1) * 128], act[t][:, m, :]) for m in range(2)],
                  [wb, act_b[t][0], act_b[t][1]])
            kb.op("dve", [pb, g.xb[dc][t]], [g.xb[dc][t]], nc.vector.tensor_tensor, out=g.x[:, dc, sl],
                  in0=g.x[:, dc, sl], in1=pt[:, :], op=ALU.add)

        for dc in range(KC):
            g.ffn_pending.append(lambda dc=dc, f=down: f(dc))


def ffn_weights(g, wgate, wup, wdown, gi):
    c0 = gi * 256
    gv = wgate.rearrange("(kc p) m -> p kc m", p=128)[:, :, c0:c0 + 256]
    uv = wup.rearrange("(kc p) m -> p kc m", p=128)[:, :, c0:c0 + 256]
    dv = wdown[c0:c0 + 256, :].rearrange("(m p) d -> p m d", p=128)
    (wg, wu, wd0, wd1), wb = load_w(g, [((128, KC, 256), gv), ((128, KC, 256), uv), ((128, D), dv[:, 0, :]),
                                          ((128, D), dv[:, 1, :])])
    return wg, wu, (wd0, wd1), wb


def layer0(g):
    kb, nc = g.kb, g.nc
    with contextlib.ExitStack() as st:
        old = kb.stack
        kb.stack = st
        alloc_ring(g, 8192)
        kmT = kb.sb("kmT", [128, 4, 256], BF16); kmT_b = kb.buf("kmT")
        vm = kb.sb("vm", [128, 2, 512], BF16); vm_b = kb.buf("vm")
        mem_kv(g, 0, kmT, kmT_b, vm, vm_b)
        hT = kb.sb("hT", [128, KC, TOK], BF16); hT_b = [kb.buf("hT0"), kb.buf("hT1")]
        mixT = kb.sb("mixT", [128, KC, TOK], BF16); mix_b = [kb.buf("mix0"), kb.buf("mix1")]
        xh = kb.sb("xh", [128, KC, 4], F32); xhb = kb.buf("xh"); kb._dbufs.append(xhb)
        hh_ = kb.sb("hh", [128, KC, 4], BF16); hhb = kb.buf("hh")
        zh = kb.sb("zh", [128, 8], F32); zhb = kb.buf("zh")
        kb.dma("sp", xh[:], g.xhT.rearrange("(kc p) n -> p kc n", p=128), xhb, [], [xhb])
        norm_x(g, PC_GMIX0, hT, hT_b)
        rs, rsb = sumsq_rstd(g, [(xh[:, kc, :], 128) for kc in range(KC)], 4, D, [xhb])
        for kc in range(KC):
            kb.op("dve", [xhb, rsb, g.par_b], [hhb], nc.vector.scalar_tensor_tensor, out=hh_[:, kc, :],
                  in0=xh[:, kc, :], scalar=pcol(g, PC_GMIX0 + kc), in1=rs[:, :4], op0=ALU.mult, op1=ALU.mult)
        wa = g.a_w_in.rearrange("(kc p) m -> p kc m", p=128)
        for j in range(12):
            w, wb = load_w(g, [((128, KC, 128), wa[:, :, s_ * 1536 + j * 128:s_ * 1536 + (j + 1) * 128])
                               for s_ in range(3)])
            ph, phb = bank(g)
            kb.mm(phb, ph[:, 0:4], [(w[0][:, kc, :], hh_[:, kc, :]) for kc in range(KC)], [wb, hhb])
            kb.mm(phb, ph[:, 4:8], [(w[2][:, kc, :], hh_[:, kc, :]) for kc in range(KC)], [wb, hhb])
            kb.op("act", [phb], [zhb], nc.scalar.copy, zh[:, 0:8], ph[:, 0:8])
            for t in range(NTL):
                sl = slice(t * TL, (t + 1) * TL)
                px, pxb = bank(g)
                kb.mm(pxb, px[:, :], [(w[0][:, kc, :], hT[:, kc, sl]) for kc in range(KC)], [wb, hT_b[t]])
                pc_, pcb = bank(g)
                kb.mm(pcb, pc_[:, :], [(w[2][:, kc, :], hT[:, kc, sl]) for kc in range(KC)], [wb, hT_b[t]])
                pg, pgb = bank(g)
                kb.mm(pgb, pg[:, :], [(w[1][:, kc, :], hT[:, kc, sl]) for kc in range(KC)], [wb, hT_b[t]])
                gc, gcb = scratch(g, "rs")
                kb.op("act", [pcb], [gcb], nc.scalar.copy, gc[:, :TL], pc_[:, :])
                z, zb = scratch(g, "fs")
                kb.op("dve", [pxb, gcb], [zb], nc.vector.tensor_tensor, out=z[:, 2:2 + TL], in0=px[:, :],
                      in1=gc[:, :TL], op=ALU.mult)
                kb.op("dve", [zhb, g.par_b, zb], [zb], nc.vector.scalar_tensor_tensor, out=z[:, 0:2],
                      in0=zh[:, 2 * t:2 * t + 2], scalar=pcol(g, PC_HALO + t), in1=zh[:, 4 + 2 * t:6 + 2 * t],
                      op0=ALU.mult, op1=ALU.mult)
                y, yb = scratch(g, "rs")
                cw = PC_CONV + j * 3
                kb.op("dve", [zb, g.par_b], [yb], nc.vector.tensor_scalar, out=y[:, :TL], in0=z[:, 0:TL],
                      scalar1=pcol(g, cw), scalar2=None, op0=ALU.mult)
                kb.op("dve", [zb, yb, g.par_b], [yb], nc.vector.scalar_tensor_tensor, out=y[:, :TL],
                      in0=z[:, 1:1 + TL], scalar=pcol(g, cw + 1), in1=y[:, :TL], op0=ALU.mult, op1=ALU.add)
                kb.op("dve", [zb, yb, g.par_b], [yb], nc.vector.scalar_tensor_tensor, out=y[:, :TL],
                      in0=z[:, 2:2 + TL], scalar=pcol(g, cw + 2), in1=y[:, :TL], op0=ALU.mult, op1=ALU.add)
                kb.op("dve", [pgb, yb], [mix_b[t]], nc.vector.tensor_tensor, out=mixT[:, j, sl], in0=pg[:, :],
                      in1=y[:, :TL], op=ALU.mult)
        (wq,), wqb = load_w(g, [((128, KC, 512),
                                  g.a_w_in.rearrange("(kc p) m -> p kc m", p=128)[:, :, 4608:5120])])
        mem_attention(g, wq, wqb, hT, hT_b, PC_GMQ0, kmT, kmT_b, vm, vm_b, mixT, mix_b)
        out_proj(g, 0, mixT, mix_b)
        kb.barrier()
        kb.stack = old
    with contextlib.ExitStack() as st:
        old = kb.stack
        kb.stack = st
        alloc_ring(g, 12288)
        hT = kb.sb("hT", [128, KC, TOK], BF16); hT_b = [kb.buf("hT0"), kb.buf("hT1")]
        norm_x(g, PC_GFFN0, hT, hT_b)
        act = [kb.sb(f"act{t}", [128, 2, TL], BF16) for t in range(NTL)]
        act_b = [[kb.buf(f"act{t}_{m}") for m in range(2)] for t in range(NTL)]
        for gi in range(DFF // 256):
            wg, wu, wd, wb = ffn_weights(g, g.ffn_gate, g.ffn_up, g.ffn_down, gi)
            ffn_group(g, wg, wu, wd, wb, hT, hT_b, act=act, act_b=act_b)
        ffn_drain(g)
        kb.barrier()
        kb.stack = old


def rope_AB(g, gcol, gswcol, AB, AB_b):
    kb, nc = g.kb, g.nc
    kb.op("dve", [g.cs_b, g.par_b], [AB_b], nc.vector.tensor_scalar, out=AB[:, 0, :], in0=g.cs[:, 0, :],
          scalar1=pcol(g, gcol, 64), scalar2=None, op0=ALU.mult)
    kb.op("dve", [g.cs_b, g.par_b], [AB_b], nc.vector.tensor_scalar, out=AB[:, 1, :], in0=g.cs[:, 1, :],
          scalar1=pcol(g, gswcol, 64), scalar2=None, op0=ALU.mult)


def shared_kv(g):
    kb, nc = g.kb, g.nc
    with contextlib.ExitStack() as st:
        old = kb.stack
        kb.stack = st
        hT = kb.sb("hT", [128, KC, TOK], BF16); hT_b = [kb.buf("hT0"), kb.buf("hT1")]
        ckv = kb.sb("ckv", [128, 2, TOK], BF16); ckv_b = [kb.buf("ckv0"), kb.buf("ckv1")]
        Rk = kb.sb("Rk", [64, TOK], F32); Rk_b = [kb.buf("Rk0"), kb.buf("Rk1")]
        sqpe = kb.sb("sqpe", [64, TOK], BF16); sqpe_b = [kb.buf("sqpe0"), kb.buf("sqpe1")]
        AB = kb.sb("ABk", [64, 2, TOK], F32); AB_b = kb.buf("ABk")
        kst = [kb.sb(f"kst{i}", [128, TOK], BF16) for i in range(2)]
        kst_b = [kb.buf(f"kst{i}") for i in range(2)]
        krst = [kb.sb(f"krst{i}", [64, TOK], BF16) for i in range(2)]
        krst_b = [kb.buf(f"krst{i}") for i in range(2)]
        vst = [kb.sb(f"vst{i}", [128, 512], BF16) for i in range(2)]
        vst_b = [kb.buf(f"vst{i}") for i in range(2)]
        for b in kst_b + krst_b + vst_b:
            kb._dbufs.append(b)
        alloc_ring(g, 8192)
        norm_x(g, PC_GKV, hT, hT_b)
        rope_AB(g, PC_GKN_R, PC_GKN_RS, AB, AB_b)
        wav = g.w_kv_a.rearrange("(kc p) m -> p kc m", p=128)
        (wa,), wab = load_w(g, [((128, KC, 320), wav)])
        wkbv = g.w_kv_b.rearrange("(kc p) m -> p kc m", p=128)
        (wb0, wb1), wbb = load_w(g, [((128, 3072), wkbv[:, 0, :]), ((128, 3072), wkbv[:, 1, :])])
        wbv = [wb0.rearrange("p (h c) -> p h c", h=12), wb1.rearrange("p (h c) -> p h c", h=12)]
        for t in range(NTL):
            sl = slice(t * TL, (t + 1) * TL)
            pl = []
            for c in range(2):
                pt, pb = bank(g)
                kb.mm(pb, pt[:, :], [(wa[:, kc, c * 128:(c + 1) * 128], hT[:, kc, sl]) for kc in range(KC)],
                      [wab, hT_b[t]])
                pl.append((pt, pb))
            rs, rsb = sumsq_rstd(g, [(pl[0][0][:, :], 128), (pl[1][0][:, :], 128)], TL, 256,
                                 [pl[0][1], pl[1][1]])
            for c in range(2):
                kb.op("dve", [pl[c][1], rsb, g.par_b], [ckv_b[t]], nc.vector.scalar_tensor_tensor,
                      out=ckv[:, c, sl], in0=pl[c][0][:, :], scalar=pcol(g, PC_GKVA + c), in1=rs[:],
                      op0=ALU.mult, op1=ALU.mult)
            pp, ppb = bank(g)
            kb.mm(ppb, pp[:64, :], [(wa[:, kc, 256:320], hT[:, kc, sl]) for kc in range(KC)], [wab, hT_b[t]])
            pq, pqb = bank(g)
            kb.mm_open([wab, hT_b[t]], [pqb])
            ins = None
            for kc in range(KC):
                nc.tensor.matmul(pq[0:32, :], wa[:, kc, 288:320], hT[:, kc, sl], start=(kc == 0), stop=(kc == KC - 1))
                ins = nc.tensor.matmul(pq[32:64, :], wa[:, kc, 256:288], hT[:, kc, sl], start=(kc == 0),
                                       stop=(kc == KC - 1))
                kb.n_inst += 2
            kb.mm_close(ins, [wab, hT_b[t]], [pqb])
            kb.op("act", [ppb], [sqpe_b[t]], nc.scalar.activation, out=sqpe[:, sl], in_=pp[:64, :], func=AF.Square)
            r1, r1b = scratch(g, "fs")
            kb.op("dve", [ppb, AB_b], [r1b], nc.vector.tensor_tensor, out=r1[:64, :TL], in0=pp[:64, :],
                  in1=AB[:, 0, sl], op=ALU.mult)
            kb.op("dve", [pqb, AB_b], [Rk_b[t]], nc.vector.tensor_tensor, out=Rk[:, sl], in0=pq[:64, :],
                  in1=AB[:, 1, sl], op=ALU.mult)
            kb.op("dve", [r1b, Rk_b[t]], [Rk_b[t]], nc.vector.tensor_tensor, out=Rk[:, sl], in0=Rk[:, sl],
                  in1=r1[:64, :TL], op=ALU.add)
        for h in range(12):
            si = h % 2
            for t in range(NTL):
                sl = slice(t * TL, (t + 1) * TL)
                pt, pb = bank(g)
                kb.mm(pb, pt[:, :], [(wbv[c][:, h, 0:128], ckv[:, c, sl]) for c in range(2)], [wbb, ckv_b[t]])
                ps_, psb_ = bank(g)
                sq, sqb = scratch(g, "sq")
                kb.op("act", [pb], [sqb], nc.scalar.activation, out=sq[:], in_=pt[:, :], func=AF.Square)
                kb.mm_open([sqb, sqpe_b[t], g.cst_b], [psb_])
                nc.tensor.matmul(ps_[:, :], g.ones[:, :], sq[:], start=True, stop=False)
                ins = nc.tensor.matmul(ps_[:, :], g.ones[:64, :], sqpe[:, sl], start=False, stop=True)
                kb.n_inst += 2
                kb.mm_close(ins, [sqb, sqpe_b[t]], [psb_])
                rs, rsb = scratch(g, "rs")
                kb.op("act", [psb_], [rsb], nc.scalar.activation, out=rs[:], in_=ps_[:, :], func=AF.Sqrt,
                      scale=float(1.0 / 192), bias=float(EPS))
                kb.op("dve", [rsb], [rsb], nc.vector.reciprocal, rs[:], rs[:])
                kb.op("dve", [pb, rsb, g.par_b], [kst_b[si]], nc.vector.scalar_tensor_tensor, out=kst[si][:, sl],
                      in0=pt[:, :], scalar=pcol(g, PC_GKN_N), in1=rs[:], op0=ALU.mult, op1=ALU.mult)
                kb.op("dve", [Rk_b[t], rsb], [krst_b[si]], nc.vector.tensor_tensor, out=krst[si][:, sl],
                      in0=Rk[:, sl], in1=rs[:64, :], op=ALU.mult)
            kb.dma("sp", g.kt_own(h)[0:128, :], kst[si][:], kst_b[si], [kst_b[si]], [])
            kb.dma("sp", g.kt_own(h)[128:192, :], krst[si][:], krst_b[si], [krst_b[si]], [])
        vi = 0
        for tb in range(TOK // 128):
            t = tb // 4
            for hg in range(3):
                pt, pb = bank(g)
                kb.mm(pb, pt[:, :], [(ckv[:, c, tb * 128:(tb + 1) * 128], wbv[c][:, hg * 4:(hg + 1) * 4, 128:256])
                                      for c in range(2)], [wbb, ckv_b[t]])
                s = vi % 2
                vi += 1
                kb.op("act", [pb], [vst_b[s]], nc.scalar.copy, vst[s][:], pt[:, :])
                for hq in range(4):
                    kb.dma("sp", g.v_own(hg * 4 + hq)[tb * 128:(tb + 1) * 128, :], vst[s][:, hq * 128:(hq + 1) * 128],
                           vst_b[s], [vst_b[s]], [])
        kb.barrier()
        kb.stack = old


def exchange_kv(g):
    kb, nc = g.kb, g.nc
    E = kb.E["pool"]
    kb.barrier()
    cs = kb.new_sem("cc")
    groups = [[0, 1, 2, 3], [4, 5, 6, 7]]
    n = 0
    for h in range(12):
        for src, dst in ((g.KT_own_l[h], g.KTg_l[h]), (g.V_own_l[h], g.Vg_l[h])):
            ins = nc.gpsimd.collective_compute("AllGather", ALU.bypass, replica_groups=groups,
                                               ins=[src.opt()], outs=[dst.opt()])
            ins.then_inc(cs)
            n += 1
    for En in kb.E.values():
        En.h.wait_ge(cs, n)


def causal_attention(g, cqn, cqn_b, mixT, mix_b):
    kb, nc = g.kb, g.nc
    sc = float(192 ** -0.5)
    with contextlib.ExitStack() as st:
        old = kb.stack
        kb.stack = st
        NKV = 2
        kn = [kb.sb(f"kn{i}", [128, SEQ], BF16) for i in range(NKV)]
        kr = [kb.sb(f"kr{i}", [64, SEQ], BF16) for i in range(NKV)]
        vv = [kb.sb(f"vv{i}", [128, 32, 128], BF16) for i in range(NKV)]
        kv_b = [kb.buf(f"kv{i}") for i in range(NKV)]
        wqh = [kb.sb(f"wqh{i}", [128, 4, 192], BF16) for i in range(2)]
        wqh_b = [kb.buf(f"wqh{i}") for i in range(2)]
        qn = [kb.sb(f"qn{i}", [128, TOK], BF16) for i in range(2)]
        qr = [kb.sb(f"qr{i}", [64, TOK], BF16) for i in range(2)]
        q_b = [[kb.buf(f"q{i}_{t}") for t in range(NTL)] for i in range(2)]
        AB = kb.sb("ABq", [64, 2, TOK], F32); AB_b = kb.buf("ABq")
        qidx = kb.sb("qidx", [128, TOK], F32); qidx_b = kb.buf("qidx")
        for b in kv_b + wqh_b + [qidx_b]:
            kb._dbufs.append(b)
        kb.dma("sp", qidx[:], g.qidx.partition_broadcast(128).rearrange("p a n -> p (a n)"), qidx_b, [], [qidx_b])
        rope_AB(g, PC_GQN_R, PC_GQN_RS, AB, AB_b)
        wqv = g.w_q_b.rearrange("(kc p) m -> p kc m", p=128)
        g.bank_set = [4, 5, 6, 7]
        si = 0
        for h in range(12):
            par = h % 2
            b = kv_b[par]
            E = kb.E["sp"]
            if getattr(g, "kv_tok", None) is not None:
                kb._wait(E, g.kv_tok)
            kb._sync(E, [], [b])
            if b.dsem is None:
                b.dsem = kb.new_sem("d_" + b.name)
            if b.dcount:
                kb._wait(E, (b.dsem, b.dcount, None))
            for i in range(8):
                r, off = min(i, 7 - i), (512 if i >= 4 else 0)
                srcs = [(kn[par][:, i * 512:(i + 1) * 512], g.kt_g(h, r)[0:128, off:off + 512]),
                        (kr[par][:, i * 512:(i + 1) * 512], g.kt_g(h, r)[128:192, off:off + 512]),
                        (vv[par][:, i * 4:(i + 1) * 4, :],
                         g.v_g(h, r)[off:off + 512, :].rearrange("(kb p) d -> p kb d", p=128))]
                for o_, i_ in srcs:
                    ins = nc.sync.dma_start(out=o_, in_=i_)
                    b.dcount += 16
                    ins.then_inc(b.dsem, 16)
                    kb.n_inst += 1
            kb._commit((b.dsem, b.dcount, None), [], [b])
            kb.dma("pool", wqh[par][:], wqv[:, :, h * 192:(h + 1) * 192], wqh_b[par], [], [wqh_b[par]])
            w = wqh[par]
            for t in range(NTL):
                sl = slice(t * TL, (t + 1) * TL)
                pn, pnb = bank(g)
                kb.mm(pnb, pn[:, :], [(w[:, kc, 0:128], cqn[:, kc, sl]) for kc in range(4)], [wqh_b[par], cqn_b[t]])
                pr, prb = bank(g)
                kb.mm(prb, pr[:64, :], [(w[:, kc, 128:192], cqn[:, kc, sl]) for kc in range(4)], [wqh_b[par], cqn_b[t]])
                pq, pqb = bank(g)
                kb.mm_open([wqh_b[par], cqn_b[t]], [pqb])
                ins = None
                for kc in range(4):
                    nc.tensor.matmul(pq[0:32, :], w[:, kc, 160:192], cqn[:, kc, sl], start=(kc == 0), stop=(kc == 3))
                    ins = nc.tensor.matmul(pq[32:64, :], w[:, kc, 128:160], cqn[:, kc, sl], start=(kc == 0),
                                           stop=(kc == 3))
                    kb.n_inst += 2
                kb.mm_close(ins, [wqh_b[par], cqn_b[t]], [pqb])
                rs, rsb = sumsq_rstd(g, [(pn[:, :], 128), (pr[:64, :], 64)], TL, 192, [pnb, prb])
                kb.op("dve", [pnb, rsb, g.par_b], [q_b[par][t]], nc.vector.scalar_tensor_tensor, out=qn[par][:, sl],
                      in0=pn[:, :], scalar=pcol(g, PC_GQN_N), in1=rs[:], op0=ALU.mult, op1=ALU.mult)
                r1, r1b = scratch(g, "fs")
                kb.op("dve", [prb, AB_b], [r1b], nc.vector.tensor_tensor, out=r1[:64, :TL], in0=pr[:64, :],
                      in1=AB[:, 0, sl], op=ALU.mult)
                r2, r2b = scratch(g, "fs")
                kb.op("dve", [pqb, AB_b], [r2b], nc.vector.tensor_tensor, out=r2[:64, :TL], in0=pq[:64, :],
                      in1=AB[:, 1, sl], op=ALU.mult)
                kb.op("dve", [r1b, r2b], [r1b], nc.vector.tensor_tensor, out=r1[:64, :TL], in0=r1[:64, :TL],
                      in1=r2[:64, :TL], op=ALU.add)
                kb.op("dve", [r1b, rsb], [q_b[par][t]], nc.vector.tensor_tensor, out=qr[par][:, sl],
                      in0=r1[:64, :TL], in1=rs[:64, :], op=ALU.mult)
            for t in range(NTL):
                sl = slice(t * TL, (t + 1) * TL)
                nkb = 16 if t == 0 else 32
                po, pob = g.pst[2], g.psb[2]
                pd, pdb = g.pst[3], g.psb[3]
                for kbi in range(nkb):
                    S, Sb = g.pst[si % 2], g.psb[si % 2]
                    si += 1
                    ks = slice(kbi * 128, (kbi + 1) * 128)
                    kb.mm(Sb, S[:, :], [(kn[par][:, ks], qn[par][:, sl]), (kr[par][:, ks], qr[par][:, sl])],
                          [kv_b[par], q_b[par][t]])
                    e, eb = scratch(g, "sq")
                    kb.op("act", [Sb], [eb], nc.scalar.activation, out=e[:], in_=S[:, :], func=AF.Exp, scale=sc)
                    if t == 0 or kbi >= 16:
                        em, emb = scratch(g, "sq")
                        kb.op("dve", [qidx_b, g.par_b, eb], [emb], nc.vector.scalar_tensor_tensor, out=em[:],
                              in0=qidx[:, sl], scalar=pcol(g, PC_KIDX + kbi), in1=e[:], op0=ALU.is_ge, op1=ALU.mult)
                        e, eb = em, emb
                    kb.mm_open([eb, kv_b[par], g.cst_b], [pob, pdb] if kbi == 0 else [])
                    nc.tensor.matmul(po[:, :], vv[par][:, kbi, :], e[:], start=(kbi == 0), stop=(kbi == nkb - 1))
                    ins = nc.tensor.matmul(pd[:, :], g.ones[:, :], e[:], start=(kbi == 0), stop=(kbi == nkb - 1))
                    kb.n_inst += 2
                    kb.mm_close(ins, [eb, kv_b[par]], [pob, pdb])
                rd, rdb = scratch(g, "rs")
                kb.op("dve", [pdb], [rdb], nc.vector.reciprocal, rd[:], pd[:, :])
                kb.op("dve", [pob, rdb], [mix_b[t]], nc.vector.tensor_tensor, out=mixT[:, h, sl], in0=po[:, :],
                      in1=rd[:], op=ALU.mult)
        g.bank_set = list(range(8))
        kb.barrier()
        kb.stack = old


def moe(g):
    kb, nc = g.kb, g.nc
    with contextlib.ExitStack() as st:
        old = kb.stack
        kb.stack = st
        alloc_ring(g, 12288)
        hT = kb.sb("hT", [128, KC, TOK], BF16); hT_b = [kb.buf("hT0"), kb.buf("hT1")]
        selsb = kb.sb("selsb", [8, NEXP * 128], F32); sel_b = kb.buf("selsb"); kb._dbufs.append(sel_b)
        wr = kb.sb("wr", [128, KC, NEXP], F32); wr_b = kb.buf("wr"); kb._dbufs.append(wr_b)
        rtok = kb.sb("rtok", [128, 8], F32); rtok_b = kb.buf("rtok")
        lg = kb.sb("lg", [128, 8, NEXP], F32); lg_b = kb.buf("lg")
        t8 = kb.sb("t8", [128, 8, 8], F32); t8_b = kb.buf("t8")
        wts = kb.sb("wts", [128, 4, 8], F32); wts_b = kb.buf("wts")
        m1 = kb.sb("m1", [128, NEXP], F32); m1_b = kb.buf("m1")
        gates = kb.sb("gates", [128, 8, NEXP], F32); gates_b = kb.buf("gates")
        gT = kb.sb("gT", [8, TOK], F32); gT_b = kb.buf("gT")
        G = [kb.sb(f"G{i}", [128, TOK], F32) for i in range(2)]
        G_b = [[kb.buf(f"G{i}_{t}") for t in range(NTL)] for i in range(2)]
        act = [kb.sb(f"act{t}", [128, 2, TL], BF16) for t in range(NTL)]
        act_b = [[kb.buf(f"act{t}_{m}") for m in range(2)] for t in range(NTL)]
        kb.dma("sp", selsb[:], g.sel, sel_b, [], [sel_b])
        kb.dma("sp", wr[:], g.w_router.rearrange("(kc p) e -> p kc e", p=128), wr_b, [], [wr_b])
        for kc in range(KC):
            kb.op("dve", [wr_b, g.par_b], [wr_b], nc.vector.tensor_scalar, out=wr[:, kc, :], in0=wr[:, kc, :],
                  scalar1=pcol(g, PC_GFFN1 + kc), scalar2=None, op0=ALU.mult)

        def on_rstd(t, rs, rsb):
            for q in range(4):
                pt, pb = bank(g)
                kb.mm_open([rsb, g.cst_b], [pb])
                ins = nc.tensor.transpose(pt[:, 0:128], rs[:, q * 128:(q + 1) * 128], g.idf[:])
                kb.n_inst += 1
                kb.mm_close(ins, [rsb], [pb])
                kb.op("dve", [pb], [rtok_b], nc.vector.tensor_copy, rtok[:, t * 4 + q:t * 4 + q + 1], pt[:, 0:1])

        norm_x(g, PC_GFFN1, hT, hT_b, on_rstd=on_rstd)
        for tb in range(8):
            t = tb // 4
            pt, pb = bank(g)
            kb.mm(pb, pt[:, 0:NEXP], [(g.x[:, kc, tb * 128:(tb + 1) * 128], wr[:, kc, :]) for kc in range(KC)],
                  [wr_b] + [g.xb[kc][t] for kc in range(KC)])
            kb.op("dve", [pb, rtok_b], [lg_b], nc.vector.tensor_scalar, out=lg[:, tb, :], in0=pt[:, 0:NEXP],
                  scalar1=rtok[:, tb:tb + 1], scalar2=None, op0=ALU.mult)
            kb.op("dve", [lg_b], [t8_b], nc.vector.max, out=t8[:, tb, :], in_=lg[:, tb, :])
        kb.op("dve", [t8_b], [wts_b], nc.vector.tensor_tensor, out=wts[:, 0, :], in0=t8[:, :, 1], in1=t8[:, :, 0],
              op=ALU.subtract)
        kb.op("act", [wts_b], [wts_b], nc.scalar.activation, out=wts[:, 1, :], in_=wts[:, 0, :], func=AF.Exp)
        kb.op("dve", [wts_b], [wts_b], nc.vector.tensor_scalar, out=wts[:, 2, :], in0=wts[:, 1, :], scalar1=1.0,
              scalar2=None, op0=ALU.add)
        kb.op("dve", [wts_b], [wts_b], nc.vector.reciprocal, wts[:, 2, :], wts[:, 2, :])
        kb.op("dve", [wts_b], [wts_b], nc.vector.tensor_tensor, out=wts[:, 3, :], in0=wts[:, 1, :], in1=wts[:, 2, :],
              op=ALU.mult)
        for tb in range(8):
            kb.op("dve", [lg_b, t8_b, wts_b], [m1_b], nc.vector.tensor_scalar, out=m1[:], in0=lg[:, tb, :],
                  scalar1=t8[:, tb, 0:1], scalar2=wts[:, 2, tb:tb + 1], op0=ALU.is_equal, op1=ALU.mult)
            kb.op("dve", [lg_b, t8_b, wts_b], [gates_b], nc.vector.tensor_scalar, out=gates[:, tb, :], in0=lg[:, tb, :],
                  scalar1=t8[:, tb, 1:2], scalar2=wts[:, 3, tb:tb + 1], op0=ALU.is_equal, op1=ALU.mult)
            kb.op("dve", [m1_b, gates_b], [gates_b], nc.vector.tensor_tensor, out=gates[:, tb, :], in0=gates[:, tb, :],
                  in1=m1[:], op=ALU.add)
        for t in range(NTL):
            pt, pb = bank(g)
            kb.mm_open([gates_b, g.cst_b], [pb])
            ins = None
            for q in range(4):
                ins = nc.tensor.transpose(pt[0:NEXP, q * 128:(q + 1) * 128], gates[:, t * 4 + q, :], g.idf[:])
                kb.n_inst += 1
            kb.mm_close(ins, [gates_b], [pb])
            kb.op("act", [pb], [gT_b], nc.scalar.copy, gT[:, t * TL:(t + 1) * TL], pt[0:NEXP, :])
        for e in range(NEXP):
            gi_ = e % 2
            for t in range(NTL):
                sl = slice(t * TL, (t + 1) * TL)
                pt, pb = bank(g)
                kb.mm(pb, pt[:, :], [(selsb[:, e * 128:(e + 1) * 128], gT[:, sl])], [sel_b, gT_b])
                kb.op("act", [pb], [G_b[gi_][t]], nc.scalar.copy, G[gi_][:, sl], pt[:, :])
            for gi in range(DFF // 256):
                wg, wu, wd, wb = ffn_weights(g, g.moe_gate[e], g.moe_up[e], g.moe_down[e], gi)
                ffn_group(g, wg, wu, wd, wb, hT, hT_b, gate_bc=G[gi_], gate_bcb=G_b[gi_], act=act, act_b=act_b)
        ffn_drain(g)
        kb.barrier()
        kb.stack = old


def layer1(g):
    kb, nc = g.kb, g.nc
    with contextlib.ExitStack() as st0:
        old0 = kb.stack
        kb.stack = st0
        kmT = kb.sb("kmT", [128, 4, 256], BF16); kmT_b = kb.buf("kmT")
        vm = kb.sb("vm", [128, 2, 512], BF16); vm_b = kb.buf("vm")
        with contextlib.ExitStack() as st:
            kb.stack = st
            alloc_ring(g, 8192)
            mem_kv(g, 1, kmT, kmT_b, vm, vm_b)
            kb.stack = st0
        mixT = kb.sb("mixT", [128, KC, TOK], BF16); mix_b = [kb.buf("mix0"), kb.buf("mix1")]
        cqn = kb.sb("cqn", [128, 4, TOK], BF16); cqn_b = [kb.buf("cqn0"), kb.buf("cqn1")]
        with contextlib.ExitStack() as st:
            kb.stack = st
            alloc_ring(g, 8192)
            hT = kb.sb("hT", [128, KC, TOK], BF16); hT_b = [kb.buf("hT0"), kb.buf("hT1")]
            norm_x(g, PC_GMIX1, hT, hT_b)
            bw = g.b_w_in.rearrange("(kc p) m -> p kc m", p=128)
            (wqa,), wqab = load_w(g, [((128, KC, 512), bw[:, :, 0:512])])
            for t in range(NTL):
                sl = slice(t * TL, (t + 1) * TL)
                pl = []
                for c in range(4):
                    pt, pb = bank(g)
                    kb.mm(pb, pt[:, :], [(wqa[:, kc, c * 128:(c + 1) * 128], hT[:, kc, sl]) for kc in range(KC)],
                          [wqab, hT_b[t]])
                    pl.append((pt, pb))
                rs, rsb = sumsq_rstd(g, [(p[0][:, :], 128) for p in pl], TL, 512, [p[1] for p in pl])
                for c in range(4):
                    kb.op("dve", [pl[c][1], rsb, g.par_b], [cqn_b[t]], nc.vector.scalar_tensor_tensor,
                          out=cqn[:, c, sl], in0=pl[c][0][:, :], scalar=pcol(g, PC_GQA + c), in1=rs[:],
                          op0=ALU.mult, op1=ALU.mult)
            (wqm,), wqmb = load_w(g, [((128, KC, 512), bw[:, :, 512:1024])])
            mem_attention(g, wqm, wqmb, hT, hT_b, PC_GMQ1, kmT, kmT_b, vm, vm_b, mixT, mix_b)
            kb.barrier()
            kb.stack = st0
        causal_attention(g, cqn, cqn_b, mixT, mix_b)
        with contextlib.ExitStack() as st:
            kb.stack = st
            alloc_ring(g, 8192)
            out_proj(g, 1, mixT, mix_b)
            kb.barrier()
            kb.stack = st0
        kb.stack = old0
    moe(g)


def _core_tokens(core):
    b, c = core // 4, core % 4
    iA, iB = c, 7 - c
    idx = np.concatenate([np.arange(512 * iA, 512 * iA + 512), np.arange(512 * iB, 512 * iB + 512)])
    return b, (iA, iB), idx


def _chunk(v):
    return np.ascontiguousarray(v.reshape(-1, 128).T)


def _params(inp, core):
    b, (iA, iB), idx = _core_tokens(core)
    P = np.zeros((128, NPC), np.float32)
    P[:, PC_GMIX0:PC_GMIX0 + 16] = _chunk(inp["g_mix"][0])
    P[:, PC_GFFN0:PC_GFFN0 + 16] = _chunk(inp["g_ffn"][0])
    P[:, PC_GKV:PC_GKV + 16] = _chunk(inp["g_kv"])
    P[:, PC_GMIX1:PC_GMIX1 + 16] = _chunk(inp["g_mix"][1])
    P[:, PC_GFFN1:PC_GFFN1 + 16] = _chunk(inp["g_ffn"][1])
    cw = inp["a_conv_w"][0]
    P[:, PC_CONV:PC_CONV + 36] = cw.reshape(3, 12, 128).transpose(2, 1, 0).reshape(128, 36)
    P[:, PC_GMQ0] = inp["g_mq"][0]
    P[:, PC_GMK0] = inp["g_mk"][0]
    P[:, PC_GMQ1] = inp["g_mq"][1]
    P[:, PC_GMK1] = inp["g_mk"][1]
    P[:, PC_GQA:PC_GQA + 4] = _chunk(inp["b_g_q_a"][0])
    P[:, PC_GKVA:PC_GKVA + 2] = _chunk(inp["g_kv_a"])
    gq, gk = inp["b_g_qn"][0], inp["g_kn"]
    P[:, PC_GQN_N] = gq[:128]
    P[:, PC_GKN_N] = gk[:128]
    P[:64, PC_GQN_R] = gq[128:]
    P[:64, PC_GQN_RS] = np.concatenate([gq[160:], gq[128:160]])
    P[:64, PC_GKN_R] = gk[128:]
    P[:64, PC_GKN_RS] = np.concatenate([gk[160:], gk[128:160]])
    P[:32, PC_SGN] = -1.0
    P[32:64, PC_SGN] = 1.0
    invf = (1.0 / (10000.0 ** (np.arange(0, 64, 2, dtype=np.float32) / np.float32(64)))).astype(np.float32)
    P[:64, PC_INVF] = np.concatenate([invf, invf])
    P[:, PC_HALO] = 0.0 if iA == 0 else 1.0
    P[:, PC_HALO + 1] = 1.0
    P[:, PC_KIDX:PC_KIDX + 32] = (np.arange(32)[None, :] * 128 + np.arange(128)[:, None]).astype(np.float32)
    return P


def _consts():
    ident = np.eye(128, dtype=np.float32)
    sel = np.zeros((8, NEXP * 128), np.float32)
    for e in range(NEXP):
        sel[e, e * 128:(e + 1) * 128] = 1.0
    return ident, sel


def host_inputs(inp, core, mode):
    b, (iA, iB), idx = _core_tokens(core)
    ident, sel = _consts()
    m = {
        "params": _params(inp, core), "ident": ident, "sel": sel,
        "mem": np.ascontiguousarray(inp["mem"][b]), "g_mem": inp["g_mem"],
        "w_mem_kv": inp["w_mem_kv"], "w_out": inp["w_out"],
        "pos": np.ascontiguousarray(inp["positions"][b, idx][None, :]).astype(np.int32),
    }
    if mode in ("L0", "fused"):
        m["xT"] = np.ascontiguousarray(inp["x"][b, idx, :].T)
        xh = np.zeros((4, D), np.float32)
        for t, i in enumerate((iA, iB)):
            if i > 0:
                xh[2 * t:2 * t + 2] = inp["x"][b, 512 * i - 2:512 * i, :]
        m["xhT"] = np.ascontiguousarray(xh.T)
        m["a_w_in"] = inp["a_w_in"][0]
        m["w_kv_a"] = inp["w_kv_a"]
        m["w_kv_b"] = inp["w_kv_b"]
        m["ffn_gate"] = inp["ffn_w_gate"][0]
        m["ffn_up"] = inp["ffn_w_up"][0]
        m["ffn_down"] = inp["ffn_w_down"][0]
    if mode in ("L1", "fused"):
        m["qidx"] = idx[None, :].astype(np.float32)
        m["b_w_in"] = inp["b_w_in"][0]
        m["w_q_b"] = inp["b_w_q_b"][0]
        m["w_router"] = inp["moe_w_router"][0]
        m["moe_gate"] = inp["moe_w_gate"][0]
        m["moe_up"] = inp["moe_w_up"][0]
        m["moe_down"] = inp["moe_w_down"][0]
    return m


_PROGS = {}


def _prog(mode):
    if mode not in _PROGS:
        _PROGS[mode] = build(mode)[0]
    return _PROGS[mode]


FUSED = True


def kernel(**inputs):
    inp = {k: np.asarray(v) for k, v in inputs.items()}
    cores = list(range(NCORE))
    if FUSED:
        maps = [host_inputs(inp, c, "fused") for c in cores]
        res = run_bass_kernel_spmd(_prog("fused"), maps, core_ids=cores).results
    else:
        maps0 = [host_inputs(inp, c, "L0") for c in cores]
        r0 = run_bass_kernel_spmd(_prog("L0"), maps0, core_ids=cores).results
        maps1 = []
        for c in cores:
            b = c // 4
            m = host_inputs(inp, c, "L1")
            m["xT"] = np.asarray(r0[c]["x1T"])
            m["KTg"] = np.concatenate([np.asarray(r0[4 * b + r]["KT_own"]) for r in range(4)], axis=0)
            m["Vg"] = np.concatenate([np.asarray(r0[4 * b + r]["V_own"]) for r in range(4)], axis=0)
            maps1.append(m)
        res = run_bass_kernel_spmd(_prog("L1"), maps1, core_ids=cores).results
    out = np.zeros((NB, SEQ, D), np.float32)
    for c in cores:
        b, _, idx = _core_tokens(c)
        out[b, idx, :] = np.asarray(res[c]["yT"]).T
    return out
```

```python
import contextlib
import numpy as np
import concourse.bass as bass
import concourse.mybir as mybir
from concourse.bass_utils import run_bass_kernel_spmd

F32 = mybir.dt.float32
BF16 = mybir.dt.bfloat16
I32 = mybir.dt.int32
AF = mybir.ActivationFunctionType
ALU = mybir.AluOpType


class Buf:
    __slots__ = ("name", "writer", "readers", "dsem", "dcount")

    def __init__(self, name):
        self.name = name
        self.writer = None
        self.readers = []
        self.dsem = None
        self.dcount = 0


class Eng:
    def __init__(self, name, h, sem):
        self.name = name
        self.h = h
        self.sem = sem
        self.cnt = 0
        self.known = {}


class KB:
    def __init__(self, nc, stack):
        self.nc = nc
        self.stack = stack
        self.gstack = stack
        self.nsem = 0
        self.E = {}
        for name, h in (("pe", nc.tensor), ("act", nc.scalar), ("dve", nc.vector),
                        ("pool", nc.gpsimd), ("sp", nc.sync)):
            self.E[name] = Eng(name, h, self.new_sem("e_" + name))
        self.nbuf = 0
        self.n_inst = 0

    def new_sem(self, name):
        self.nsem += 1
        return self.gstack.enter_context(self.nc.semaphore(f"{name}_{self.nsem}"))

    def buf(self, name=None):
        self.nbuf += 1
        return Buf(name or f"b{self.nbuf}")

    def sb(self, name, shape, dt):
        self.nbuf += 1
        return self.stack.enter_context(self.nc.sbuf_tensor(f"{name}_{self.nbuf}", list(shape), dt))

    def ps(self, name, shape, dt=F32):
        return self.stack.enter_context(self.nc.psum_tensor(name, list(shape), dt))

    def _wait(self, E, tok):
        sem, val, src = tok
        if src is E and E.name == "pe":
            return
        key = id(sem)
        if E.known.get(key, 0) >= val:
            return
        E.h.wait_ge(sem, val)
        E.known[key] = val

    def _sync(self, E, reads, writes):
        for b in reads:
            if b.writer is not None:
                self._wait(E, b.writer)
        for b in writes:
            if b.writer is not None and b.writer[2] is not E:
                self._wait(E, b.writer)
            for r in b.readers:
                if r[2] is not E:
                    self._wait(E, r)

    def _commit(self, tok, reads, writes):
        for b in reads:
            b.readers.append(tok)
            if len(b.readers) > 64:
                best = {}
                for t in b.readers:
                    k = id(t[0])
                    if k not in best or best[k][1] < t[1]:
                        best[k] = t
                b.readers = list(best.values())
        for b in writes:
            b.writer = tok
            b.readers = []

    def op(self, eng, reads, writes, fn, *a, **kw):
        E = self.E[eng]
        self._sync(E, reads, writes)
        ins = fn(*a, **kw)
        E.cnt += 1
        ins.then_inc(E.sem, 1)
        self.n_inst += 1
        self._commit((E.sem, E.cnt, E), reads, writes)
        return ins

    def mm(self, out_buf, out_ap, pairs, reads, transpose=False):
        E = self.E["pe"]
        self._sync(E, reads, [out_buf])
        n = len(pairs)
        ins = None
        for i, (l, r) in enumerate(pairs):
            ins = self.nc.tensor.matmul(out_ap, l, r, start=(i == 0), stop=(i == n - 1))
            self.n_inst += 1
        E.cnt += 1
        ins.then_inc(E.sem, 1)
        self._commit((E.sem, E.cnt, E), reads, [out_buf])

    def mm_open(self, reads, writes):
        E = self.E["pe"]
        self._sync(E, reads, writes)

    def mm_close(self, ins, reads, writes):
        E = self.E["pe"]
        E.cnt += 1
        ins.then_inc(E.sem, 1)
        self._commit((E.sem, E.cnt, E), reads, writes)

    def dma(self, q, out_ap, in_ap, sbuf_buf, reads, writes, **kw):
        E = self.E[q]
        b = sbuf_buf
        if b.dsem is None:
            b.dsem = self.new_sem("d_" + b.name)
        self._sync(E, reads, writes)
        if b.dcount:
            self._wait(E, (b.dsem, b.dcount, None))
        ins = E.h.dma_start(out=out_ap, in_=in_ap, **kw)
        b.dcount += 16
        ins.then_inc(b.dsem, 16)
        self.n_inst += 1
        tok = (b.dsem, b.dcount, None)
        self._commit(tok, reads, writes)
        return tok

    def wait_tok(self, eng, tok):
        self._wait(self.E[eng], tok)


    def barrier(self):
        toks = [(E.sem, E.cnt, E) for E in self.E.values() if E.cnt > 0]
        toks += [(b.dsem, b.dcount, None) for b in self._dbufs if b.dcount > 0]
        for E in self.E.values():
            for t in toks:
                if t[2] is E:
                    continue
                self._wait(E, t)


D = 2048
SEQ = 4096
NB = 2
NCORE = 8
TOK = 1024
TL = 512
NTL = 2
KC = 16
DFF = 7168
NEXP = 8
EPS = 1e-6
NPC = 168

PC_GMIX0, PC_GFFN0, PC_GKV, PC_GMIX1, PC_GFFN1 = 0, 16, 32, 48, 64
PC_CONV = 80
PC_GMQ0, PC_GMK0, PC_GMQ1, PC_GMK1 = 116, 117, 118, 119
PC_GQA = 120
PC_GKVA = 124
PC_GQN_N, PC_GKN_N = 126, 127
PC_GQN_R, PC_GQN_RS, PC_GKN_R, PC_GKN_RS, PC_SGN, PC_INVF = 128, 129, 130, 131, 132, 133
PC_HALO = 134
PC_KIDX = 136


class Ctx:
    pass


def build(mode):
    nc = bass.Bass("TRN2", target_bir_lowering=False)
    do0 = mode in ("L0", "fused")
    do1 = mode in ("L1", "fused")

    def din(name, shape, dt=F32):
        return nc.dram_tensor(name, list(shape), dt, kind="ExternalInput").ap()

    def dout(name, shape, dt=F32):
        return nc.dram_tensor(name, list(shape), dt, kind="ExternalOutput").ap()

    def dint(name, shape, dt=F32):
        return nc.dram_tensor(name, list(shape), dt, kind="Internal").ap()

    g = Ctx()
    g.nc = nc
    g.xT = din("xT", [D, TOK])
    g.params = din("params", [128, NPC])
    g.ident = din("ident", [128, 128])
    g.sel = din("sel", [8, NEXP * 128])
    g.mem = din("mem", [256, D])
    g.g_mem = din("g_mem", [2, D])
    g.w_mem_kv = din("w_mem_kv", [2, D, 1024])
    g.w_out = din("w_out", [2, D, D])
    if do0:
        g.xhT = din("xhT", [D, 4])
        g.pos = din("pos", [1, TOK], I32)
        g.a_w_in = din("a_w_in", [D, 5120])
        g.w_kv_a = din("w_kv_a", [D, 320])
        g.w_kv_b = din("w_kv_b", [256, 3072])
        g.ffn_gate = din("ffn_gate", [D, DFF])
        g.ffn_up = din("ffn_up", [D, DFF])
        g.ffn_down = din("ffn_down", [DFF, D])
    if do1:
        if not do0:
            g.pos = din("pos", [1, TOK], I32)
        g.qidx = din("qidx", [1, TOK])
        g.b_w_in = din("b_w_in", [D, 1024])
        g.w_q_b = din("w_q_b", [512, 2304])
        g.w_router = din("w_router", [D, NEXP])
        g.moe_gate = din("moe_gate", [NEXP, D, DFF])
        g.moe_up = din("moe_up", [NEXP, D, DFF])
        g.moe_down = din("moe_down", [NEXP, DFF, D])
        g.yT = dout("yT", [D, TOK])
    if mode == "L0":
        g.x1T = dout("x1T", [D, TOK])
        KT_own = dout("KT_own", [2304, TOK], BF16)
        V_own = dout("V_own", [TOK, 1536], BF16)
        g.kt_own = lambda h: KT_own[h * 192:(h + 1) * 192, :]
        g.v_own = lambda h: V_own[:, h * 128:(h + 1) * 128]
    elif mode == "L1":
        KTg = din("KTg", [4 * 2304, TOK], BF16)
        Vg = din("Vg", [4 * TOK, 1536], BF16)
        g.kt_g = lambda h, r: KTg[r * 2304 + h * 192:r * 2304 + (h + 1) * 192, :]
        g.v_g = lambda h, r: Vg[r * TOK:(r + 1) * TOK, h * 128:(h + 1) * 128]
    else:
        g.KT_own_l = [dint(f"KT_own{h}", [192, TOK], BF16) for h in range(12)]
        g.V_own_l = [dint(f"V_own{h}", [TOK, 128], BF16) for h in range(12)]
        g.KTg_l = [dint(f"KTg{h}", [4 * 192, TOK], BF16) for h in range(12)]
        g.Vg_l = [dint(f"Vg{h}", [4 * TOK, 128], BF16) for h in range(12)]
        g.kt_own = lambda h: g.KT_own_l[h]
        g.v_own = lambda h: g.V_own_l[h]
        g.kt_g = lambda h, r: g.KTg_l[h][r * 192:(r + 1) * 192, :]
        g.v_g = lambda h, r: g.Vg_l[h][r * TOK:(r + 1) * TOK, :]

    with contextlib.ExitStack() as st:
        kb = KB(nc, st)
        kb._dbufs = []
        g.kb = kb
        setup_globals(g)
        if do0:
            layer0(g)
            shared_kv(g)
            if mode == "L0":
                store_x(g, g.x1T)
        if mode == "fused":
            exchange_kv(g)
        if do1:
            layer1(g)
            store_x(g, g.yT)
        kb.barrier()
        g.n_inst = kb.n_inst
    return nc, g


def setup_globals(g):
    kb, nc = g.kb, g.nc
    g.x = kb.sb("x", [128, KC, TOK], F32)
    g.xb = [[kb.buf(f"x{kc}_{t}") for t in range(NTL)] for kc in range(KC)]
    g.par = kb.sb("par", [128, NPC], F32)
    g.par_b = kb.buf("par")
    g.idf = kb.sb("idf", [128, 128], F32)
    g.idb = kb.sb("idb", [128, 128], BF16)
    g.ones = kb.sb("ones", [128, 128], BF16)
    g.cst_b = kb.buf("cst")
    g.cs = kb.sb("cs", [64, 2, TOK], F32)
    g.cs_b = kb.buf("cs")
    g.pst = [kb.ps(f"ps{i}", [128, 512]) for i in range(8)]
    g.psb = [kb.buf(f"ps{i}") for i in range(8)]
    g.bank_i = 0
    g.bank_set = list(range(8))
    g.ffn_pending = []
    g.sq = [kb.sb(f"sq{i}", [128, 512], BF16) for i in range(4)]
    g.sq_b = [kb.buf(f"sq{i}") for i in range(4)]
    g.sq_i = 0
    g.rs = [kb.sb(f"rs{i}", [128, 512], F32) for i in range(3)]
    g.rs_b = [kb.buf(f"rs{i}") for i in range(3)]
    g.rs_i = 0
    g.fs = [kb.sb(f"fs{i}", [128, 514], F32) for i in range(3)]
    g.fs_b = [kb.buf(f"fs{i}") for i in range(3)]
    g.fs_i = 0

    xb_all = [b for row in g.xb for b in row]
    xv = g.xT.rearrange("(kc p) n -> p kc n", p=128)
    ldb = kb.buf("xload"); kb._dbufs.append(ldb)
    for t in range(NTL):
        kb.dma("sp", g.x[:, :, t * TL:(t + 1) * TL], xv[:, :, t * TL:(t + 1) * TL], ldb, [],
               [g.xb[kc][t] for kc in range(KC)])
    cb = kb.buf("cload"); kb._dbufs.append(cb)
    kb.dma("sp", g.par[:], g.params, cb, [], [g.par_b])
    kb.dma("sp", g.idf[:], g.ident, cb, [], [g.cst_b])
    kb.op("dve", [g.cst_b], [g.cst_b], nc.vector.tensor_copy, g.idb[:], g.idf[:])
    kb.op("dve", [], [g.cst_b], nc.vector.memset, g.ones[:], 1.0)
    rope_tables(g)


def bank(g):
    bs = g.bank_set
    for _ in range(len(bs)):
        i = bs[g.bank_i % len(bs)]
        g.bank_i += 1
        b = g.psb[i]
        if b.writer is None or b.readers:
            return g.pst[i], b
    raise RuntimeError("no free PSUM bank")


def alloc_ring(g, slot_el, nslot=2):
    kb = g.kb
    g.NSLOT = nslot
    g.SLOT = slot_el
    g.ring_gen = getattr(g, "ring_gen", 0) + 1
    g.wslot = [kb.sb(f"wslot{g.ring_gen}_{i}", [128, slot_el], BF16) for i in range(nslot)]
    g.wslot_b = [kb.buf(f"wslot{g.ring_gen}_{i}") for i in range(nslot)]
    for b in g.wslot_b:
        kb._dbufs.append(b)
    g.slot_i = 0


def scratch(g, kind):
    lst, bl, key = {"sq": (g.sq, g.sq_b, "sq_i"), "rs": (g.rs, g.rs_b, "rs_i"),
                    "fs": (g.fs, g.fs_b, "fs_i")}[kind]
    i = getattr(g, key)
    setattr(g, key, (i + 1) % len(lst))
    return lst[i], bl[i]


def load_w(g, pieces):
    kb = g.kb
    i = g.slot_i
    g.slot_i = (i + 1) % g.NSLOT
    t, b = g.wslot[i], g.wslot_b[i]
    views = []
    off = 0
    E = kb.E["pool"]
    kb._sync(E, [], [b])
    if b.dsem is None:
        b.dsem = kb.new_sem("d_" + b.name)
    if b.dcount:
        kb._wait(E, (b.dsem, b.dcount, None))
    for shape, src in pieces:
        n = int(np.prod(shape[1:]))
        v = t[:shape[0], off:off + n]
        if len(shape) == 3:
            v = v.rearrange("p (a b) -> p a b", a=shape[1])
        elif len(shape) == 4:
            v = v.rearrange("p (a b c) -> p a b c", a=shape[1], b=shape[2])
        ins = g.nc.gpsimd.dma_start(out=v, in_=src, max_dma_last_dim=4096)
        b.dcount += 16
        ins.then_inc(b.dsem, 16)
        kb.n_inst += 1
        views.append(v)
        off += n
    assert off <= g.SLOT
    kb._commit((b.dsem, b.dcount, None), [], [b])
    return views, b


def pcol(g, c, P=128):
    return g.par[:P, c:c + 1]


def rope_tables(g):
    kb, nc = g.kb, g.nc
    with contextlib.ExitStack() as st:
        old = kb.stack
        kb.stack = st
        posi = kb.sb("posi", [64, TOK], I32); pb = kb.buf("posi"); kb._dbufs.append(pb)
        ang = kb.sb("ang", [64, TOK], F32); ab = kb.buf("ang")
        t1 = kb.sb("rt1", [64, TOK], F32); t1b = kb.buf("rt1")
        ki = kb.sb("rki", [64, TOK], I32); kib = kb.buf("rki")
        kf = kb.sb("rkf", [64, TOK], F32); kfb = kb.buf("rkf")
        m = kb.sb("rm", [64, TOK], F32); mb = kb.buf("rm")
        kb.dma("sp", posi[:], g.pos.partition_broadcast(64).rearrange("p a n -> p (a n)"), pb, [], [pb])
        kb.op("dve", [pb], [ab], nc.vector.tensor_copy, ang[:], posi[:])
        kb.op("dve", [ab, g.par_b], [ab], nc.vector.tensor_scalar, out=ang[:], in0=ang[:],
              scalar1=pcol(g, PC_INVF, 64), scalar2=None, op0=ALU.mult)
        TWO_PI = 2.0 * np.pi
        for which, shift in ((1, 0.0), (0, np.pi / 2)):
            kb.op("dve", [ab], [t1b], nc.vector.tensor_scalar, out=t1[:], in0=ang[:],
                  scalar1=float(shift), scalar2=float(1.0 / TWO_PI), op0=ALU.add, op1=ALU.mult)
            kb.op("dve", [t1b], [kib], nc.vector.tensor_copy, ki[:], t1[:])
            kb.op("dve", [kib], [kfb], nc.vector.tensor_copy, kf[:], ki[:])
            kb.op("dve", [kfb, ab], [t1b], nc.vector.scalar_tensor_tensor, out=t1[:], in0=kf[:],
                  scalar=float(-TWO_PI), in1=ang[:], op0=ALU.mult, op1=ALU.add)
            if shift:
                kb.op("dve", [t1b], [t1b], nc.vector.tensor_scalar, out=t1[:], in0=t1[:],
                      scalar1=float(shift), scalar2=None, op0=ALU.add)
            kb.op("dve", [t1b], [mb], nc.vector.tensor_scalar, out=m[:], in0=t1[:],
                  scalar1=float(np.pi), scalar2=float(-TWO_PI), op0=ALU.is_gt, op1=ALU.mult)
            kb.op("dve", [mb, t1b], [t1b], nc.vector.tensor_tensor, out=t1[:], in0=t1[:], in1=m[:], op=ALU.add)
            kb.op("dve", [t1b], [mb], nc.vector.tensor_scalar, out=m[:], in0=t1[:],
                  scalar1=float(-np.pi), scalar2=float(TWO_PI), op0=ALU.is_lt, op1=ALU.mult)
            kb.op("dve", [mb, t1b], [t1b], nc.vector.tensor_tensor, out=t1[:], in0=t1[:], in1=m[:], op=ALU.add)
            kb.op("dve", [t1b], [t1b], nc.vector.tensor_scalar, out=t1[:], in0=t1[:],
                  scalar1=float(np.pi), scalar2=float(-np.pi), op0=ALU.min, op1=ALU.max)
            kb.op("act", [t1b], [g.cs_b], nc.scalar.activation, out=g.cs[:, which, :], in_=t1[:], func=AF.Sin)
        kb.op("dve", [g.cs_b, g.par_b], [g.cs_b], nc.vector.tensor_scalar, out=g.cs[:, 1, :], in0=g.cs[:, 1, :],
              scalar1=pcol(g, PC_SGN, 64), scalar2=None, op0=ALU.mult)
        kb.barrier()
        kb.stack = old


def sumsq_rstd(g, srcs, N, Dn, reads):
    kb, nc = g.kb, g.nc
    pt, pb = bank(g)
    n = len(srcs)
    for i, (ap, P) in enumerate(srcs):
        sq, sqb = scratch(g, "sq")
        kb.op("act", reads, [sqb], nc.scalar.activation, out=sq[:P, :N], in_=ap, func=AF.Square)
        kb.mm_open([sqb, g.cst_b], [pb] if i == 0 else [])
        ins = nc.tensor.matmul(pt[:, :N], g.ones[:P, :], sq[:P, :N], start=(i == 0), stop=(i == n - 1))
        kb.n_inst += 1
        kb.mm_close(ins, [sqb], [pb])
    rs, rsb = scratch(g, "rs")
    kb.op("act", [pb], [rsb], nc.scalar.activation, out=rs[:, :N], in_=pt[:, :N], func=AF.Sqrt,
          scale=float(1.0 / Dn), bias=float(EPS))
    kb.op("dve", [rsb], [rsb], nc.vector.reciprocal, rs[:, :N], rs[:, :N])
    return rs, rsb


def norm_x(g, gcol0, hT, hT_b, tiles=(0, 1), on_rstd=None):
    kb, nc = g.kb, g.nc
    for t in tiles:
        sl = slice(t * TL, (t + 1) * TL)
        rs, rsb = sumsq_rstd(g, [(g.x[:, kc, sl], 128) for kc in range(KC)], TL, D,
                             [g.xb[kc][t] for kc in range(KC)])
        if on_rstd is not None:
            on_rstd(t, rs, rsb)
        for kc in range(KC):
            kb.op("dve", [g.xb[kc][t], rsb, g.par_b], [hT_b[t]], nc.vector.scalar_tensor_tensor,
                  out=hT[:, kc, sl], in0=g.x[:, kc, sl], scalar=pcol(g, gcol0 + kc), in1=rs[:, :TL],
                  op0=ALU.mult, op1=ALU.mult)


def store_x(g, dst):
    kb = g.kb
    dv = dst.rearrange("(kc p) n -> p kc n", p=128)
    sb = kb.buf("xstore"); kb._dbufs.append(sb)
    for t in range(NTL):
        kb.dma("sp", dv[:, :, t * TL:(t + 1) * TL], g.x[:, :, t * TL:(t + 1) * TL], sb,
               [g.xb[kc][t] for kc in range(KC)], [])


def mem_kv(g, l, kmT, kmT_b, vm, vm_b):
    kb, nc = g.kb, g.nc
    with contextlib.ExitStack() as st:
        old = kb.stack
        kb.stack = st
        mt = kb.sb("mem_t", [128, 2, D], F32); mtb = kb.buf("mem_t"); kb._dbufs.append(mtb)
        gb = kb.sb("gmem_bc", [128, D], F32); gbb = kb.buf("gmem_bc"); kb._dbufs.append(gbb)
        mn = kb.sb("mem_n", [128, 2, D], BF16); mnb = kb.buf("mem_n")
        mnT = kb.sb("mem_nT", [128, KC, 256], BF16); mnTb = kb.buf("mem_nT")
        ssq = kb.sb("mem_ss", [128, 2], F32); ssb = kb.buf("mem_ss")
        kb.dma("sp", mt[:], g.mem.rearrange("(mb p) d -> p mb d", p=128), mtb, [], [mtb])
        kb.dma("sp", gb[:], g.g_mem[l:l + 1, :].partition_broadcast(128).rearrange("p a n -> p (a n)"),
               gbb, [], [gbb])
        for mb in range(2):
            kb.op("act", [mtb], [mnb, ssb], nc.scalar.activation, out=mn[:, mb, :], in_=mt[:, mb, :],
                  func=AF.Square, accum_out=ssq[:, mb:mb + 1])
        kb.op("act", [ssb], [ssb], nc.scalar.activation, out=ssq[:], in_=ssq[:], func=AF.Sqrt,
              scale=float(1.0 / D), bias=float(EPS))
        kb.op("dve", [ssb], [ssb], nc.vector.reciprocal, ssq[:], ssq[:])
        for mb in range(2):
            kb.op("dve", [mtb, ssb, gbb], [mnb], nc.vector.scalar_tensor_tensor, out=mn[:, mb, :],
                  in0=mt[:, mb, :], scalar=ssq[:, mb:mb + 1], in1=gb[:], op0=ALU.mult, op1=ALU.mult)
        for mb in range(2):
            for k4 in range(4):
                pt, pb = bank(g)
                ptb = pt[:].bitcast(BF16)
                kb.mm_open([mnb, g.cst_b], [pb])
                ins = None
                for q in range(4):
                    kc = k4 * 4 + q
                    ins = nc.tensor.transpose(ptb[:, q * 128:(q + 1) * 128], mn[:, mb, kc * 128:(kc + 1) * 128],
                                              g.idb[:])
                    kb.n_inst += 1
                kb.mm_close(ins, [mnb], [pb])
                kb.op("dve", [pb], [mnTb], nc.vector.tensor_copy,
                      mnT[:, k4 * 4:(k4 + 1) * 4, mb * 128:(mb + 1) * 128],
                      ptb[:, 0:512].rearrange("p (q n) -> p q n", q=4))
        wv = g.w_mem_kv[l].rearrange("(kc p) m -> p kc m", p=128)
        (wk,), wkb = load_w(g, [((128, KC, 512), wv[:, :, 0:512])])
        (wvv,), wvb = load_w(g, [((128, KC, 512), wv[:, :, 512:1024])])
        gk = PC_GMK0 if l == 0 else PC_GMK1
        for hh in range(4):
            pt, pb = bank(g)
            kb.mm(pb, pt[:, :256], [(wk[:, kc, hh * 128:(hh + 1) * 128], mnT[:, kc, :]) for kc in range(KC)],
                  [wkb, mnTb])
            rs, rsb = sumsq_rstd(g, [(pt[:, :256], 128)], 256, 128, [pb])
            kb.op("dve", [pb, rsb, g.par_b], [kmT_b], nc.vector.scalar_tensor_tensor, out=kmT[:, hh, :],
                  in0=pt[:, :256], scalar=pcol(g, gk), in1=rs[:, :256], op0=ALU.mult, op1=ALU.mult)
        for mb in range(2):
            pt, pb = bank(g)
            kb.mm(pb, pt[:, :], [(mnT[:, kc, mb * 128:(mb + 1) * 128], wvv[:, kc, :]) for kc in range(KC)],
                  [wvb, mnTb])
            kb.op("act", [pb], [vm_b], nc.scalar.copy, vm[:, mb, :], pt[:, :])
        kb.barrier()
        kb.stack = old


def mem_attention(g, wq, wqb, hT, hT_b, gq_col, kmT, kmT_b, vm, vm_b, mixT, mix_b):
    kb, nc = g.kb, g.nc
    sc = float(128 ** -0.5)
    qm = [kb.sb(f"qm{i}", [128, TL], BF16) for i in range(2)]
    qm_b = [kb.buf(f"qm{i}") for i in range(2)]
    qi = 0
    for t in range(NTL):
        sl = slice(t * TL, (t + 1) * TL)
        for hh in range(4):
            pt, pb = bank(g)
            kb.mm(pb, pt[:, :], [(wq[:, kc, hh * 128:(hh + 1) * 128], hT[:, kc, sl]) for kc in range(KC)],
                  [wqb, hT_b[t]])
            rs, rsb = sumsq_rstd(g, [(pt[:, :], 128)], TL, 128, [pb])
            q, qb = qm[qi % 2], qm_b[qi % 2]
            qi += 1
            kb.op("dve", [pb, rsb, g.par_b], [qb], nc.vector.scalar_tensor_tensor, out=q[:],
                  in0=pt[:, :], scalar=pcol(g, gq_col), in1=rs[:], op0=ALU.mult, op1=ALU.mult)
            po, pob = bank(g)
            pd, pdb = bank(g)
            for mb in range(2):
                ps_, psb_ = bank(g)
                kb.mm(psb_, ps_[:, :], [(kmT[:, hh, mb * 128:(mb + 1) * 128], q[:])], [kmT_b, qb])
                e, eb = scratch(g, "sq")
                kb.op("act", [psb_], [eb], nc.scalar.activation, out=e[:], in_=ps_[:, :], func=AF.Exp, scale=sc)
                kb.mm_open([eb, vm_b, g.cst_b], [pob, pdb] if mb == 0 else [])
                nc.tensor.matmul(po[:, :], vm[:, mb, hh * 128:(hh + 1) * 128], e[:], start=(mb == 0), stop=(mb == 1))
                ins = nc.tensor.matmul(pd[:, :], g.ones[:, :], e[:], start=(mb == 0), stop=(mb == 1))
                kb.n_inst += 2
                kb.mm_close(ins, [eb, vm_b], [pob, pdb])
            rd, rdb = scratch(g, "rs")
            kb.op("dve", [pdb], [rdb], nc.vector.reciprocal, rd[:], pd[:, :])
            kb.op("dve", [pob, rdb], [mix_b[t]], nc.vector.tensor_tensor, out=mixT[:, 12 + hh, sl], in0=po[:, :],
                  in1=rd[:], op=ALU.mult)


def out_proj(g, l, mixT, mix_b):
    kb, nc = g.kb, g.nc
    wv = g.w_out[l].rearrange("(kc p) m -> p kc m", p=128)
    for pc in range(4):
        (w,), wb = load_w(g, [((128, KC, 512), wv[:, :, pc * 512:(pc + 1) * 512])])
        for t in range(NTL):
            sl = slice(t * TL, (t + 1) * TL)
            for dq in range(4):
                dc = pc * 4 + dq
                pt, pb = bank(g)
                kb.mm(pb, pt[:, :], [(w[:, kc, dq * 128:(dq + 1) * 128], mixT[:, kc, sl]) for kc in range(KC)],
                      [wb, mix_b[t]])
                kb.op("dve", [pb, g.xb[dc][t]], [g.xb[dc][t]], nc.vector.tensor_tensor, out=g.x[:, dc, sl],
                      in0=g.x[:, dc, sl], in1=pt[:, :], op=ALU.add)


def ffn_drain(g, n=None):
    pend = g.ffn_pending
    k = len(pend) if n is None else min(n, len(pend))
    for _ in range(k):
        pend.pop(0)()


def ffn_group(g, wg, wu, wd, wb, hT, hT_b, gate_bc=None, gate_bcb=None, act=None, act_b=None):
    kb, nc = g.kb, g.nc
    for t in range(NTL):
        sl = slice(t * TL, (t + 1) * TL)
        for m in range(2):
            pg, pgb = bank(g)
            kb.mm(pgb, pg[:, :], [(wg[:, kc, m * 128:(m + 1) * 128], hT[:, kc, sl]) for kc in range(KC)],
                  [wb, hT_b[t]])
            sg, sgb = scratch(g, "sq")
            kb.op("act", [pgb], [sgb], nc.scalar.activation, out=sg[:], in_=pg[:, :], func=AF.Silu)
            ffn_drain(g, 4)
            pu, pub = bank(g)
            kb.mm(pub, pu[:, :], [(wu[:, kc, m * 128:(m + 1) * 128], hT[:, kc, sl]) for kc in range(KC)],
                  [wb, hT_b[t]])
            if gate_bc is None:
                kb.op("dve", [pub, sgb], [act_b[t][m]], nc.vector.tensor_tensor, out=act[t][:, m, :], in0=pu[:, :],
                      in1=sg[:], op=ALU.mult)
            else:
                ug, ugb = scratch(g, "fs")
                kb.op("dve", [pub, gate_bcb[t]], [ugb], nc.vector.tensor_tensor, out=ug[:, :TL], in0=pu[:, :],
                      in1=gate_bc[:, sl], op=ALU.mult)
                kb.op("dve", [ugb, sgb], [act_b[t][m]], nc.vector.tensor_tensor, out=act[t][:, m, :],
                      in0=ug[:, :TL], in1=sg[:], op=ALU.mult)
            ffn_drain(g, 4)
        ffn_drain(g)

        def down(dc, t=t, sl=sl, wd=wd, wb=wb):
            pt, pb = bank(g)
            kb.mm(pb, pt[:, :], [(wd[m][:, dc * 128:(dc + 1) * 128], act[t][:, m, :]) for m in range(2)],
                  [wb, act_b[t][0], act_b[t][1]])
            kb.op("dve", [pb, g.xb[dc][t]], [g.xb[dc][t]], nc.vector.tensor_tensor, out=g.x[:, dc, sl],
                  in0=g.x[:, dc, sl], in1=pt[:, :], op=ALU.add)

        for dc in range(KC):
            g.ffn_pending.append(lambda dc=dc, f=down: f(dc))


def ffn_weights(g, wgate, wup, wdown, gi):
    c0 = gi * 256
    gv = wgate.rearrange("(kc p) m -> p kc m", p=128)[:, :, c0:c0 + 256]
    uv = wup.rearrange("(kc p) m -> p kc m", p=128)[:, :, c0:c0 + 256]
    dv = wdown[c0:c0 + 256, :].rearrange("(m p) d -> p m d", p=128)
    (wg, wu, wd0, wd1), wb = load_w(g, [((128, KC, 256), gv), ((128, KC, 256), uv), ((128, D), dv[:, 0, :]),
                                          ((128, D), dv[:, 1, :])])
    return wg, wu, (wd0, wd1), wb


def layer0(g):
    kb, nc = g.kb, g.nc
    with contextlib.ExitStack() as st:
        old = kb.stack
        kb.stack = st
        alloc_ring(g, 8192)
        kmT = kb.sb("kmT", [128, 4, 256], BF16); kmT_b = kb.buf("kmT")
        vm = kb.sb("vm", [128, 2, 512], BF16); vm_b = kb.buf("vm")
        mem_kv(g, 0, kmT, kmT_b, vm, vm_b)
        hT = kb.sb("hT", [128, KC, TOK], BF16); hT_b = [kb.buf("hT0"), kb.buf("hT1")]
        mixT = kb.sb("mixT", [128, KC, TOK], BF16); mix_b = [kb.buf("mix0"), kb.buf("mix1")]
        xh = kb.sb("xh", [128, KC, 4], F32); xhb = kb.buf("xh"); kb._dbufs.append(xhb)
        hh_ = kb.sb("hh", [128, KC, 4], BF16); hhb = kb.buf("hh")
        zh = kb.sb("zh", [128, 8], F32); zhb = kb.buf("zh")
        kb.dma("sp", xh[:], g.xhT.rearrange("(kc p) n -> p kc n", p=128), xhb, [], [xhb])
        norm_x(g, PC_GMIX0, hT, hT_b)
        rs, rsb = sumsq_rstd(g, [(xh[:, kc, :], 128) for kc in range(KC)], 4, D, [xhb])
        for kc in range(KC):
            kb.op("dve", [xhb, rsb, g.par_b], [hhb], nc.vector.scalar_tensor_tensor, out=hh_[:, kc, :],
                  in0=xh[:, kc, :], scalar=pcol(g, PC_GMIX0 + kc), in1=rs[:, :4], op0=ALU.mult, op1=ALU.mult)
        wa = g.a_w_in.rearrange("(kc p) m -> p kc m", p=128)
        for j in range(12):
            w, wb = load_w(g, [((128, KC, 128), wa[:, :, s_ * 1536 + j * 128:s_ * 1536 + (j + 1) * 128])
                               for s_ in range(3)])
            ph, phb = bank(g)
            kb.mm(phb, ph[:, 0:4], [(w[0][:, kc, :], hh_[:, kc, :]) for kc in range(KC)], [wb, hhb])
            kb.mm(phb, ph[:, 4:8], [(w[2][:, kc, :], hh_[:, kc, :]) for kc in range(KC)], [wb, hhb])
            kb.op("act", [phb], [zhb], nc.scalar.copy, zh[:, 0:8], ph[:, 0:8])
            for t in range(NTL):
                sl = slice(t * TL, (t + 1) * TL)
                px, pxb = bank(g)
                kb.mm(pxb, px[:, :], [(w[0][:, kc, :], hT[:, kc, sl]) for kc in range(KC)], [wb, hT_b[t]])
                pc_, pcb = bank(g)
                kb.mm(pcb, pc_[:, :], [(w[2][:, kc, :], hT[:, kc, sl]) for kc in range(KC)], [wb, hT_b[t]])
                pg, pgb = bank(g)
                kb.mm(pgb, pg[:, :], [(w[1][:, kc, :], hT[:, kc, sl]) for kc in range(KC)], [wb, hT_b[t]])
                gc, gcb = scratch(g, "rs")
                kb.op("act", [pcb], [gcb], nc.scalar.copy, gc[:, :TL], pc_[:, :])
                z, zb = scratch(g, "fs")
                kb.op("dve", [pxb, gcb], [zb], nc.vector.tensor_tensor, out=z[:, 2:2 + TL], in0=px[:, :],
                      in1=gc[:, :TL], op=ALU.mult)
                kb.op("dve", [zhb, g.par_b, zb], [zb], nc.vector.scalar_tensor_tensor, out=z[:, 0:2],
                      in0=zh[:, 2 * t:2 * t + 2], scalar=pcol(g, PC_HALO + t), in1=zh[:, 4 + 2 * t:6 + 2 * t],
                      op0=ALU.mult, op1=ALU.mult)
                y, yb = scratch(g, "rs")
                cw = PC_CONV + j * 3
                kb.op("dve", [zb, g.par_b], [yb], nc.vector.tensor_scalar, out=y[:, :TL], in0=z[:, 0:TL],
                      scalar1=pcol(g, cw), scalar2=None, op0=ALU.mult)
                kb.op("dve", [zb, yb, g.par_b], [yb], nc.vector.scalar_tensor_tensor, out=y[:, :TL],
                      in0=z[:, 1:1 + TL], scalar=pcol(g, cw + 1), in1=y[:, :TL], op0=ALU.mult, op1=ALU.add)
                kb.op("dve", [zb, yb, g.par_b], [yb], nc.vector.scalar_tensor_tensor, out=y[:, :TL],
                      in0=z[:, 2:2 + TL], scalar=pcol(g, cw + 2), in1=y[:, :TL], op0=ALU.mult, op1=ALU.add)
                kb.op("dve", [pgb, yb], [mix_b[t]], nc.vector.tensor_tensor, out=mixT[:, j, sl], in0=pg[:, :],
                      in1=y[:, :TL], op=ALU.mult)
        (wq,), wqb = load_w(g, [((128, KC, 512),
                                  g.a_w_in.rearrange("(kc p) m -> p kc m", p=128)[:, :, 4608:5120])])
        mem_attention(g, wq, wqb, hT, hT_b, PC_GMQ0, kmT, kmT_b, vm, vm_b, mixT, mix_b)
        out_proj(g, 0, mixT, mix_b)
        kb.barrier()
        kb.stack = old
    with contextlib.ExitStack() as st:
        old = kb.stack
        kb.stack = st
        alloc_ring(g, 12288)
        hT = kb.sb("hT", [128, KC, TOK], BF16); hT_b = [kb.buf("hT0"), kb.buf("hT1")]
        norm_x(g, PC_GFFN0, hT, hT_b)
        act = [kb.sb(f"act{t}", [128, 2, TL], BF16) for t in range(NTL)]
        act_b = [[kb.buf(f"act{t}_{m}") for m in range(2)] for t in range(NTL)]
        for gi in range(DFF // 256):
            wg, wu, wd, wb = ffn_weights(g, g.ffn_gate, g.ffn_up, g.ffn_down, gi)
            ffn_group(g, wg, wu, wd, wb, hT, hT_b, act=act, act_b=act_b)
        ffn_drain(g)
        kb.barrier()
        kb.stack = old


def rope_AB(g, gcol, gswcol, AB, AB_b):
    kb, nc = g.kb, g.nc
    kb.op("dve", [g.cs_b, g.par_b], [AB_b], nc.vector.tensor_scalar, out=AB[:, 0, :], in0=g.cs[:, 0, :],
          scalar1=pcol(g, gcol, 64), scalar2=None, op0=ALU.mult)
    kb.op("dve", [g.cs_b, g.par_b], [AB_b], nc.vector.tensor_scalar, out=AB[:, 1, :], in0=g.cs[:, 1, :],
          scalar1=pcol(g, gswcol, 64), scalar2=None, op0=ALU.mult)


def shared_kv(g):
    kb, nc = g.kb, g.nc
    with contextlib.ExitStack() as st:
        old = kb.stack
        kb.stack = st
        hT = kb.sb("hT", [128, KC, TOK], BF16); hT_b = [kb.buf("hT0"), kb.buf("hT1")]
        ckv = kb.sb("ckv", [128, 2, TOK], BF16); ckv_b = [kb.buf("ckv0"), kb.buf("ckv1")]
        Rk = kb.sb("Rk", [64, TOK], F32); Rk_b = [kb.buf("Rk0"), kb.buf("Rk1")]
        sqpe = kb.sb("sqpe", [64, TOK], BF16); sqpe_b = [kb.buf("sqpe0"), kb.buf("sqpe1")]
        AB = kb.sb("ABk", [64, 2, TOK], F32); AB_b = kb.buf("ABk")
        kst = [kb.sb(f"kst{i}", [128, TOK], BF16) for i in range(2)]
        kst_b = [kb.buf(f"kst{i}") for i in range(2)]
        krst = [kb.sb(f"krst{i}", [64, TOK], BF16) for i in range(2)]
        krst_b = [kb.buf(f"krst{i}") for i in range(2)]
        vst = [kb.sb(f"vst{i}", [128, 512], BF16) for i in range(2)]
        vst_b = [kb.buf(f"vst{i}") for i in range(2)]
        for b in kst_b + krst_b + vst_b:
            kb._dbufs.append(b)
        alloc_ring(g, 8192)
        norm_x(g, PC_GKV, hT, hT_b)
        rope_AB(g, PC_GKN_R, PC_GKN_RS, AB, AB_b)
        wav = g.w_kv_a.rearrange("(kc p) m -> p kc m", p=128)
        (wa,), wab = load_w(g, [((128, KC, 320), wav)])
        wkbv = g.w_kv_b.rearrange("(kc p) m -> p kc m", p=128)
        (wb0, wb1), wbb = load_w(g, [((128, 3072), wkbv[:, 0, :]), ((128, 3072), wkbv[:, 1, :])])
        wbv = [wb0.rearrange("p (h c) -> p h c", h=12), wb1.rearrange("p (h c) -> p h c", h=12)]
        for t in range(NTL):
            sl = slice(t * TL, (t + 1) * TL)
            pl = []
            for c in range(2):
                pt, pb = bank(g)
                kb.mm(pb, pt[:, :], [(wa[:, kc, c * 128:(c + 1) * 128], hT[:, kc, sl]) for kc in range(KC)],
                      [wab, hT_b[t]])
                pl.append((pt, pb))
            rs, rsb = sumsq_rstd(g, [(pl[0][0][:, :], 128), (pl[1][0][:, :], 128)], TL, 256,
                                 [pl[0][1], pl[1][1]])
            for c in range(2):
                kb.op("dve", [pl[c][1], rsb, g.par_b], [ckv_b[t]], nc.vector.scalar_tensor_tensor,
                      out=ckv[:, c, sl], in0=pl[c][0][:, :], scalar=pcol(g, PC_GKVA + c), in1=rs[:],
                      op0=ALU.mult, op1=ALU.mult)
            pp, ppb = bank(g)
            kb.mm(ppb, pp[:64, :], [(wa[:, kc, 256:320], hT[:, kc, sl]) for kc in range(KC)], [wab, hT_b[t]])
            pq, pqb = bank(g)
            kb.mm_open([wab, hT_b[t]], [pqb])
            ins = None
            for kc in range(KC):
                nc.tensor.matmul(pq[0:32, :], wa[:, kc, 288:320], hT[:, kc, sl], start=(kc == 0), stop=(kc == KC - 1))
                ins = nc.tensor.matmul(pq[32:64, :], wa[:, kc, 256:288], hT[:, kc, sl], start=(kc == 0),
                                       stop=(kc == KC - 1))
                kb.n_inst += 2
            kb.mm_close(ins, [wab, hT_b[t]], [pqb])
            kb.op("act", [ppb], [sqpe_b[t]], nc.scalar.activation, out=sqpe[:, sl], in_=pp[:64, :], func=AF.Square)
            r1, r1b = scratch(g, "fs")
            kb.op("dve", [ppb, AB_b], [r1b], nc.vector.tensor_tensor, out=r1[:64, :TL], in0=pp[:64, :],
                  in1=AB[:, 0, sl], op=ALU.mult)
            kb.op("dve", [pqb, AB_b], [Rk_b[t]], nc.vector.tensor_tensor, out=Rk[:, sl], in0=pq[:64, :],
                  in1=AB[:, 1, sl], op=ALU.mult)
            kb.op("dve", [r1b, Rk_b[t]], [Rk_b[t]], nc.vector.tensor_tensor, out=Rk[:, sl], in0=Rk[:, sl],
                  in1=r1[:64, :TL], op=ALU.add)
        for h in range(12):
            si = h % 2
            for t in range(NTL):
                sl = slice(t * TL, (t + 1) * TL)
                pt, pb = bank(g)
                kb.mm(pb, pt[:, :], [(wbv[c][:, h, 0:128], ckv[:, c, sl]) for c in range(2)], [wbb, ckv_b[t]])
                ps_, psb_ = bank(g)
                sq, sqb = scratch(g, "sq")
                kb.op("act", [pb], [sqb], nc.scalar.activation, out=sq[:], in_=pt[:, :], func=AF.Square)
                kb.mm_open([sqb, sqpe_b[t], g.cst_b], [psb_])
                nc.tensor.matmul(ps_[:, :], g.ones[:, :], sq[:], start=True, stop=False)
                ins = nc.tensor.matmul(ps_[:, :], g.ones[:64, :], sqpe[:, sl], start=False, stop=True)
                kb.n_inst += 2
                kb.mm_close(ins, [sqb, sqpe_b[t]], [psb_])
                rs, rsb = scratch(g, "rs")
                kb.op("act", [psb_], [rsb], nc.scalar.activation, out=rs[:], in_=ps_[:, :], func=AF.Sqrt,
                      scale=float(1.0 / 192), bias=float(EPS))
                kb.op("dve", [rsb], [rsb], nc.vector.reciprocal, rs[:], rs[:])
                kb.op("dve", [pb, rsb, g.par_b], [kst_b[si]], nc.vector.scalar_tensor_tensor, out=kst[si][:, sl],
                      in0=pt[:, :], scalar=pcol(g, PC_GKN_N), in1=rs[:], op0=ALU.mult, op1=ALU.mult)
                kb.op("dve", [Rk_b[t], rsb], [krst_b[si]], nc.vector.tensor_tensor, out=krst[si][:, sl],
                      in0=Rk[:, sl], in1=rs[:64, :], op=ALU.mult)
            kb.dma("sp", g.kt_own(h)[0:128, :], kst[si][:], kst_b[si], [kst_b[si]], [])
            kb.dma("sp", g.kt_own(h)[128:192, :], krst[si][:], krst_b[si], [krst_b[si]], [])
        vi = 0
        for tb in range(TOK // 128):
            t = tb // 4
            for hg in range(3):
                pt, pb = bank(g)
                kb.mm(pb, pt[:, :], [(ckv[:, c, tb * 128:(tb + 1) * 128], wbv[c][:, hg * 4:(hg + 1) * 4, 128:256])
                                      for c in range(2)], [wbb, ckv_b[t]])
                s = vi % 2
                vi += 1
                kb.op("act", [pb], [vst_b[s]], nc.scalar.copy, vst[s][:], pt[:, :])
                for hq in range(4):
                    kb.dma("sp", g.v_own(hg * 4 + hq)[tb * 128:(tb + 1) * 128, :], vst[s][:, hq * 128:(hq + 1) * 128],
                           vst_b[s], [vst_b[s]], [])
        kb.barrier()
        kb.stack = old


def exchange_kv(g):
    kb, nc = g.kb, g.nc
    E = kb.E["pool"]
    kb.barrier()
    cs = kb.new_sem("cc")
    groups = [[0, 1, 2, 3], [4, 5, 6, 7]]
    n = 0
    for h in range(12):
        for src, dst in ((g.KT_own_l[h], g.KTg_l[h]), (g.V_own_l[h], g.Vg_l[h])):
            ins = nc.gpsimd.collective_compute("AllGather", ALU.bypass, replica_groups=groups,
                                               ins=[src.opt()], outs=[dst.opt()])
            ins.then_inc(cs)
            n += 1
    for En in kb.E.values():
        En.h.wait_ge(cs, n)


def causal_attention(g, cqn, cqn_b, mixT, mix_b):
    kb, nc = g.kb, g.nc
    sc = float(192 ** -0.5)
    with contextlib.ExitStack() as st:
        old = kb.stack
        kb.stack = st
        NKV = 2
        kn = [kb.sb(f"kn{i}", [128, SEQ], BF16) for i in range(NKV)]
        kr = [kb.sb(f"kr{i}", [64, SEQ], BF16) for i in range(NKV)]
        vv = [kb.sb(f"vv{i}", [128, 32, 128], BF16) for i in range(NKV)]
        kv_b = [kb.buf(f"kv{i}") for i in range(NKV)]
        wqh = [kb.sb(f"wqh{i}", [128, 4, 192], BF16) for i in range(2)]
        wqh_b = [kb.buf(f"wqh{i}") for i in range(2)]
        qn = [kb.sb(f"qn{i}", [128, TOK], BF16) for i in range(2)]
        qr = [kb.sb(f"qr{i}", [64, TOK], BF16) for i in range(2)]
        q_b = [[kb.buf(f"q{i}_{t}") for t in range(NTL)] for i in range(2)]
        AB = kb.sb("ABq", [64, 2, TOK], F32); AB_b = kb.buf("ABq")
        qidx = kb.sb("qidx", [128, TOK], F32); qidx_b = kb.buf("qidx")
        for b in kv_b + wqh_b + [qidx_b]:
            kb._dbufs.append(b)
        kb.dma("sp", qidx[:], g.qidx.partition_broadcast(128).rearrange("p a n -> p (a n)"), qidx_b, [], [qidx_b])
        rope_AB(g, PC_GQN_R, PC_GQN_RS, AB, AB_b)
        wqv = g.w_q_b.rearrange("(kc p) m -> p kc m", p=128)
        g.bank_set = [4, 5, 6, 7]
        si = 0
        for h in range(12):
            par = h % 2
            b = kv_b[par]
            E = kb.E["sp"]
            if getattr(g, "kv_tok", None) is not None:
                kb._wait(E, g.kv_tok)
            kb._sync(E, [], [b])
            if b.dsem is None:
                b.dsem = kb.new_sem("d_" + b.name)
            if b.dcount:
                kb._wait(E, (b.dsem, b.dcount, None))
            for i in range(8):
                r, off = min(i, 7 - i), (512 if i >= 4 else 0)
                srcs = [(kn[par][:, i * 512:(i + 1) * 512], g.kt_g(h, r)[0:128, off:off + 512]),
                        (kr[par][:, i * 512:(i + 1) * 512], g.kt_g(h, r)[128:192, off:off + 512]),
                        (vv[par][:, i * 4:(i + 1) * 4, :],
                         g.v_g(h, r)[off:off + 512, :].rearrange("(kb p) d -> p kb d", p=128))]
                for o_, i_ in srcs:
                    ins = nc.sync.dma_start(out=o_, in_=i_)
                    b.dcount += 16
                    ins.then_inc(b.dsem, 16)
                    kb.n_inst += 1
            kb._commit((b.dsem, b.dcount, None), [], [b])
            kb.dma("pool", wqh[par][:], wqv[:, :, h * 192:(h + 1) * 192], wqh_b[par], [], [wqh_b[par]])
            w = wqh[par]
            for t in range(NTL):
                sl = slice(t * TL, (t + 1) * TL)
                pn, pnb = bank(g)
                kb.mm(pnb, pn[:, :], [(w[:, kc, 0:128], cqn[:, kc, sl]) for kc in range(4)], [wqh_b[par], cqn_b[t]])
                pr, prb = bank(g)
                kb.mm(prb, pr[:64, :], [(w[:, kc, 128:192], cqn[:, kc, sl]) for kc in range(4)], [wqh_b[par], cqn_b[t]])
                pq, pqb = bank(g)
                kb.mm_open([wqh_b[par], cqn_b[t]], [pqb])
                ins = None
                for kc in range(4):
                    nc.tensor.matmul(pq[0:32, :], w[:, kc, 160:192], cqn[:, kc, sl], start=(kc == 0), stop=(kc == 3))
                    ins = nc.tensor.matmul(pq[32:64, :], w[:, kc, 128:160], cqn[:, kc, sl], start=(kc == 0),
                                           stop=(kc == 3))
                    kb.n_inst += 2
                kb.mm_close(ins, [wqh_b[par], cqn_b[t]], [pqb])
                rs, rsb = sumsq_rstd(g, [(pn[:, :], 128), (pr[:64, :], 64)], TL, 192, [pnb, prb])
                kb.op("dve", [pnb, rsb, g.par_b], [q_b[par][t]], nc.vector.scalar_tensor_tensor, out=qn[par][:, sl],
                      in0=pn[:, :], scalar=pcol(g, PC_GQN_N), in1=rs[:], op0=ALU.mult, op1=ALU.mult)
                r1, r1b = scratch(g, "fs")
                kb.op("dve", [prb, AB_b], [r1b], nc.vector.tensor_tensor, out=r1[:64, :TL], in0=pr[:64, :],
                      in1=AB[:, 0, sl], op=ALU.mult)
                r2, r2b = scratch(g, "fs")
                kb.op("dve", [pqb, AB_b], [r2b], nc.vector.tensor_tensor, out=r2[:64, :TL], in0=pq[:64, :],
                      in1=AB[:, 1, sl], op=ALU.mult)
                kb.op("dve", [r1b, r2b], [r1b], nc.vector.tensor_tensor, out=r1[:64, :TL], in0=r1[:64, :TL],
                      in1=r2[:64, :TL], op=ALU.add)
                kb.op("dve", [r1b, rsb], [q_b[par][t]], nc.vector.tensor_tensor, out=qr[par][:, sl],
                      in0=r1[:64, :TL], in1=rs[:64, :], op=ALU.mult)
            for t in range(NTL):
                sl = slice(t * TL, (t + 1) * TL)
                nkb = 16 if t == 0 else 32
                po, pob = g.pst[2], g.psb[2]
                pd, pdb = g.pst[3], g.psb[3]
                def scores(kbi):
                    nonlocal si
                    S, Sb = g.pst[si % 2], g.psb[si % 2]
                    si += 1
                    ks = slice(kbi * 128, (kbi + 1) * 128)
                    kb.mm(Sb, S[:, :], [(kn[par][:, ks], qn[par][:, sl]), (kr[par][:, ks], qr[par][:, sl])],
                          [kv_b[par], q_b[par][t]])
                    return S, Sb

                nxt = scores(0)
                for kbi in range(nkb):
                    S, Sb = nxt
                    if kbi + 1 < nkb:
                        nxt = scores(kbi + 1)
                    e, eb = scratch(g, "sq")
                    kb.op("act", [Sb], [eb], nc.scalar.activation, out=e[:], in_=S[:, :], func=AF.Exp, scale=sc)
                    if t == 0 or kbi >= 16:
                        em, emb = scratch(g, "sq")
                        kb.op("dve", [qidx_b, g.par_b, eb], [emb], nc.vector.scalar_tensor_tensor, out=em[:],
                              in0=qidx[:, sl], scalar=pcol(g, PC_KIDX + kbi), in1=e[:], op0=ALU.is_ge, op1=ALU.mult)
                        e, eb = em, emb
                    kb.mm_open([eb, kv_b[par], g.cst_b], [pob, pdb] if kbi == 0 else [])
                    nc.tensor.matmul(po[:, :], vv[par][:, kbi, :], e[:], start=(kbi == 0), stop=(kbi == nkb - 1))
                    ins = nc.tensor.matmul(pd[:, :], g.ones[:, :], e[:], start=(kbi == 0), stop=(kbi == nkb - 1))
                    kb.n_inst += 2
                    kb.mm_close(ins, [eb, kv_b[par]], [pob, pdb])
                rd, rdb = scratch(g, "rs")
                kb.op("dve", [pdb], [rdb], nc.vector.reciprocal, rd[:], pd[:, :])
                kb.op("dve", [pob, rdb], [mix_b[t]], nc.vector.tensor_tensor, out=mixT[:, h, sl], in0=po[:, :],
                      in1=rd[:], op=ALU.mult)
        g.bank_set = list(range(8))
        kb.barrier()
        kb.stack = old


def moe(g):
    kb, nc = g.kb, g.nc
    with contextlib.ExitStack() as st:
        old = kb.stack
        kb.stack = st
        alloc_ring(g, 12288)
        hT = kb.sb("hT", [128, KC, TOK], BF16); hT_b = [kb.buf("hT0"), kb.buf("hT1")]
        selsb = kb.sb("selsb", [8, NEXP * 128], F32); sel_b = kb.buf("selsb"); kb._dbufs.append(sel_b)
        wr = kb.sb("wr", [128, KC, NEXP], F32); wr_b = kb.buf("wr"); kb._dbufs.append(wr_b)
        rtok = kb.sb("rtok", [128, 8], F32); rtok_b = kb.buf("rtok")
        lg = kb.sb("lg", [128, 8, NEXP], F32); lg_b = kb.buf("lg")
        t8 = kb.sb("t8", [128, 8, 8], F32); t8_b = kb.buf("t8")
        wts = kb.sb("wts", [128, 4, 8], F32); wts_b = kb.buf("wts")
        m1 = kb.sb("m1", [128, NEXP], F32); m1_b = kb.buf("m1")
        gates = kb.sb("gates", [128, 8, NEXP], F32); gates_b = kb.buf("gates")
        gT = kb.sb("gT", [8, TOK], F32); gT_b = kb.buf("gT")
        G = [kb.sb(f"G{i}", [128, TOK], F32) for i in range(2)]
        G_b = [[kb.buf(f"G{i}_{t}") for t in range(NTL)] for i in range(2)]
        act = [kb.sb(f"act{t}", [128, 2, TL], BF16) for t in range(NTL)]
        act_b = [[kb.buf(f"act{t}_{m}") for m in range(2)] for t in range(NTL)]
        kb.dma("sp", selsb[:], g.sel, sel_b, [], [sel_b])
        kb.dma("sp", wr[:], g.w_router.rearrange("(kc p) e -> p kc e", p=128), wr_b, [], [wr_b])
        for kc in range(KC):
            kb.op("dve", [wr_b, g.par_b], [wr_b], nc.vector.tensor_scalar, out=wr[:, kc, :], in0=wr[:, kc, :],
                  scalar1=pcol(g, PC_GFFN1 + kc), scalar2=None, op0=ALU.mult)

        def on_rstd(t, rs, rsb):
            for q in range(4):
                pt, pb = bank(g)
                kb.mm_open([rsb, g.cst_b], [pb])
                ins = nc.tensor.transpose(pt[:, 0:128], rs[:, q * 128:(q + 1) * 128], g.idf[:])
                kb.n_inst += 1
                kb.mm_close(ins, [rsb], [pb])
                kb.op("dve", [pb], [rtok_b], nc.vector.tensor_copy, rtok[:, t * 4 + q:t * 4 + q + 1], pt[:, 0:1])

        norm_x(g, PC_GFFN1, hT, hT_b, on_rstd=on_rstd)
        for tb in range(8):
            t = tb // 4
            pt, pb = bank(g)
            kb.mm(pb, pt[:, 0:NEXP], [(g.x[:, kc, tb * 128:(tb + 1) * 128], wr[:, kc, :]) for kc in range(KC)],
                  [wr_b] + [g.xb[kc][t] for kc in range(KC)])
            kb.op("dve", [pb, rtok_b], [lg_b], nc.vector.tensor_scalar, out=lg[:, tb, :], in0=pt[:, 0:NEXP],
                  scalar1=rtok[:, tb:tb + 1], scalar2=None, op0=ALU.mult)
            kb.op("dve", [lg_b], [t8_b], nc.vector.max, out=t8[:, tb, :], in_=lg[:, tb, :])
        kb.op("dve", [t8_b], [wts_b], nc.vector.tensor_tensor, out=wts[:, 0, :], in0=t8[:, :, 1], in1=t8[:, :, 0],
              op=ALU.subtract)
        kb.op("act", [wts_b], [wts_b], nc.scalar.activation, out=wts[:, 1, :], in_=wts[:, 0, :], func=AF.Exp)
        kb.op("dve", [wts_b], [wts_b], nc.vector.tensor_scalar, out=wts[:, 2, :], in0=wts[:, 1, :], scalar1=1.0,
              scalar2=None, op0=ALU.add)
        kb.op("dve", [wts_b], [wts_b], nc.vector.reciprocal, wts[:, 2, :], wts[:, 2, :])
        kb.op("dve", [wts_b], [wts_b], nc.vector.tensor_tensor, out=wts[:, 3, :], in0=wts[:, 1, :], in1=wts[:, 2, :],
              op=ALU.mult)
        for tb in range(8):
            kb.op("dve", [lg_b, t8_b, wts_b], [m1_b], nc.vector.tensor_scalar, out=m1[:], in0=lg[:, tb, :],
                  scalar1=t8[:, tb, 0:1], scalar2=wts[:, 2, tb:tb + 1], op0=ALU.is_equal, op1=ALU.mult)
            kb.op("dve", [lg_b, t8_b, wts_b], [gates_b], nc.vector.tensor_scalar, out=gates[:, tb, :], in0=lg[:, tb, :],
                  scalar1=t8[:, tb, 1:2], scalar2=wts[:, 3, tb:tb + 1], op0=ALU.is_equal, op1=ALU.mult)
            kb.op("dve", [m1_b, gates_b], [gates_b], nc.vector.tensor_tensor, out=gates[:, tb, :], in0=gates[:, tb, :],
                  in1=m1[:], op=ALU.add)
        for t in range(NTL):
            pt, pb = bank(g)
            kb.mm_open([gates_b, g.cst_b], [pb])
            ins = None
            for q in range(4):
                ins = nc.tensor.transpose(pt[0:NEXP, q * 128:(q + 1) * 128], gates[:, t * 4 + q, :], g.idf[:])
                kb.n_inst += 1
            kb.mm_close(ins, [gates_b], [pb])
            kb.op("act", [pb], [gT_b], nc.scalar.copy, gT[:, t * TL:(t + 1) * TL], pt[0:NEXP, :])
        for e in range(NEXP):
            gi_ = e % 2
            for t in range(NTL):
                sl = slice(t * TL, (t + 1) * TL)
                pt, pb = bank(g)
                kb.mm(pb, pt[:, :], [(selsb[:, e * 128:(e + 1) * 128], gT[:, sl])], [sel_b, gT_b])
                kb.op("act", [pb], [G_b[gi_][t]], nc.scalar.copy, G[gi_][:, sl], pt[:, :])
            for gi in range(DFF // 256):
                wg, wu, wd, wb = ffn_weights(g, g.moe_gate[e], g.moe_up[e], g.moe_down[e], gi)
                ffn_group(g, wg, wu, wd, wb, hT, hT_b, gate_bc=G[gi_], gate_bcb=G_b[gi_], act=act, act_b=act_b)
        ffn_drain(g)
        kb.barrier()
        kb.stack = old


def layer1(g):
    kb, nc = g.kb, g.nc
    with contextlib.ExitStack() as st0:
        old0 = kb.stack
        kb.stack = st0
        kmT = kb.sb("kmT", [128, 4, 256], BF16); kmT_b = kb.buf("kmT")
        vm = kb.sb("vm", [128, 2, 512], BF16); vm_b = kb.buf("vm")
        with contextlib.ExitStack() as st:
            kb.stack = st
            alloc_ring(g, 8192)
            mem_kv(g, 1, kmT, kmT_b, vm, vm_b)
            kb.stack = st0
        mixT = kb.sb("mixT", [128, KC, TOK], BF16); mix_b = [kb.buf("mix0"), kb.buf("mix1")]
        cqn = kb.sb("cqn", [128, 4, TOK], BF16); cqn_b = [kb.buf("cqn0"), kb.buf("cqn1")]
        with contextlib.ExitStack() as st:
            kb.stack = st
            alloc_ring(g, 8192)
            hT = kb.sb("hT", [128, KC, TOK], BF16); hT_b = [kb.buf("hT0"), kb.buf("hT1")]
            norm_x(g, PC_GMIX1, hT, hT_b)
            bw = g.b_w_in.rearrange("(kc p) m -> p kc m", p=128)
            (wqa,), wqab = load_w(g, [((128, KC, 512), bw[:, :, 0:512])])
            for t in range(NTL):
                sl = slice(t * TL, (t + 1) * TL)
                pl = []
                for c in range(4):
                    pt, pb = bank(g)
                    kb.mm(pb, pt[:, :], [(wqa[:, kc, c * 128:(c + 1) * 128], hT[:, kc, sl]) for kc in range(KC)],
                          [wqab, hT_b[t]])
                    pl.append((pt, pb))
                rs, rsb = sumsq_rstd(g, [(p[0][:, :], 128) for p in pl], TL, 512, [p[1] for p in pl])
                for c in range(4):
                    kb.op("dve", [pl[c][1], rsb, g.par_b], [cqn_b[t]], nc.vector.scalar_tensor_tensor,
                          out=cqn[:, c, sl], in0=pl[c][0][:, :], scalar=pcol(g, PC_GQA + c), in1=rs[:],
                          op0=ALU.mult, op1=ALU.mult)
            (wqm,), wqmb = load_w(g, [((128, KC, 512), bw[:, :, 512:1024])])
            mem_attention(g, wqm, wqmb, hT, hT_b, PC_GMQ1, kmT, kmT_b, vm, vm_b, mixT, mix_b)
            kb.barrier()
            kb.stack = st0
        causal_attention(g, cqn, cqn_b, mixT, mix_b)
        with contextlib.ExitStack() as st:
            kb.stack = st
            alloc_ring(g, 8192)
            out_proj(g, 1, mixT, mix_b)
            kb.barrier()
            kb.stack = st0
        kb.stack = old0
    moe(g)


def _core_tokens(core):
    b, c = core // 4, core % 4
    iA, iB = c, 7 - c
    idx = np.concatenate([np.arange(512 * iA, 512 * iA + 512), np.arange(512 * iB, 512 * iB + 512)])
    return b, (iA, iB), idx


def _chunk(v):
    return np.ascontiguousarray(v.reshape(-1, 128).T)


def _params(inp, core):
    b, (iA, iB), idx = _core_tokens(core)
    P = np.zeros((128, NPC), np.float32)
    P[:, PC_GMIX0:PC_GMIX0 + 16] = _chunk(inp["g_mix"][0])
    P[:, PC_GFFN0:PC_GFFN0 + 16] = _chunk(inp["g_ffn"][0])
    P[:, PC_GKV:PC_GKV + 16] = _chunk(inp["g_kv"])
    P[:, PC_GMIX1:PC_GMIX1 + 16] = _chunk(inp["g_mix"][1])
    P[:, PC_GFFN1:PC_GFFN1 + 16] = _chunk(inp["g_ffn"][1])
    cw = inp["a_conv_w"][0]
    P[:, PC_CONV:PC_CONV + 36] = cw.reshape(3, 12, 128).transpose(2, 1, 0).reshape(128, 36)
    P[:, PC_GMQ0] = inp["g_mq"][0]
    P[:, PC_GMK0] = inp["g_mk"][0]
    P[:, PC_GMQ1] = inp["g_mq"][1]
    P[:, PC_GMK1] = inp["g_mk"][1]
    P[:, PC_GQA:PC_GQA + 4] = _chunk(inp["b_g_q_a"][0])
    P[:, PC_GKVA:PC_GKVA + 2] = _chunk(inp["g_kv_a"])
    gq, gk = inp["b_g_qn"][0], inp["g_kn"]
    P[:, PC_GQN_N] = gq[:128]
    P[:, PC_GKN_N] = gk[:128]
    P[:64, PC_GQN_R] = gq[128:]
    P[:64, PC_GQN_RS] = np.concatenate([gq[160:], gq[128:160]])
    P[:64, PC_GKN_R] = gk[128:]
    P[:64, PC_GKN_RS] = np.concatenate([gk[160:], gk[128:160]])
    P[:32, PC_SGN] = -1.0
    P[32:64, PC_SGN] = 1.0
    invf = (1.0 / (10000.0 ** (np.arange(0, 64, 2, dtype=np.float32) / np.float32(64)))).astype(np.float32)
    P[:64, PC_INVF] = np.concatenate([invf, invf])
    P[:, PC_HALO] = 0.0 if iA == 0 else 1.0
    P[:, PC_HALO + 1] = 1.0
    P[:, PC_KIDX:PC_KIDX + 32] = (np.arange(32)[None, :] * 128 + np.arange(128)[:, None]).astype(np.float32)
    return P


def _consts():
    ident = np.eye(128, dtype=np.float32)
    sel = np.zeros((8, NEXP * 128), np.float32)
    for e in range(NEXP):
        sel[e, e * 128:(e + 1) * 128] = 1.0
    return ident, sel


def host_inputs(inp, core, mode):
    b, (iA, iB), idx = _core_tokens(core)
    ident, sel = _consts()
    m = {
        "params": _params(inp, core), "ident": ident, "sel": sel,
        "mem": np.ascontiguousarray(inp["mem"][b]), "g_mem": inp["g_mem"],
        "w_mem_kv": inp["w_mem_kv"], "w_out": inp["w_out"],
        "pos": np.ascontiguousarray(inp["positions"][b, idx][None, :]).astype(np.int32),
    }
    if mode in ("L0", "fused"):
        m["xT"] = np.ascontiguousarray(inp["x"][b, idx, :].T)
        xh = np.zeros((4, D), np.float32)
        for t, i in enumerate((iA, iB)):
            if i > 0:
                xh[2 * t:2 * t + 2] = inp["x"][b, 512 * i - 2:512 * i, :]
        m["xhT"] = np.ascontiguousarray(xh.T)
        m["a_w_in"] = inp["a_w_in"][0]
        m["w_kv_a"] = inp["w_kv_a"]
        m["w_kv_b"] = inp["w_kv_b"]
        m["ffn_gate"] = inp["ffn_w_gate"][0]
        m["ffn_up"] = inp["ffn_w_up"][0]
        m["ffn_down"] = inp["ffn_w_down"][0]
    if mode in ("L1", "fused"):
        m["qidx"] = idx[None, :].astype(np.float32)
        m["b_w_in"] = inp["b_w_in"][0]
        m["w_q_b"] = inp["b_w_q_b"][0]
        m["w_router"] = inp["moe_w_router"][0]
        m["moe_gate"] = inp["moe_w_gate"][0]
        m["moe_up"] = inp["moe_w_up"][0]
        m["moe_down"] = inp["moe_w_down"][0]
    return m


_PROGS = {}


def _prog(mode):
    if mode not in _PROGS:
        _PROGS[mode] = build(mode)[0]
    return _PROGS[mode]


FUSED = True


def kernel(**inputs):
    inp = {k: np.asarray(v) for k, v in inputs.items()}
    cores = list(range(NCORE))
    if FUSED:
        maps = [host_inputs(inp, c, "fused") for c in cores]
        res = run_bass_kernel_spmd(_prog("fused"), maps, core_ids=cores).results
    else:
        maps0 = [host_inputs(inp, c, "L0") for c in cores]
        r0 = run_bass_kernel_spmd(_prog("L0"), maps0, core_ids=cores).results
        maps1 = []
        for c in cores:
            b = c // 4
            m = host_inputs(inp, c, "L1")
            m["xT"] = np.asarray(r0[c]["x1T"])
            m["KTg"] = np.concatenate([np.asarray(r0[4 * b + r]["KT_own"]) for r in range(4)], axis=0)
            m["Vg"] = np.concatenate([np.asarray(r0[4 * b + r]["V_own"]) for r in range(4)], axis=0)
            maps1.append(m)
        res = run_bass_kernel_spmd(_prog("L1"), maps1, core_ids=cores).results
    out = np.zeros((NB, SEQ, D), np.float32)
    for c in cores:
        b, _, idx = _core_tokens(c)
        out[b, idx, :] = np.asarray(res[c]["yT"]).T
    return out
```

```python
import contextlib
import numpy as np
import concourse.bass as bass
import concourse.mybir as mybir
from concourse.bass_utils import run_bass_kernel_spmd

F32 = mybir.dt.float32
BF16 = mybir.dt.bfloat16
I32 = mybir.dt.int32
AF = mybir.ActivationFunctionType
ALU = mybir.AluOpType


class Buf:
    __slots__ = ("name", "writer", "readers", "dsem", "dcount")

    def __init__(self, name):
        self.name = name
        self.writer = None
        self.readers = []
        self.dsem = None
        self.dcount = 0


class Eng:
    def __init__(self, name, h, sem):
        self.name = name
        self.h = h
        self.sem = sem
        self.cnt = 0
        self.known = {}


class KB:
    def __init__(self, nc, stack):
        self.nc = nc
        self.stack = stack
        self.gstack = stack
        self.nsem = 0
        self.E = {}
        for name, h in (("pe", nc.tensor), ("act", nc.scalar), ("dve", nc.vector),
                        ("pool", nc.gpsimd), ("sp", nc.sync)):
            self.E[name] = Eng(name, h, self.new_sem("e_" + name))
        self.nbuf = 0
        self.n_inst = 0

    def new_sem(self, name):
        self.nsem += 1
        return self.gstack.enter_context(self.nc.semaphore(f"{name}_{self.nsem}"))

    def buf(self, name=None):
        self.nbuf += 1
        return Buf(name or f"b{self.nbuf}")

    def sb(self, name, shape, dt):
        self.nbuf += 1
        return self.stack.enter_context(self.nc.sbuf_tensor(f"{name}_{self.nbuf}", list(shape), dt))

    def ps(self, name, shape, dt=F32):
        return self.stack.enter_context(self.nc.psum_tensor(name, list(shape), dt))

    def _wait(self, E, tok):
        sem, val, src = tok
        if src is E and E.name == "pe":
            return
        key = id(sem)
        if E.known.get(key, 0) >= val:
            return
        E.h.wait_ge(sem, val)
        E.known[key] = val

    def _sync(self, E, reads, writes):
        for b in reads:
            if b.writer is not None:
                self._wait(E, b.writer)
        for b in writes:
            if b.writer is not None and b.writer[2] is not E:
                self._wait(E, b.writer)
            for r in b.readers:
                if r[2] is not E:
                    self._wait(E, r)

    def _commit(self, tok, reads, writes):
        for b in reads:
            b.readers.append(tok)
            if len(b.readers) > 64:
                best = {}
                for t in b.readers:
                    k = id(t[0])
                    if k not in best or best[k][1] < t[1]:
                        best[k] = t
                b.readers = list(best.values())
        for b in writes:
            b.writer = tok
            b.readers = []

    def op(self, eng, reads, writes, fn, *a, **kw):
        E = self.E[eng]
        self._sync(E, reads, writes)
        ins = fn(*a, **kw)
        E.cnt += 1
        ins.then_inc(E.sem, 1)
        self.n_inst += 1
        self._commit((E.sem, E.cnt, E), reads, writes)
        return ins

    def mm(self, out_buf, out_ap, pairs, reads, transpose=False):
        E = self.E["pe"]
        self._sync(E, reads, [out_buf])
        n = len(pairs)
        ins = None
        for i, (l, r) in enumerate(pairs):
            ins = self.nc.tensor.matmul(out_ap, l, r, start=(i == 0), stop=(i == n - 1))
            self.n_inst += 1
        E.cnt += 1
        ins.then_inc(E.sem, 1)
        self._commit((E.sem, E.cnt, E), reads, [out_buf])

    def mm_open(self, reads, writes):
        E = self.E["pe"]
        self._sync(E, reads, writes)

    def mm_close(self, ins, reads, writes):
        E = self.E["pe"]
        E.cnt += 1
        ins.then_inc(E.sem, 1)
        self._commit((E.sem, E.cnt, E), reads, writes)

    def dma(self, q, out_ap, in_ap, sbuf_buf, reads, writes, **kw):
        E = self.E[q]
        b = sbuf_buf
        if b.dsem is None:
            b.dsem = self.new_sem("d_" + b.name)
        self._sync(E, reads, writes)
        if b.dcount:
            self._wait(E, (b.dsem, b.dcount, None))
        ins = E.h.dma_start(out=out_ap, in_=in_ap, **kw)
        b.dcount += 16
        ins.then_inc(b.dsem, 16)
        self.n_inst += 1
        tok = (b.dsem, b.dcount, None)
        self._commit(tok, reads, writes)
        return tok

    def wait_tok(self, eng, tok):
        self._wait(self.E[eng], tok)


    def barrier(self):
        toks = [(E.sem, E.cnt, E) for E in self.E.values() if E.cnt > 0]
        toks += [(b.dsem, b.dcount, None) for b in self._dbufs if b.dcount > 0]
        for E in self.E.values():
            for t in toks:
                if t[2] is E:
                    continue
                self._wait(E, t)


D = 2048
SEQ = 4096
NB = 2
NCORE = 8
TOK = 1024
TL = 512
NTL = 2
KC = 16
DFF = 7168
NEXP = 8
EPS = 1e-6
NPC = 168

PC_GMIX0, PC_GFFN0, PC_GKV, PC_GMIX1, PC_GFFN1 = 0, 16, 32, 48, 64
PC_CONV = 80
PC_GMQ0, PC_GMK0, PC_GMQ1, PC_GMK1 = 116, 117, 118, 119
PC_GQA = 120
PC_GKVA = 124
PC_GQN_N, PC_GKN_N = 126, 127
PC_GQN_R, PC_GQN_RS, PC_GKN_R, PC_GKN_RS, PC_SGN, PC_INVF = 128, 129, 130, 131, 132, 133
PC_HALO = 134
PC_KIDX = 136


class Ctx:
    pass


def build(mode):
    nc = bass.Bass("TRN2", target_bir_lowering=False)
    do0 = mode in ("L0", "fused")
    do1 = mode in ("L1", "fused")

    def din(name, shape, dt=F32):
        return nc.dram_tensor(name, list(shape), dt, kind="ExternalInput").ap()

    def dout(name, shape, dt=F32):
        return nc.dram_tensor(name, list(shape), dt, kind="ExternalOutput").ap()

    def dint(name, shape, dt=F32):
        return nc.dram_tensor(name, list(shape), dt, kind="Internal").ap()

    g = Ctx()
    g.nc = nc
    g.xT = din("xT", [D, TOK])
    g.params = din("params", [128, NPC])
    g.ident = din("ident", [128, 128])
    g.sel = din("sel", [8, NEXP * 128])
    g.mem = din("mem", [256, D])
    g.g_mem = din("g_mem", [2, D])
    g.w_mem_kv = din("w_mem_kv", [2, D, 1024])
    g.w_out = din("w_out", [2, D, D])
    if do0:
        g.xhT = din("xhT", [D, 4])
        g.pos = din("pos", [1, TOK], I32)
        g.a_w_in = din("a_w_in", [D, 5120])
        g.w_kv_a = din("w_kv_a", [D, 320])
        g.w_kv_b = din("w_kv_b", [256, 3072])
        g.ffn_gate = din("ffn_gate", [D, DFF])
        g.ffn_up = din("ffn_up", [D, DFF])
        g.ffn_down = din("ffn_down", [DFF, D])
    if do1:
        if not do0:
            g.pos = din("pos", [1, TOK], I32)
        g.qidx = din("qidx", [1, TOK])
        g.b_w_in = din("b_w_in", [D, 1024])
        g.w_q_b = din("w_q_b", [512, 2304])
        g.w_router = din("w_router", [D, NEXP])
        g.moe_gate = din("moe_gate", [NEXP, D, DFF])
        g.moe_up = din("moe_up", [NEXP, D, DFF])
        g.moe_down = din("moe_down", [NEXP, DFF, D])
        g.yT = dout("yT", [D, TOK])
    if mode == "L0":
        g.x1T = dout("x1T", [D, TOK])
        KT_own = dout("KT_own", [2304, TOK], BF16)
        V_own = dout("V_own", [TOK, 1536], BF16)
        g.kt_own = lambda h: KT_own[h * 192:(h + 1) * 192, :]
        g.v_own = lambda h: V_own[:, h * 128:(h + 1) * 128]
    elif mode == "L1":
        KTg = din("KTg", [4 * 2304, TOK], BF16)
        Vg = din("Vg", [4 * TOK, 1536], BF16)
        g.kt_g = lambda h, r: KTg[r * 2304 + h * 192:r * 2304 + (h + 1) * 192, :]
        g.v_g = lambda h, r: Vg[r * TOK:(r + 1) * TOK, h * 128:(h + 1) * 128]
    else:
        g.KT_own_l = [dint(f"KT_own{h}", [192, TOK], BF16) for h in range(12)]
        g.V_own_l = [dint(f"V_own{h}", [TOK, 128], BF16) for h in range(12)]
        g.KTg_l = [dint(f"KTg{h}", [4 * 192, TOK], BF16) for h in range(12)]
        g.Vg_l = [dint(f"Vg{h}", [4 * TOK, 128], BF16) for h in range(12)]
        g.kt_own = lambda h: g.KT_own_l[h]
        g.v_own = lambda h: g.V_own_l[h]
        g.kt_g = lambda h, r: g.KTg_l[h][r * 192:(r + 1) * 192, :]
        g.v_g = lambda h, r: g.Vg_l[h][r * TOK:(r + 1) * TOK, :]

    with contextlib.ExitStack() as st:
        kb = KB(nc, st)
        kb._dbufs = []
        g.kb = kb
        setup_globals(g)
        if do0:
            layer0(g)
            shared_kv(g)
            if mode == "L0":
                store_x(g, g.x1T)
        if mode == "fused":
            exchange_kv(g)
        if do1:
            layer1(g)
            store_x(g, g.yT)
        kb.barrier()
        g.n_inst = kb.n_inst
    return nc, g


def setup_globals(g):
    kb, nc = g.kb, g.nc
    g.x = kb.sb("x", [128, KC, TOK], F32)
    g.xb = [[kb.buf(f"x{kc}_{t}") for t in range(NTL)] for kc in range(KC)]
    g.par = kb.sb("par", [128, NPC], F32)
    g.par_b = kb.buf("par")
    g.idf = kb.sb("idf", [128, 128], F32)
    g.idb = kb.sb("idb", [128, 128], BF16)
    g.ones = kb.sb("ones", [128, 128], BF16)
    g.cst_b = kb.buf("cst")
    g.cs = kb.sb("cs", [64, 2, TOK], F32)
    g.cs_b = kb.buf("cs")
    g.pst = [kb.ps(f"ps{i}", [128, 512]) for i in range(8)]
    g.psb = [kb.buf(f"ps{i}") for i in range(8)]
    g.bank_i = 0
    g.bank_set = list(range(8))
    g.ffn_pending = []
    g.sq = [kb.sb(f"sq{i}", [128, 512], BF16) for i in range(4)]
    g.sq_b = [kb.buf(f"sq{i}") for i in range(4)]
    g.sq_i = 0
    g.rs = [kb.sb(f"rs{i}", [128, 512], F32) for i in range(3)]
    g.rs_b = [kb.buf(f"rs{i}") for i in range(3)]
    g.rs_i = 0
    g.fs = [kb.sb(f"fs{i}", [128, 514], F32) for i in range(3)]
    g.fs_b = [kb.buf(f"fs{i}") for i in range(3)]
    g.fs_i = 0

    xb_all = [b for row in g.xb for b in row]
    xv = g.xT.rearrange("(kc p) n -> p kc n", p=128)
    ldb = kb.buf("xload"); kb._dbufs.append(ldb)
    for t in range(NTL):
        kb.dma("sp", g.x[:, :, t * TL:(t + 1) * TL], xv[:, :, t * TL:(t + 1) * TL], ldb, [],
               [g.xb[kc][t] for kc in range(KC)])
    cb = kb.buf("cload"); kb._dbufs.append(cb)
    kb.dma("sp", g.par[:], g.params, cb, [], [g.par_b])
    kb.dma("sp", g.idf[:], g.ident, cb, [], [g.cst_b])
    kb.op("dve", [g.cst_b], [g.cst_b], nc.vector.tensor_copy, g.idb[:], g.idf[:])
    kb.op("dve", [], [g.cst_b], nc.vector.memset, g.ones[:], 1.0)
    rope_tables(g)


def bank(g):
    bs = g.bank_set
    for _ in range(len(bs)):
        i = bs[g.bank_i % len(bs)]
        g.bank_i += 1
        b = g.psb[i]
        if b.writer is None or b.readers:
            return g.pst[i], b
    raise RuntimeError("no free PSUM bank")


def alloc_ring(g, slot_el, nslot=2):
    kb = g.kb
    g.NSLOT = nslot
    g.SLOT = slot_el
    g.ring_gen = getattr(g, "ring_gen", 0) + 1
    g.wslot = [kb.sb(f"wslot{g.ring_gen}_{i}", [128, slot_el], BF16) for i in range(nslot)]
    g.wslot_b = [kb.buf(f"wslot{g.ring_gen}_{i}") for i in range(nslot)]
    for b in g.wslot_b:
        kb._dbufs.append(b)
    g.slot_i = 0


def scratch(g, kind):
    lst, bl, key = {"sq": (g.sq, g.sq_b, "sq_i"), "rs": (g.rs, g.rs_b, "rs_i"),
                    "fs": (g.fs, g.fs_b, "fs_i")}[kind]
    i = getattr(g, key)
    setattr(g, key, (i + 1) % len(lst))
    return lst[i], bl[i]


def load_w(g, pieces):
    kb = g.kb
    i = g.slot_i
    g.slot_i = (i + 1) % g.NSLOT
    t, b = g.wslot[i], g.wslot_b[i]
    views = []
    off = 0
    E = kb.E["pool"]
    kb._sync(E, [], [b])
    if b.dsem is None:
        b.dsem = kb.new_sem("d_" + b.name)
    if b.dcount:
        kb._wait(E, (b.dsem, b.dcount, None))
    for shape, src in pieces:
        n = int(np.prod(shape[1:]))
        v = t[:shape[0], off:off + n]
        if len(shape) == 3:
            v = v.rearrange("p (a b) -> p a b", a=shape[1])
        elif len(shape) == 4:
            v = v.rearrange("p (a b c) -> p a b c", a=shape[1], b=shape[2])
        ins = g.nc.gpsimd.dma_start(out=v, in_=src, max_dma_last_dim=4096)
        b.dcount += 16
        ins.then_inc(b.dsem, 16)
        kb.n_inst += 1
        views.append(v)
        off += n
    assert off <= g.SLOT
    kb._commit((b.dsem, b.dcount, None), [], [b])
    return views, b


def pcol(g, c, P=128):
    return g.par[:P, c:c + 1]


def rope_tables(g):
    kb, nc = g.kb, g.nc
    with contextlib.ExitStack() as st:
        old = kb.stack
        kb.stack = st
        posi = kb.sb("posi", [64, TOK], I32); pb = kb.buf("posi"); kb._dbufs.append(pb)
        ang = kb.sb("ang", [64, TOK], F32); ab = kb.buf("ang")
        t1 = kb.sb("rt1", [64, TOK], F32); t1b = kb.buf("rt1")
        ki = kb.sb("rki", [64, TOK], I32); kib = kb.buf("rki")
        kf = kb.sb("rkf", [64, TOK], F32); kfb = kb.buf("rkf")
        m = kb.sb("rm", [64, TOK], F32); mb = kb.buf("rm")
        kb.dma("sp", posi[:], g.pos.partition_broadcast(64).rearrange("p a n -> p (a n)"), pb, [], [pb])
        kb.op("dve", [pb], [ab], nc.vector.tensor_copy, ang[:], posi[:])
        kb.op("dve", [ab, g.par_b], [ab], nc.vector.tensor_scalar, out=ang[:], in0=ang[:],
              scalar1=pcol(g, PC_INVF, 64), scalar2=None, op0=ALU.mult)
        TWO_PI = 2.0 * np.pi
        for which, shift in ((1, 0.0), (0, np.pi / 2)):
            kb.op("dve", [ab], [t1b], nc.vector.tensor_scalar, out=t1[:], in0=ang[:],
                  scalar1=float(shift), scalar2=float(1.0 / TWO_PI), op0=ALU.add, op1=ALU.mult)
            kb.op("dve", [t1b], [kib], nc.vector.tensor_copy, ki[:], t1[:])
            kb.op("dve", [kib], [kfb], nc.vector.tensor_copy, kf[:], ki[:])
            kb.op("dve", [kfb, ab], [t1b], nc.vector.scalar_tensor_tensor, out=t1[:], in0=kf[:],
                  scalar=float(-TWO_PI), in1=ang[:], op0=ALU.mult, op1=ALU.add)
            if shift:
                kb.op("dve", [t1b], [t1b], nc.vector.tensor_scalar, out=t1[:], in0=t1[:],
                      scalar1=float(shift), scalar2=None, op0=ALU.add)
            kb.op("dve", [t1b], [mb], nc.vector.tensor_scalar, out=m[:], in0=t1[:],
                  scalar1=float(np.pi), scalar2=float(-TWO_PI), op0=ALU.is_gt, op1=ALU.mult)
            kb.op("dve", [mb, t1b], [t1b], nc.vector.tensor_tensor, out=t1[:], in0=t1[:], in1=m[:], op=ALU.add)
            kb.op("dve", [t1b], [mb], nc.vector.tensor_scalar, out=m[:], in0=t1[:],
                  scalar1=float(-np.pi), scalar2=float(TWO_PI), op0=ALU.is_lt, op1=ALU.mult)
            kb.op("dve", [mb, t1b], [t1b], nc.vector.tensor_tensor, out=t1[:], in0=t1[:], in1=m[:], op=ALU.add)
            kb.op("dve", [t1b], [t1b], nc.vector.tensor_scalar, out=t1[:], in0=t1[:],
                  scalar1=float(np.pi), scalar2=float(-np.pi), op0=ALU.min, op1=ALU.max)
            kb.op("act", [t1b], [g.cs_b], nc.scalar.activation, out=g.cs[:, which, :], in_=t1[:], func=AF.Sin)
        kb.op("dve", [g.cs_b, g.par_b], [g.cs_b], nc.vector.tensor_scalar, out=g.cs[:, 1, :], in0=g.cs[:, 1, :],
              scalar1=pcol(g, PC_SGN, 64), scalar2=None, op0=ALU.mult)
        kb.barrier()
        kb.stack = old


def sumsq_rstd(g, srcs, N, Dn, reads):
    kb, nc = g.kb, g.nc
    pt, pb = bank(g)
    n = len(srcs)
    for i, (ap, P) in enumerate(srcs):
        sq, sqb = scratch(g, "sq")
        kb.op("act", reads, [sqb], nc.scalar.activation, out=sq[:P, :N], in_=ap, func=AF.Square)
        kb.mm_open([sqb, g.cst_b], [pb] if i == 0 else [])
        ins = nc.tensor.matmul(pt[:, :N], g.ones[:P, :], sq[:P, :N], start=(i == 0), stop=(i == n - 1))
        kb.n_inst += 1
        kb.mm_close(ins, [sqb], [pb])
    rs, rsb = scratch(g, "rs")
    kb.op("act", [pb], [rsb], nc.scalar.activation, out=rs[:, :N], in_=pt[:, :N], func=AF.Sqrt,
          scale=float(1.0 / Dn), bias=float(EPS))
    kb.op("dve", [rsb], [rsb], nc.vector.reciprocal, rs[:, :N], rs[:, :N])
    return rs, rsb


def norm_x(g, gcol0, hT, hT_b, tiles=(0, 1), on_rstd=None):
    kb, nc = g.kb, g.nc
    for t in tiles:
        sl = slice(t * TL, (t + 1) * TL)
        rs, rsb = sumsq_rstd(g, [(g.x[:, kc, sl], 128) for kc in range(KC)], TL, D,
                             [g.xb[kc][t] for kc in range(KC)])
        if on_rstd is not None:
            on_rstd(t, rs, rsb)
        for kc in range(KC):
            kb.op("dve", [g.xb[kc][t], rsb, g.par_b], [hT_b[t]], nc.vector.scalar_tensor_tensor,
                  out=hT[:, kc, sl], in0=g.x[:, kc, sl], scalar=pcol(g, gcol0 + kc), in1=rs[:, :TL],
                  op0=ALU.mult, op1=ALU.mult)


def store_x(g, dst):
    kb = g.kb
    dv = dst.rearrange("(kc p) n -> p kc n", p=128)
    sb = kb.buf("xstore"); kb._dbufs.append(sb)
    for t in range(NTL):
        kb.dma("sp", dv[:, :, t * TL:(t + 1) * TL], g.x[:, :, t * TL:(t + 1) * TL], sb,
               [g.xb[kc][t] for kc in range(KC)], [])


def mem_kv(g, l, kmT, kmT_b, vm, vm_b):
    kb, nc = g.kb, g.nc
    with contextlib.ExitStack() as st:
        old = kb.stack
        kb.stack = st
        mt = kb.sb("mem_t", [128, 2, D], F32); mtb = kb.buf("mem_t"); kb._dbufs.append(mtb)
        gb = kb.sb("gmem_bc", [128, D], F32); gbb = kb.buf("gmem_bc"); kb._dbufs.append(gbb)
        mn = kb.sb("mem_n", [128, 2, D], BF16); mnb = kb.buf("mem_n")
        mnT = kb.sb("mem_nT", [128, KC, 256], BF16); mnTb = kb.buf("mem_nT")
        ssq = kb.sb("mem_ss", [128, 2], F32); ssb = kb.buf("mem_ss")
        kb.dma("sp", mt[:], g.mem.rearrange("(mb p) d -> p mb d", p=128), mtb, [], [mtb])
        kb.dma("sp", gb[:], g.g_mem[l:l + 1, :].partition_broadcast(128).rearrange("p a n -> p (a n)"),
               gbb, [], [gbb])
        for mb in range(2):
            kb.op("act", [mtb], [mnb, ssb], nc.scalar.activation, out=mn[:, mb, :], in_=mt[:, mb, :],
                  func=AF.Square, accum_out=ssq[:, mb:mb + 1])
        kb.op("act", [ssb], [ssb], nc.scalar.activation, out=ssq[:], in_=ssq[:], func=AF.Sqrt,
              scale=float(1.0 / D), bias=float(EPS))
        kb.op("dve", [ssb], [ssb], nc.vector.reciprocal, ssq[:], ssq[:])
        for mb in range(2):
            kb.op("dve", [mtb, ssb, gbb], [mnb], nc.vector.scalar_tensor_tensor, out=mn[:, mb, :],
                  in0=mt[:, mb, :], scalar=ssq[:, mb:mb + 1], in1=gb[:], op0=ALU.mult, op1=ALU.mult)
        for mb in range(2):
            for k4 in range(4):
                pt, pb = bank(g)
                ptb = pt[:].bitcast(BF16)
                kb.mm_open([mnb, g.cst_b], [pb])
                ins = None
                for q in range(4):
                    kc = k4 * 4 + q
                    ins = nc.tensor.transpose(ptb[:, q * 128:(q + 1) * 128], mn[:, mb, kc * 128:(kc + 1) * 128],
                                              g.idb[:])
                    kb.n_inst += 1
                kb.mm_close(ins, [mnb], [pb])
                kb.op("dve", [pb], [mnTb], nc.vector.tensor_copy,
                      mnT[:, k4 * 4:(k4 + 1) * 4, mb * 128:(mb + 1) * 128],
                      ptb[:, 0:512].rearrange("p (q n) -> p q n", q=4))
        wv = g.w_mem_kv[l].rearrange("(kc p) m -> p kc m", p=128)
        (wk,), wkb = load_w(g, [((128, KC, 512), wv[:, :, 0:512])])
        (wvv,), wvb = load_w(g, [((128, KC, 512), wv[:, :, 512:1024])])
        gk = PC_GMK0 if l == 0 else PC_GMK1
        for hh in range(4):
            pt, pb = bank(g)
            kb.mm(pb, pt[:, :256], [(wk[:, kc, hh * 128:(hh + 1) * 128], mnT[:, kc, :]) for kc in range(KC)],
                  [wkb, mnTb])
            rs, rsb = sumsq_rstd(g, [(pt[:, :256], 128)], 256, 128, [pb])
            kb.op("dve", [pb, rsb, g.par_b], [kmT_b], nc.vector.scalar_tensor_tensor, out=kmT[:, hh, :],
                  in0=pt[:, :256], scalar=pcol(g, gk), in1=rs[:, :256], op0=ALU.mult, op1=ALU.mult)
        for mb in range(2):
            pt, pb = bank(g)
            kb.mm(pb, pt[:, :], [(mnT[:, kc, mb * 128:(mb + 1) * 128], wvv[:, kc, :]) for kc in range(KC)],
                  [wvb, mnTb])
            kb.op("act", [pb], [vm_b], nc.scalar.copy, vm[:, mb, :], pt[:, :])
        kb.barrier()
        kb.stack = old


def mem_attention(g, wq, wqb, hT, hT_b, gq_col, kmT, kmT_b, vm, vm_b, mixT, mix_b):
    kb, nc = g.kb, g.nc
    sc = float(128 ** -0.5)
    qm = [kb.sb(f"qm{i}", [128, TL], BF16) for i in range(2)]
    qm_b = [kb.buf(f"qm{i}") for i in range(2)]
    qi = 0
    for t in range(NTL):
        sl = slice(t * TL, (t + 1) * TL)
        for hh in range(4):
            pt, pb = bank(g)
            kb.mm(pb, pt[:, :], [(wq[:, kc, hh * 128:(hh + 1) * 128], hT[:, kc, sl]) for kc in range(KC)],
                  [wqb, hT_b[t]])
            rs, rsb = sumsq_rstd(g, [(pt[:, :], 128)], TL, 128, [pb])
            q, qb = qm[qi % 2], qm_b[qi % 2]
            qi += 1
            kb.op("dve", [pb, rsb, g.par_b], [qb], nc.vector.scalar_tensor_tensor, out=q[:],
                  in0=pt[:, :], scalar=pcol(g, gq_col), in1=rs[:], op0=ALU.mult, op1=ALU.mult)
            po, pob = bank(g)
            pd, pdb = bank(g)
            for mb in range(2):
                ps_, psb_ = bank(g)
                kb.mm(psb_, ps_[:, :], [(kmT[:, hh, mb * 128:(mb + 1) * 128], q[:])], [kmT_b, qb])
                e, eb = scratch(g, "sq")
                kb.op("act", [psb_], [eb], nc.scalar.activation, out=e[:], in_=ps_[:, :], func=AF.Exp, scale=sc)
                kb.mm_open([eb, vm_b, g.cst_b], [pob, pdb] if mb == 0 else [])
                nc.tensor.matmul(po[:, :], vm[:, mb, hh * 128:(hh + 1) * 128], e[:], start=(mb == 0), stop=(mb == 1))
                ins = nc.tensor.matmul(pd[:, :], g.ones[:, :], e[:], start=(mb == 0), stop=(mb == 1))
                kb.n_inst += 2
                kb.mm_close(ins, [eb, vm_b], [pob, pdb])
            rd, rdb = scratch(g, "rs")
            kb.op("dve", [pdb], [rdb], nc.vector.reciprocal, rd[:], pd[:, :])
            kb.op("dve", [pob, rdb], [mix_b[t]], nc.vector.tensor_tensor, out=mixT[:, 12 + hh, sl], in0=po[:, :],
                  in1=rd[:], op=ALU.mult)


def out_proj(g, l, mixT, mix_b):
    kb, nc = g.kb, g.nc
    wv = g.w_out[l].rearrange("(kc p) m -> p kc m", p=128)
    for pc in range(4):
        (w,), wb = load_w(g, [((128, KC, 512), wv[:, :, pc * 512:(pc + 1) * 512])])
        for t in range(NTL):
            sl = slice(t * TL, (t + 1) * TL)
            for dq in range(4):
                dc = pc * 4 + dq
                pt, pb = bank(g)
                kb.mm(pb, pt[:, :], [(w[:, kc, dq * 128:(dq + 1) * 128], mixT[:, kc, sl]) for kc in range(KC)],
                      [wb, mix_b[t]])
                kb.op("dve", [pb, g.xb[dc][t]], [g.xb[dc][t]], nc.vector.tensor_tensor, out=g.x[:, dc, sl],
                      in0=g.x[:, dc, sl], in1=pt[:, :], op=ALU.add)


def ffn_drain(g, n=None):
    pend = g.ffn_pending
    k = len(pend) if n is None else min(n, len(pend))
    for _ in range(k):
        pend.pop(0)()


def ffn_group(g, wg, wu, wd, wb, hT, hT_b, gate_bc=None, gate_bcb=None, act=None, act_b=None):
    kb, nc = g.kb, g.nc
    for t in range(NTL):
        sl = slice(t * TL, (t + 1) * TL)
        for m in range(2):
            pg, pgb = bank(g)
            kb.mm(pgb, pg[:, :], [(wg[:, kc, m * 128:(m + 1) * 128], hT[:, kc, sl]) for kc in range(KC)],
                  [wb, hT_b[t]])
            sg, sgb = scratch(g, "sq")
            kb.op("act", [pgb], [sgb], nc.scalar.activation, out=sg[:], in_=pg[:, :], func=AF.Silu)
            ffn_drain(g, 4)
            pu, pub = bank(g)
            kb.mm(pub, pu[:, :], [(wu[:, kc, m * 128:(m + 1) * 128], hT[:, kc, sl]) for kc in range(KC)],
                  [wb, hT_b[t]])
            if gate_bc is None:
                kb.op("dve", [pub, sgb], [act_b[t][m]], nc.vector.tensor_tensor, out=act[t][:, m, :], in0=pu[:, :],
                      in1=sg[:], op=ALU.mult)
            else:
                ug, ugb = scratch(g, "fs")
                kb.op("dve", [pub, gate_bcb[t]], [ugb], nc.vector.tensor_tensor, out=ug[:, :TL], in0=pu[:, :],
                      in1=gate_bc[:, sl], op=ALU.mult)
                kb.op("dve", [ugb, sgb], [act_b[t][m]], nc.vector.tensor_tensor, out=act[t][:, m, :],
                      in0=ug[:, :TL], in1=sg[:], op=ALU.mult)
            ffn_drain(g, 4)
        ffn_drain(g)

        def down(dc, t=t, sl=sl, wd=wd, wb=wb):
            pt, pb = bank(g)
            kb.mm(pb, pt[:, :], [(wd[m][:, dc * 128:(dc + 1) * 128], act[t][:, m, :]) for m in range(2)],
                  [wb, act_b[t][0], act_b[t][1]])
            kb.op("dve", [pb, g.xb[dc][t]], [g.xb[dc][t]], nc.vector.tensor_tensor, out=g.x[:, dc, sl],
                  in0=g.x[:, dc, sl], in1=pt[:, :], op=ALU.add)

        for dc in range(KC):
            g.ffn_pending.append(lambda dc=dc, f=down: f(dc))


def ffn_weights(g, wgate, wup, wdown, gi):
    c0 = gi * 256
    gv = wgate.rearrange("(kc p) m -> p kc m", p=128)[:, :, c0:c0 + 256]
    uv = wup.rearrange("(kc p) m -> p kc m", p=128)[:, :, c0:c0 + 256]
    dv = wdown[c0:c0 + 256, :].rearrange("(m p) d -> p m d", p=128)
    (wg, wu, wd0, wd1), wb = load_w(g, [((128, KC, 256), gv), ((128, KC, 256), uv), ((128, D), dv[:, 0, :]),
                                          ((128, D), dv[:, 1, :])])
    return wg, wu, (wd0, wd1), wb


def layer0(g):
    kb, nc = g.kb, g.nc
    with contextlib.ExitStack() as st:
        old = kb.stack
        kb.stack = st
        alloc_ring(g, 8192)
        kmT = kb.sb("kmT", [128, 4, 256], BF16); kmT_b = kb.buf("kmT")
        vm = kb.sb("vm", [128, 2, 512], BF16); vm_b = kb.buf("vm")
        mem_kv(g, 0, kmT, kmT_b, vm, vm_b)
        hT = kb.sb("hT", [128, KC, TOK], BF16); hT_b = [kb.buf("hT0"), kb.buf("hT1")]
        mixT = kb.sb("mixT", [128, KC, TOK], BF16); mix_b = [kb.buf("mix0"), kb.buf("mix1")]
        xh = kb.sb("xh", [128, KC, 4], F32); xhb = kb.buf("xh"); kb._dbufs.append(xhb)
        hh_ = kb.sb("hh", [128, KC, 4], BF16); hhb = kb.buf("hh")
        zh = kb.sb("zh", [128, 8], F32); zhb = kb.buf("zh")
        kb.dma("sp", xh[:], g.xhT.rearrange("(kc p) n -> p kc n", p=128), xhb, [], [xhb])
        norm_x(g, PC_GMIX0, hT, hT_b)
        rs, rsb = sumsq_rstd(g, [(xh[:, kc, :], 128) for kc in range(KC)], 4, D, [xhb])
        for kc in range(KC):
            kb.op("dve", [xhb, rsb, g.par_b], [hhb], nc.vector.scalar_tensor_tensor, out=hh_[:, kc, :],
                  in0=xh[:, kc, :], scalar=pcol(g, PC_GMIX0 + kc), in1=rs[:, :4], op0=ALU.mult, op1=ALU.mult)
        wa = g.a_w_in.rearrange("(kc p) m -> p kc m", p=128)
        for j in range(12):
            w, wb = load_w(g, [((128, KC, 128), wa[:, :, s_ * 1536 + j * 128:s_ * 1536 + (j + 1) * 128])
                               for s_ in range(3)])
            ph, phb = bank(g)
            kb.mm(phb, ph[:, 0:4], [(w[0][:, kc, :], hh_[:, kc, :]) for kc in range(KC)], [wb, hhb])
            kb.mm(phb, ph[:, 4:8], [(w[2][:, kc, :], hh_[:, kc, :]) for kc in range(KC)], [wb, hhb])
            kb.op("act", [phb], [zhb], nc.scalar.copy, zh[:, 0:8], ph[:, 0:8])
            for t in range(NTL):
                sl = slice(t * TL, (t + 1) * TL)
                px, pxb = bank(g)
                kb.mm(pxb, px[:, :], [(w[0][:, kc, :], hT[:, kc, sl]) for kc in range(KC)], [wb, hT_b[t]])
                pc_, pcb = bank(g)
                kb.mm(pcb, pc_[:, :], [(w[2][:, kc, :], hT[:, kc, sl]) for kc in range(KC)], [wb, hT_b[t]])
                pg, pgb = bank(g)
                kb.mm(pgb, pg[:, :], [(w[1][:, kc, :], hT[:, kc, sl]) for kc in range(KC)], [wb, hT_b[t]])
                gc, gcb = scratch(g, "rs")
                kb.op("act", [pcb], [gcb], nc.scalar.copy, gc[:, :TL], pc_[:, :])
                z, zb = scratch(g, "fs")
                kb.op("dve", [pxb, gcb], [zb], nc.vector.tensor_tensor, out=z[:, 2:2 + TL], in0=px[:, :],
                      in1=gc[:, :TL], op=ALU.mult)
                kb.op("dve", [zhb, g.par_b, zb], [zb], nc.vector.scalar_tensor_tensor, out=z[:, 0:2],
                      in0=zh[:, 2 * t:2 * t + 2], scalar=pcol(g, PC_HALO + t), in1=zh[:, 4 + 2 * t:6 + 2 * t],
                      op0=ALU.mult, op1=ALU.mult)
                y, yb = scratch(g, "rs")
                cw = PC_CONV + j * 3
                kb.op("dve", [zb, g.par_b], [yb], nc.vector.tensor_scalar, out=y[:, :TL], in0=z[:, 0:TL],
                      scalar1=pcol(g, cw), scalar2=None, op0=ALU.mult)
                kb.op("dve", [zb, yb, g.par_b], [yb], nc.vector.scalar_tensor_tensor, out=y[:, :TL],
                      in0=z[:, 1:1 + TL], scalar=pcol(g, cw + 1), in1=y[:, :TL], op0=ALU.mult, op1=ALU.add)
                kb.op("dve", [zb, yb, g.par_b], [yb], nc.vector.scalar_tensor_tensor, out=y[:, :TL],
                      in0=z[:, 2:2 + TL], scalar=pcol(g, cw + 2), in1=y[:, :TL], op0=ALU.mult, op1=ALU.add)
                kb.op("dve", [pgb, yb], [mix_b[t]], nc.vector.tensor_tensor, out=mixT[:, j, sl], in0=pg[:, :],
                      in1=y[:, :TL], op=ALU.mult)
        (wq,), wqb = load_w(g, [((128, KC, 512),
                                  g.a_w_in.rearrange("(kc p) m -> p kc m", p=128)[:, :, 4608:5120])])
        mem_attention(g, wq, wqb, hT, hT_b, PC_GMQ0, kmT, kmT_b, vm, vm_b, mixT, mix_b)
        out_proj(g, 0, mixT, mix_b)
        kb.barrier()
        kb.stack = old
    with contextlib.ExitStack() as st:
        old = kb.stack
        kb.stack = st
        alloc_ring(g, 12288)
        hT = kb.sb("hT", [128, KC, TOK], BF16); hT_b = [kb.buf("hT0"), kb.buf("hT1")]
        norm_x(g, PC_GFFN0, hT, hT_b)
        act = [kb.sb(f"act{t}", [128, 2, TL], BF16) for t in range(NTL)]
        act_b = [[kb.buf(f"act{t}_{m}") for m in range(2)] for t in range(NTL)]
        for gi in range(DFF // 256):
            wg, wu, wd, wb = ffn_weights(g, g.ffn_gate, g.ffn_up, g.ffn_down, gi)
            ffn_group(g, wg, wu, wd, wb, hT, hT_b, act=act, act_b=act_b)
        ffn_drain(g)
        kb.barrier()
        kb.stack = old


def rope_AB(g, gcol, gswcol, AB, AB_b):
    kb, nc = g.kb, g.nc
    kb.op("dve", [g.cs_b, g.par_b], [AB_b], nc.vector.tensor_scalar, out=AB[:, 0, :], in0=g.cs[:, 0, :],
          scalar1=pcol(g, gcol, 64), scalar2=None, op0=ALU.mult)
    kb.op("dve", [g.cs_b, g.par_b], [AB_b], nc.vector.tensor_scalar, out=AB[:, 1, :], in0=g.cs[:, 1, :],
          scalar1=pcol(g, gswcol, 64), scalar2=None, op0=ALU.mult)


def shared_kv(g):
    kb, nc = g.kb, g.nc
    with contextlib.ExitStack() as st:
        old = kb.stack
        kb.stack = st
        hT = kb.sb("hT", [128, KC, TOK], BF16); hT_b = [kb.buf("hT0"), kb.buf("hT1")]
        ckv = kb.sb("ckv", [128, 2, TOK], BF16); ckv_b = [kb.buf("ckv0"), kb.buf("ckv1")]
        Rk = kb.sb("Rk", [64, TOK], F32); Rk_b = [kb.buf("Rk0"), kb.buf("Rk1")]
        sqpe = kb.sb("sqpe", [64, TOK], BF16); sqpe_b = [kb.buf("sqpe0"), kb.buf("sqpe1")]
        AB = kb.sb("ABk", [64, 2, TOK], F32); AB_b = kb.buf("ABk")
        kst = [kb.sb(f"kst{i}", [128, TOK], BF16) for i in range(2)]
        kst_b = [kb.buf(f"kst{i}") for i in range(2)]
        krst = [kb.sb(f"krst{i}", [64, TOK], BF16) for i in range(2)]
        krst_b = [kb.buf(f"krst{i}") for i in range(2)]
        vst = [kb.sb(f"vst{i}", [128, 512], BF16) for i in range(2)]
        vst_b = [kb.buf(f"vst{i}") for i in range(2)]
        for b in kst_b + krst_b + vst_b:
            kb._dbufs.append(b)
        alloc_ring(g, 8192)
        norm_x(g, PC_GKV, hT, hT_b)
        rope_AB(g, PC_GKN_R, PC_GKN_RS, AB, AB_b)
        wav = g.w_kv_a.rearrange("(kc p) m -> p kc m", p=128)
        (wa,), wab = load_w(g, [((128, KC, 320), wav)])
        wkbv = g.w_kv_b.rearrange("(kc p) m -> p kc m", p=128)
        (wb0, wb1), wbb = load_w(g, [((128, 3072), wkbv[:, 0, :]), ((128, 3072), wkbv[:, 1, :])])
        wbv = [wb0.rearrange("p (h c) -> p h c", h=12), wb1.rearrange("p (h c) -> p h c", h=12)]
        for t in range(NTL):
            sl = slice(t * TL, (t + 1) * TL)
            pl = []
            for c in range(2):
                pt, pb = bank(g)
                kb.mm(pb, pt[:, :], [(wa[:, kc, c * 128:(c + 1) * 128], hT[:, kc, sl]) for kc in range(KC)],
                      [wab, hT_b[t]])
                pl.append((pt, pb))
            rs, rsb = sumsq_rstd(g, [(pl[0][0][:, :], 128), (pl[1][0][:, :], 128)], TL, 256,
                                 [pl[0][1], pl[1][1]])
            for c in range(2):
                kb.op("dve", [pl[c][1], rsb, g.par_b], [ckv_b[t]], nc.vector.scalar_tensor_tensor,
                      out=ckv[:, c, sl], in0=pl[c][0][:, :], scalar=pcol(g, PC_GKVA + c), in1=rs[:],
                      op0=ALU.mult, op1=ALU.mult)
            pp, ppb = bank(g)
            kb.mm(ppb, pp[:64, :], [(wa[:, kc, 256:320], hT[:, kc, sl]) for kc in range(KC)], [wab, hT_b[t]])
            pq, pqb = bank(g)
            kb.mm_open([wab, hT_b[t]], [pqb])
            ins = None
            for kc in range(KC):
                nc.tensor.matmul(pq[0:32, :], wa[:, kc, 288:320], hT[:, kc, sl], start=(kc == 0), stop=(kc == KC - 1))
                ins = nc.tensor.matmul(pq[32:64, :], wa[:, kc, 256:288], hT[:, kc, sl], start=(kc == 0),
                                       stop=(kc == KC - 1))
                kb.n_inst += 2
            kb.mm_close(ins, [wab, hT_b[t]], [pqb])
            kb.op("act", [ppb], [sqpe_b[t]], nc.scalar.activation, out=sqpe[:, sl], in_=pp[:64, :], func=AF.Square)
            r1, r1b = scratch(g, "fs")
            kb.op("dve", [ppb, AB_b], [r1b], nc.vector.tensor_tensor, out=r1[:64, :TL], in0=pp[:64, :],
                  in1=AB[:, 0, sl], op=ALU.mult)
            kb.op("dve", [pqb, AB_b], [Rk_b[t]], nc.vector.tensor_tensor, out=Rk[:, sl], in0=pq[:64, :],
                  in1=AB[:, 1, sl], op=ALU.mult)
            kb.op("dve", [r1b, Rk_b[t]], [Rk_b[t]], nc.vector.tensor_tensor, out=Rk[:, sl], in0=Rk[:, sl],
                  in1=r1[:64, :TL], op=ALU.add)
        for h in range(12):
            si = h % 2
            for t in range(NTL):
                sl = slice(t * TL, (t + 1) * TL)
                pt, pb = bank(g)
                kb.mm(pb, pt[:, :], [(wbv[c][:, h, 0:128], ckv[:, c, sl]) for c in range(2)], [wbb, ckv_b[t]])
                ps_, psb_ = bank(g)
                sq, sqb = scratch(g, "sq")
                kb.op("act", [pb], [sqb], nc.scalar.activation, out=sq[:], in_=pt[:, :], func=AF.Square)
                kb.mm_open([sqb, sqpe_b[t], g.cst_b], [psb_])
                nc.tensor.matmul(ps_[:, :], g.ones[:, :], sq[:], start=True, stop=False)
                ins = nc.tensor.matmul(ps_[:, :], g.ones[:64, :], sqpe[:, sl], start=False, stop=True)
                kb.n_inst += 2
                kb.mm_close(ins, [sqb, sqpe_b[t]], [psb_])
                rs, rsb = scratch(g, "rs")
                kb.op("act", [psb_], [rsb], nc.scalar.activation, out=rs[:], in_=ps_[:, :], func=AF.Sqrt,
                      scale=float(1.0 / 192), bias=float(EPS))
                kb.op("dve", [rsb], [rsb], nc.vector.reciprocal, rs[:], rs[:])
                kb.op("dve", [pb, rsb, g.par_b], [kst_b[si]], nc.vector.scalar_tensor_tensor, out=kst[si][:, sl],
                      in0=pt[:, :], scalar=pcol(g, PC_GKN_N), in1=rs[:], op0=ALU.mult, op1=ALU.mult)
                kb.op("dve", [Rk_b[t], rsb], [krst_b[si]], nc.vector.tensor_tensor, out=krst[si][:, sl],
                      in0=Rk[:, sl], in1=rs[:64, :], op=ALU.mult)
            kb.dma("sp", g.kt_own(h)[0:128, :], kst[si][:], kst_b[si], [kst_b[si]], [])
            kb.dma("sp", g.kt_own(h)[128:192, :], krst[si][:], krst_b[si], [krst_b[si]], [])
        vi = 0
        for tb in range(TOK // 128):
            t = tb // 4
            for hg in range(3):
                pt, pb = bank(g)
                kb.mm(pb, pt[:, :], [(ckv[:, c, tb * 128:(tb + 1) * 128], wbv[c][:, hg * 4:(hg + 1) * 4, 128:256])
                                      for c in range(2)], [wbb, ckv_b[t]])
                s = vi % 2
                vi += 1
                kb.op("act", [pb], [vst_b[s]], nc.scalar.copy, vst[s][:], pt[:, :])
                for hq in range(4):
                    kb.dma("sp", g.v_own(hg * 4 + hq)[tb * 128:(tb + 1) * 128, :], vst[s][:, hq * 128:(hq + 1) * 128],
                           vst_b[s], [vst_b[s]], [])
        kb.barrier()
        kb.stack = old


def exchange_kv(g):
    kb, nc = g.kb, g.nc
    E = kb.E["pool"]
    kb.barrier()
    cs = kb.new_sem("cc")
    groups = [[0, 1, 2, 3], [4, 5, 6, 7]]
    n = 0
    for h in range(12):
        for src, dst in ((g.KT_own_l[h], g.KTg_l[h]), (g.V_own_l[h], g.Vg_l[h])):
            ins = nc.gpsimd.collective_compute("AllGather", ALU.bypass, replica_groups=groups,
                                               ins=[src.opt()], outs=[dst.opt()])
            ins.then_inc(cs)
            n += 1
    g.kv_tok = (cs, n, None)


def causal_attention(g, cqn, cqn_b, mixT, mix_b):
    kb, nc = g.kb, g.nc
    sc = float(192 ** -0.5)
    with contextlib.ExitStack() as st:
        old = kb.stack
        kb.stack = st
        NKV = 2
        kn = [kb.sb(f"kn{i}", [128, SEQ], BF16) for i in range(NKV)]
        kr = [kb.sb(f"kr{i}", [64, SEQ], BF16) for i in range(NKV)]
        vv = [kb.sb(f"vv{i}", [128, 32, 128], BF16) for i in range(NKV)]
        kv_b = [kb.buf(f"kv{i}") for i in range(NKV)]
        wqh = [kb.sb(f"wqh{i}", [128, 4, 192], BF16) for i in range(2)]
        wqh_b = [kb.buf(f"wqh{i}") for i in range(2)]
        qn = [kb.sb(f"qn{i}", [128, TOK], BF16) for i in range(2)]
        qr = [kb.sb(f"qr{i}", [64, TOK], BF16) for i in range(2)]
        q_b = [[kb.buf(f"q{i}_{t}") for t in range(NTL)] for i in range(2)]
        AB = kb.sb("ABq", [64, 2, TOK], F32); AB_b = kb.buf("ABq")
        qidx = kb.sb("qidx", [128, TOK], F32); qidx_b = kb.buf("qidx")
        for b in kv_b + wqh_b + [qidx_b]:
            kb._dbufs.append(b)
        kb.dma("sp", qidx[:], g.qidx.partition_broadcast(128).rearrange("p a n -> p (a n)"), qidx_b, [], [qidx_b])
        rope_AB(g, PC_GQN_R, PC_GQN_RS, AB, AB_b)
        wqv = g.w_q_b.rearrange("(kc p) m -> p kc m", p=128)
        g.bank_set = [4, 5, 6, 7]
        si = 0
        for h in range(12):
            par = h % 2
            b = kv_b[par]
            E = kb.E["sp"]
            if getattr(g, "kv_tok", None) is not None:
                kb._wait(E, g.kv_tok)
            kb._sync(E, [], [b])
            if b.dsem is None:
                b.dsem = kb.new_sem("d_" + b.name)
            if b.dcount:
                kb._wait(E, (b.dsem, b.dcount, None))
            for i in range(8):
                r, off = min(i, 7 - i), (512 if i >= 4 else 0)
                srcs = [(kn[par][:, i * 512:(i + 1) * 512], g.kt_g(h, r)[0:128, off:off + 512]),
                        (kr[par][:, i * 512:(i + 1) * 512], g.kt_g(h, r)[128:192, off:off + 512]),
                        (vv[par][:, i * 4:(i + 1) * 4, :],
                         g.v_g(h, r)[off:off + 512, :].rearrange("(kb p) d -> p kb d", p=128))]
                for o_, i_ in srcs:
                    ins = nc.sync.dma_start(out=o_, in_=i_)
                    b.dcount += 16
                    ins.then_inc(b.dsem, 16)
                    kb.n_inst += 1
            kb._commit((b.dsem, b.dcount, None), [], [b])
            kb.dma("pool", wqh[par][:], wqv[:, :, h * 192:(h + 1) * 192], wqh_b[par], [], [wqh_b[par]])
            w = wqh[par]
            for t in range(NTL):
                sl = slice(t * TL, (t + 1) * TL)
                pn, pnb = bank(g)
                kb.mm(pnb, pn[:, :], [(w[:, kc, 0:128], cqn[:, kc, sl]) for kc in range(4)], [wqh_b[par], cqn_b[t]])
                pr, prb = bank(g)
                kb.mm(prb, pr[:64, :], [(w[:, kc, 128:192], cqn[:, kc, sl]) for kc in range(4)], [wqh_b[par], cqn_b[t]])
                pq, pqb = bank(g)
                kb.mm_open([wqh_b[par], cqn_b[t]], [pqb])
                ins = None
                for kc in range(4):
                    nc.tensor.matmul(pq[0:32, :], w[:, kc, 160:192], cqn[:, kc, sl], start=(kc == 0), stop=(kc == 3))
                    ins = nc.tensor.matmul(pq[32:64, :], w[:, kc, 128:160], cqn[:, kc, sl], start=(kc == 0),
                                           stop=(kc == 3))
                    kb.n_inst += 2
                kb.mm_close(ins, [wqh_b[par], cqn_b[t]], [pqb])
                rs, rsb = sumsq_rstd(g, [(pn[:, :], 128), (pr[:64, :], 64)], TL, 192, [pnb, prb])
                kb.op("dve", [pnb, rsb, g.par_b], [q_b[par][t]], nc.vector.scalar_tensor_tensor, out=qn[par][:, sl],
                      in0=pn[:, :], scalar=pcol(g, PC_GQN_N), in1=rs[:], op0=ALU.mult, op1=ALU.mult)
                r1, r1b = scratch(g, "fs")
                kb.op("dve", [prb, AB_b], [r1b], nc.vector.tensor_tensor, out=r1[:64, :TL], in0=pr[:64, :],
                      in1=AB[:, 0, sl], op=ALU.mult)
                r2, r2b = scratch(g, "fs")
                kb.op("dve", [pqb, AB_b], [r2b], nc.vector.tensor_tensor, out=r2[:64, :TL], in0=pq[:64, :],
                      in1=AB[:, 1, sl], op=ALU.mult)
                kb.op("dve", [r1b, r2b], [r1b], nc.vector.tensor_tensor, out=r1[:64, :TL], in0=r1[:64, :TL],
                      in1=r2[:64, :TL], op=ALU.add)
                kb.op("dve", [r1b, rsb], [q_b[par][t]], nc.vector.tensor_tensor, out=qr[par][:, sl],
                      in0=r1[:64, :TL], in1=rs[:64, :], op=ALU.mult)
            for t in range(NTL):
                sl = slice(t * TL, (t + 1) * TL)
                nkb = 16 if t == 0 else 32
                po, pob = g.pst[2], g.psb[2]
                pd, pdb = g.pst[3], g.psb[3]
                def scores(kbi):
                    nonlocal si
                    S, Sb = g.pst[si % 2], g.psb[si % 2]
                    si += 1
                    ks = slice(kbi * 128, (kbi + 1) * 128)
                    kb.mm(Sb, S[:, :], [(kn[par][:, ks], qn[par][:, sl]), (kr[par][:, ks], qr[par][:, sl])],
                          [kv_b[par], q_b[par][t]])
                    return S, Sb

                nxt = scores(0)
                for kbi in range(nkb):
                    S, Sb = nxt
                    if kbi + 1 < nkb:
                        nxt = scores(kbi + 1)
                    e, eb = scratch(g, "sq")
                    kb.op("act", [Sb], [eb], nc.scalar.activation, out=e[:], in_=S[:, :], func=AF.Exp, scale=sc)
                    if t == 0 or kbi >= 16:
                        em, emb = scratch(g, "sq")
                        kb.op("dve", [qidx_b, g.par_b, eb], [emb], nc.vector.scalar_tensor_tensor, out=em[:],
                              in0=qidx[:, sl], scalar=pcol(g, PC_KIDX + kbi), in1=e[:], op0=ALU.is_ge, op1=ALU.mult)
                        e, eb = em, emb
                    kb.mm_open([eb, kv_b[par], g.cst_b], [pob, pdb] if kbi == 0 else [])
                    nc.tensor.matmul(po[:, :], vv[par][:, kbi, :], e[:], start=(kbi == 0), stop=(kbi == nkb - 1))
                    ins = nc.tensor.matmul(pd[:, :], g.ones[:, :], e[:], start=(kbi == 0), stop=(kbi == nkb - 1))
                    kb.n_inst += 2
                    kb.mm_close(ins, [eb, kv_b[par]], [pob, pdb])
                rd, rdb = scratch(g, "rs")
                kb.op("dve", [pdb], [rdb], nc.vector.reciprocal, rd[:], pd[:, :])
                kb.op("dve", [pob, rdb], [mix_b[t]], nc.vector.tensor_tensor, out=mixT[:, h, sl], in0=po[:, :],
                      in1=rd[:], op=ALU.mult)
        g.bank_set = list(range(8))
        kb.barrier()
        kb.stack = old


def moe(g):
    kb, nc = g.kb, g.nc
    with contextlib.ExitStack() as st:
        old = kb.stack
        kb.stack = st
        alloc_ring(g, 12288)
        hT = kb.sb("hT", [128, KC, TOK], BF16); hT_b = [kb.buf("hT0"), kb.buf("hT1")]
        selsb = kb.sb("selsb", [8, NEXP * 128], F32); sel_b = kb.buf("selsb"); kb._dbufs.append(sel_b)
        wr = kb.sb("wr", [128, KC, NEXP], F32); wr_b = kb.buf("wr"); kb._dbufs.append(wr_b)
        rtok = kb.sb("rtok", [128, 8], F32); rtok_b = kb.buf("rtok")
        lg = kb.sb("lg", [128, 8, NEXP], F32); lg_b = kb.buf("lg")
        t8 = kb.sb("t8", [128, 8, 8], F32); t8_b = kb.buf("t8")
        wts = kb.sb("wts", [128, 4, 8], F32); wts_b = kb.buf("wts")
        m1 = kb.sb("m1", [128, NEXP], F32); m1_b = kb.buf("m1")
        gates = kb.sb("gates", [128, 8, NEXP], F32); gates_b = kb.buf("gates")
        gT = kb.sb("gT", [8, TOK], F32); gT_b = kb.buf("gT")
        G = [kb.sb(f"G{i}", [128, TOK], F32) for i in range(2)]
        G_b = [[kb.buf(f"G{i}_{t}") for t in range(NTL)] for i in range(2)]
        act = [kb.sb(f"act{t}", [128, 2, TL], BF16) for t in range(NTL)]
        act_b = [[kb.buf(f"act{t}_{m}") for m in range(2)] for t in range(NTL)]
        kb.dma("sp", selsb[:], g.sel, sel_b, [], [sel_b])
        kb.dma("sp", wr[:], g.w_router.rearrange("(kc p) e -> p kc e", p=128), wr_b, [], [wr_b])
        for kc in range(KC):
            kb.op("dve", [wr_b, g.par_b], [wr_b], nc.vector.tensor_scalar, out=wr[:, kc, :], in0=wr[:, kc, :],
                  scalar1=pcol(g, PC_GFFN1 + kc), scalar2=None, op0=ALU.mult)

        def on_rstd(t, rs, rsb):
            for q in range(4):
                pt, pb = bank(g)
                kb.mm_open([rsb, g.cst_b], [pb])
                ins = nc.tensor.transpose(pt[:, 0:128], rs[:, q * 128:(q + 1) * 128], g.idf[:])
                kb.n_inst += 1
                kb.mm_close(ins, [rsb], [pb])
                kb.op("dve", [pb], [rtok_b], nc.vector.tensor_copy, rtok[:, t * 4 + q:t * 4 + q + 1], pt[:, 0:1])

        norm_x(g, PC_GFFN1, hT, hT_b, on_rstd=on_rstd)
        for tb in range(8):
            t = tb // 4
            pt, pb = bank(g)
            kb.mm(pb, pt[:, 0:NEXP], [(g.x[:, kc, tb * 128:(tb + 1) * 128], wr[:, kc, :]) for kc in range(KC)],
                  [wr_b] + [g.xb[kc][t] for kc in range(KC)])
            kb.op("dve", [pb, rtok_b], [lg_b], nc.vector.tensor_scalar, out=lg[:, tb, :], in0=pt[:, 0:NEXP],
                  scalar1=rtok[:, tb:tb + 1], scalar2=None, op0=ALU.mult)
            kb.op("dve", [lg_b], [t8_b], nc.vector.max, out=t8[:, tb, :], in_=lg[:, tb, :])
        kb.op("dve", [t8_b], [wts_b], nc.vector.tensor_tensor, out=wts[:, 0, :], in0=t8[:, :, 1], in1=t8[:, :, 0],
              op=ALU.subtract)
        kb.op("act", [wts_b], [wts_b], nc.scalar.activation, out=wts[:, 1, :], in_=wts[:, 0, :], func=AF.Exp)
        kb.op("dve", [wts_b], [wts_b], nc.vector.tensor_scalar, out=wts[:, 2, :], in0=wts[:, 1, :], scalar1=1.0,
              scalar2=None, op0=ALU.add)
        kb.op("dve", [wts_b], [wts_b], nc.vector.reciprocal, wts[:, 2, :], wts[:, 2, :])
        kb.op("dve", [wts_b], [wts_b], nc.vector.tensor_tensor, out=wts[:, 3, :], in0=wts[:, 1, :], in1=wts[:, 2, :],
              op=ALU.mult)
        for tb in range(8):
            kb.op("dve", [lg_b, t8_b, wts_b], [m1_b], nc.vector.tensor_scalar, out=m1[:], in0=lg[:, tb, :],
                  scalar1=t8[:, tb, 0:1], scalar2=wts[:, 2, tb:tb + 1], op0=ALU.is_equal, op1=ALU.mult)
            kb.op("dve", [lg_b, t8_b, wts_b], [gates_b], nc.vector.tensor_scalar, out=gates[:, tb, :], in0=lg[:, tb, :],
                  scalar1=t8[:, tb, 1:2], scalar2=wts[:, 3, tb:tb + 1], op0=ALU.is_equal, op1=ALU.mult)
            kb.op("dve", [m1_b, gates_b], [gates_b], nc.vector.tensor_tensor, out=gates[:, tb, :], in0=gates[:, tb, :],
                  in1=m1[:], op=ALU.add)
        for t in range(NTL):
            pt, pb = bank(g)
            kb.mm_open([gates_b, g.cst_b], [pb])
            ins = None
            for q in range(4):
                ins = nc.tensor.transpose(pt[0:NEXP, q * 128:(q + 1) * 128], gates[:, t * 4 + q, :], g.idf[:])
                kb.n_inst += 1
            kb.mm_close(ins, [gates_b], [pb])
            kb.op("act", [pb], [gT_b], nc.scalar.copy, gT[:, t * TL:(t + 1) * TL], pt[0:NEXP, :])
        for e in range(NEXP):
            gi_ = e % 2
            for t in range(NTL):
                sl = slice(t * TL, (t + 1) * TL)
                pt, pb = bank(g)
                kb.mm(pb, pt[:, :], [(selsb[:, e * 128:(e + 1) * 128], gT[:, sl])], [sel_b, gT_b])
                kb.op("act", [pb], [G_b[gi_][t]], nc.scalar.copy, G[gi_][:, sl], pt[:, :])
            for gi in range(DFF // 256):
                wg, wu, wd, wb = ffn_weights(g, g.moe_gate[e], g.moe_up[e], g.moe_down[e], gi)
                ffn_group(g, wg, wu, wd, wb, hT, hT_b, gate_bc=G[gi_], gate_bcb=G_b[gi_], act=act, act_b=act_b)
        ffn_drain(g)
        kb.barrier()
        kb.stack = old


def layer1(g):
    kb, nc = g.kb, g.nc
    with contextlib.ExitStack() as st0:
        old0 = kb.stack
        kb.stack = st0
        kmT = kb.sb("kmT", [128, 4, 256], BF16); kmT_b = kb.buf("kmT")
        vm = kb.sb("vm", [128, 2, 512], BF16); vm_b = kb.buf("vm")
        with contextlib.ExitStack() as st:
            kb.stack = st
            alloc_ring(g, 8192)
            mem_kv(g, 1, kmT, kmT_b, vm, vm_b)
            kb.stack = st0
        mixT = kb.sb("mixT", [128, KC, TOK], BF16); mix_b = [kb.buf("mix0"), kb.buf("mix1")]
        cqn = kb.sb("cqn", [128, 4, TOK], BF16); cqn_b = [kb.buf("cqn0"), kb.buf("cqn1")]
        with contextlib.ExitStack() as st:
            kb.stack = st
            alloc_ring(g, 8192)
            hT = kb.sb("hT", [128, KC, TOK], BF16); hT_b = [kb.buf("hT0"), kb.buf("hT1")]
            norm_x(g, PC_GMIX1, hT, hT_b)
            bw = g.b_w_in.rearrange("(kc p) m -> p kc m", p=128)
            (wqa,), wqab = load_w(g, [((128, KC, 512), bw[:, :, 0:512])])
            for t in range(NTL):
                sl = slice(t * TL, (t + 1) * TL)
                pl = []
                for c in range(4):
                    pt, pb = bank(g)
                    kb.mm(pb, pt[:, :], [(wqa[:, kc, c * 128:(c + 1) * 128], hT[:, kc, sl]) for kc in range(KC)],
                          [wqab, hT_b[t]])
                    pl.append((pt, pb))
                rs, rsb = sumsq_rstd(g, [(p[0][:, :], 128) for p in pl], TL, 512, [p[1] for p in pl])
                for c in range(4):
                    kb.op("dve", [pl[c][1], rsb, g.par_b], [cqn_b[t]], nc.vector.scalar_tensor_tensor,
                          out=cqn[:, c, sl], in0=pl[c][0][:, :], scalar=pcol(g, PC_GQA + c), in1=rs[:],
                          op0=ALU.mult, op1=ALU.mult)
            (wqm,), wqmb = load_w(g, [((128, KC, 512), bw[:, :, 512:1024])])
            mem_attention(g, wqm, wqmb, hT, hT_b, PC_GMQ1, kmT, kmT_b, vm, vm_b, mixT, mix_b)
            kb.barrier()
            kb.stack = st0
        causal_attention(g, cqn, cqn_b, mixT, mix_b)
        with contextlib.ExitStack() as st:
            kb.stack = st
            alloc_ring(g, 8192)
            out_proj(g, 1, mixT, mix_b)
            kb.barrier()
            kb.stack = st0
        kb.stack = old0
    moe(g)


def _core_tokens(core):
    b, c = core // 4, core % 4
    iA, iB = c, 7 - c
    idx = np.concatenate([np.arange(512 * iA, 512 * iA + 512), np.arange(512 * iB, 512 * iB + 512)])
    return b, (iA, iB), idx


def _chunk(v):
    return np.ascontiguousarray(v.reshape(-1, 128).T)


def _params(inp, core):
    b, (iA, iB), idx = _core_tokens(core)
    P = np.zeros((128, NPC), np.float32)
    P[:, PC_GMIX0:PC_GMIX0 + 16] = _chunk(inp["g_mix"][0])
    P[:, PC_GFFN0:PC_GFFN0 + 16] = _chunk(inp["g_ffn"][0])
    P[:, PC_GKV:PC_GKV + 16] = _chunk(inp["g_kv"])
    P[:, PC_GMIX1:PC_GMIX1 + 16] = _chunk(inp["g_mix"][1])
    P[:, PC_GFFN1:PC_GFFN1 + 16] = _chunk(inp["g_ffn"][1])
    cw = inp["a_conv_w"][0]
    P[:, PC_CONV:PC_CONV + 36] = cw.reshape(3, 12, 128).transpose(2, 1, 0).reshape(128, 36)
    P[:, PC_GMQ0] = inp["g_mq"][0]
    P[:, PC_GMK0] = inp["g_mk"][0]
    P[:, PC_GMQ1] = inp["g_mq"][1]
    P[:, PC_GMK1] = inp["g_mk"][1]
    P[:, PC_GQA:PC_GQA + 4] = _chunk(inp["b_g_q_a"][0])
    P[:, PC_GKVA:PC_GKVA + 2] = _chunk(inp["g_kv_a"])
    gq, gk = inp["b_g_qn"][0], inp["g_kn"]
    P[:, PC_GQN_N] = gq[:128]
    P[:, PC_GKN_N] = gk[:128]
    P[:64, PC_GQN_R] = gq[128:]
    P[:64, PC_GQN_RS] = np.concatenate([gq[160:], gq[128:160]])
    P[:64, PC_GKN_R] = gk[128:]
    P[:64, PC_GKN_RS] = np.concatenate([gk[160:], gk[128:160]])
    P[:32, PC_SGN] = -1.0
    P[32:64, PC_SGN] = 1.0
    invf = (1.0 / (10000.0 ** (np.arange(0, 64, 2, dtype=np.float32) / np.float32(64)))).astype(np.float32)
    P[:64, PC_INVF] = np.concatenate([invf, invf])
    P[:, PC_HALO] = 0.0 if iA == 0 else 1.0
    P[:, PC_HALO + 1] = 1.0
    P[:, PC_KIDX:PC_KIDX + 32] = (np.arange(32)[None, :] * 128 + np.arange(128)[:, None]).astype(np.float32)
    return P


def _consts():
    ident = np.eye(128, dtype=np.float32)
    sel = np.zeros((8, NEXP * 128), np.float32)
    for e in range(NEXP):
        sel[e, e * 128:(e + 1) * 128] = 1.0
    return ident, sel


def host_inputs(inp, core, mode):
    b, (iA, iB), idx = _core_tokens(core)
    ident, sel = _consts()
    m = {
        "params": _params(inp, core), "ident": ident, "sel": sel,
        "mem": np.ascontiguousarray(inp["mem"][b]), "g_mem": inp["g_mem"],
        "w_mem_kv": inp["w_mem_kv"], "w_out": inp["w_out"],
        "pos": np.ascontiguousarray(inp["positions"][b, idx][None, :]).astype(np.int32),
    }
    if mode in ("L0", "fused"):
        m["xT"] = np.ascontiguousarray(inp["x"][b, idx, :].T)
        xh = np.zeros((4, D), np.float32)
        for t, i in enumerate((iA, iB)):
            if i > 0:
                xh[2 * t:2 * t + 2] = inp["x"][b, 512 * i - 2:512 * i, :]
        m["xhT"] = np.ascontiguousarray(xh.T)
        m["a_w_in"] = inp["a_w_in"][0]
        m["w_kv_a"] = inp["w_kv_a"]
        m["w_kv_b"] = inp["w_kv_b"]
        m["ffn_gate"] = inp["ffn_w_gate"][0]
        m["ffn_up"] = inp["ffn_w_up"][0]
        m["ffn_down"] = inp["ffn_w_down"][0]
    if mode in ("L1", "fused"):
        m["qidx"] = idx[None, :].astype(np.float32)
        m["b_w_in"] = inp["b_w_in"][0]
        m["w_q_b"] = inp["b_w_q_b"][0]
        m["w_router"] = inp["moe_w_router"][0]
        m["moe_gate"] = inp["moe_w_gate"][0]
        m["moe_up"] = inp["moe_w_up"][0]
        m["moe_down"] = inp["moe_w_down"][0]
    return m


_PROGS = {}


def _prog(mode):
    if mode not in _PROGS:
        _PROGS[mode] = build(mode)[0]
    return _PROGS[mode]


FUSED = True


def kernel(**inputs):
    inp = {k: np.asarray(v) for k, v in inputs.items()}
    cores = list(range(NCORE))
    if FUSED:
        maps = [host_inputs(inp, c, "fused") for c in cores]
        res = run_bass_kernel_spmd(_prog("fused"), maps, core_ids=cores).results
    else:
        maps0 = [host_inputs(inp, c, "L0") for c in cores]
        r0 = run_bass_kernel_spmd(_prog("L0"), maps0, core_ids=cores).results
        maps1 = []
        for c in cores:
            b = c // 4
            m = host_inputs(inp, c, "L1")
            m["xT"] = np.asarray(r0[c]["x1T"])
            m["KTg"] = np.concatenate([np.asarray(r0[4 * b + r]["KT_own"]) for r in range(4)], axis=0)
            m["Vg"] = np.concatenate([np.asarray(r0[4 * b + r]["V_own"]) for r in range(4)], axis=0)
            maps1.append(m)
        res = run_bass_kernel_spmd(_prog("L1"), maps1, core_ids=cores).results
    out = np.zeros((NB, SEQ, D), np.float32)
    for c in cores:
        b, _, idx = _core_tokens(c)
        out[b, idx, :] = np.asarray(res[c]["yT"]).T
    return out
```

```python
import contextlib
import numpy as np
import concourse.bass as bass
import concourse.mybir as mybir
from concourse.bass_utils import run_bass_kernel_spmd

F32 = mybir.dt.float32
BF16 = mybir.dt.bfloat16
I32 = mybir.dt.int32
AF = mybir.ActivationFunctionType
ALU = mybir.AluOpType


class Buf:
    __slots__ = ("name", "writer", "readers", "dsem", "dcount")

    def __init__(self, name):
        self.name = name
        self.writer = None
        self.readers = []
        self.dsem = None
        self.dcount = 0


class Eng:
    def __init__(self, name, h, sem):
        self.name = name
        self.h = h
        self.sem = sem
        self.cnt = 0
        self.known = {}


class KB:
    def __init__(self, nc, stack):
        self.nc = nc
        self.stack = stack
        self.gstack = stack
        self.nsem = 0
        self.E = {}
        for name, h in (("pe", nc.tensor), ("act", nc.scalar), ("dve", nc.vector),
                        ("pool", nc.gpsimd), ("sp", nc.sync)):
            self.E[name] = Eng(name, h, self.new_sem("e_" + name))
        self.nbuf = 0
        self.n_inst = 0

    def new_sem(self, name):
        self.nsem += 1
        return self.gstack.enter_context(self.nc.semaphore(f"{name}_{self.nsem}"))

    def buf(self, name=None):
        self.nbuf += 1
        return Buf(name or f"b{self.nbuf}")

    def sb(self, name, shape, dt):
        self.nbuf += 1
        return self.stack.enter_context(self.nc.sbuf_tensor(f"{name}_{self.nbuf}", list(shape), dt))

    def ps(self, name, shape, dt=F32):
        return self.stack.enter_context(self.nc.psum_tensor(name, list(shape), dt))

    def _wait(self, E, tok):
        sem, val, src = tok
        if src is E and E.name == "pe":
            return
        key = id(sem)
        if E.known.get(key, 0) >= val:
            return
        E.h.wait_ge(sem, val)
        E.known[key] = val

    def _sync(self, E, reads, writes):
        for b in reads:
            if b.writer is not None:
                self._wait(E, b.writer)
        for b in writes:
            if b.writer is not None and b.writer[2] is not E:
                self._wait(E, b.writer)
            for r in b.readers:
                if r[2] is not E:
                    self._wait(E, r)

    def _commit(self, tok, reads, writes):
        for b in reads:
            b.readers.append(tok)
            if len(b.readers) > 64:
                best = {}
                for t in b.readers:
                    k = id(t[0])
                    if k not in best or best[k][1] < t[1]:
                        best[k] = t
                b.readers = list(best.values())
        for b in writes:
            b.writer = tok
            b.readers = []

    def op(self, eng, reads, writes, fn, *a, **kw):
        E = self.E[eng]
        self._sync(E, reads, writes)
        ins = fn(*a, **kw)
        E.cnt += 1
        ins.then_inc(E.sem, 1)
        self.n_inst += 1
        self._commit((E.sem, E.cnt, E), reads, writes)
        return ins

    def mm(self, out_buf, out_ap, pairs, reads, transpose=False):
        E = self.E["pe"]
        self._sync(E, reads, [out_buf])
        n = len(pairs)
        ins = None
        for i, (l, r) in enumerate(pairs):
            ins = self.nc.tensor.matmul(out_ap, l, r, start=(i == 0), stop=(i == n - 1))
            self.n_inst += 1
        E.cnt += 1
        ins.then_inc(E.sem, 1)
        self._commit((E.sem, E.cnt, E), reads, [out_buf])

    def mm_open(self, reads, writes):
        E = self.E["pe"]
        self._sync(E, reads, writes)

    def mm_close(self, ins, reads, writes):
        E = self.E["pe"]
        E.cnt += 1
        ins.then_inc(E.sem, 1)
        self._commit((E.sem, E.cnt, E), reads, writes)

    def dma(self, q, out_ap, in_ap, sbuf_buf, reads, writes, **kw):
        E = self.E[q]
        b = sbuf_buf
        if b.dsem is None:
            b.dsem = self.new_sem("d_" + b.name)
        self._sync(E, reads, writes)
        if b.dcount:
            self._wait(E, (b.dsem, b.dcount, None))
        ins = E.h.dma_start(out=out_ap, in_=in_ap, **kw)
        b.dcount += 16
        ins.then_inc(b.dsem, 16)
        self.n_inst += 1
        tok = (b.dsem, b.dcount, None)
        self._commit(tok, reads, writes)
        return tok

    def wait_tok(self, eng, tok):
        self._wait(self.E[eng], tok)


    def barrier(self):
        toks = [(E.sem, E.cnt, E) for E in self.E.values() if E.cnt > 0]
        toks += [(b.dsem, b.dcount, None) for b in self._dbufs if b.dcount > 0]
        for E in self.E.values():
            for t in toks:
                if t[2] is E:
                    continue
                self._wait(E, t)


D = 2048
SEQ = 4096
NB = 2
NCORE = 8
TOK = 1024
TL = 512
NTL = 2
KC = 16
DFF = 7168
NEXP = 8
EPS = 1e-6
NPC = 168

PC_GMIX0, PC_GFFN0, PC_GKV, PC_GMIX1, PC_GFFN1 = 0, 16, 32, 48, 64
PC_CONV = 80
PC_GMQ0, PC_GMK0, PC_GMQ1, PC_GMK1 = 116, 117, 118, 119
PC_GQA = 120
PC_GKVA = 124
PC_GQN_N, PC_GKN_N = 126, 127
PC_GQN_R, PC_GQN_RS, PC_GKN_R, PC_GKN_RS, PC_SGN, PC_INVF = 128, 129, 130, 131, 132, 133
PC_HALO = 134
PC_KIDX = 136


class Ctx:
    pass


def build(mode):
    nc = bass.Bass("TRN2", target_bir_lowering=False)
    do0 = mode in ("L0", "fused")
    do1 = mode in ("L1", "fused")

    def din(name, shape, dt=F32):
        return nc.dram_tensor(name, list(shape), dt, kind="ExternalInput").ap()

    def dout(name, shape, dt=F32):
        return nc.dram_tensor(name, list(shape), dt, kind="ExternalOutput").ap()

    def dint(name, shape, dt=F32):
        return nc.dram_tensor(name, list(shape), dt, kind="Internal").ap()

    g = Ctx()
    g.nc = nc
    g.xT = din("xT", [D, TOK])
    g.params = din("params", [128, NPC])
    g.ident = din("ident", [128, 128])
    g.sel = din("sel", [8, NEXP * 128])
    g.mem = din("mem", [256, D])
    g.g_mem = din("g_mem", [2, D])
    g.w_mem_kv = din("w_mem_kv", [2, D, 1024])
    g.w_out = din("w_out", [2, D, D])
    if do0:
        g.xhT = din("xhT", [D, 4])
        g.pos = din("pos", [1, TOK], I32)
        g.a_w_in = din("a_w_in", [D, 5120])
        g.w_kv_a = din("w_kv_a", [D, 320])
        g.w_kv_b = din("w_kv_b", [256, 3072])
        g.ffn_gate = din("ffn_gate", [D, DFF])
        g.ffn_up = din("ffn_up", [D, DFF])
        g.ffn_down = din("ffn_down", [DFF, D])
    if do1:
        if not do0:
            g.pos = din("pos", [1, TOK], I32)
        g.qidx = din("qidx", [1, TOK])
        g.b_w_in = din("b_w_in", [D, 1024])
        g.w_q_b = din("w_q_b", [512, 2304])
        g.w_router = din("w_router", [D, NEXP])
        g.moe_gate = din("moe_gate", [NEXP, D, DFF])
        g.moe_up = din("moe_up", [NEXP, D, DFF])
        g.moe_down = din("moe_down", [NEXP, DFF, D])
        g.yT = dout("yT", [D, TOK])
    if mode == "L0":
        g.x1T = dout("x1T", [D, TOK])
        KT_own = dout("KT_own", [2304, TOK], BF16)
        V_own = dout("V_own", [TOK, 1536], BF16)
        g.kt_own = lambda h: KT_own[h * 192:(h + 1) * 192, :]
        g.v_own = lambda h: V_own[:, h * 128:(h + 1) * 128]
    elif mode == "L1":
        KTg = din("KTg", [4 * 2304, TOK], BF16)
        Vg = din("Vg", [4 * TOK, 1536], BF16)
        g.kt_g = lambda h, r: KTg[r * 2304 + h * 192:r * 2304 + (h + 1) * 192, :]
        g.v_g = lambda h, r: Vg[r * TOK:(r + 1) * TOK, h * 128:(h + 1) * 128]
    else:
        g.KT_own_l = [dint(f"KT_own{h}", [192, TOK], BF16) for h in range(12)]
        g.V_own_l = [dint(f"V_own{h}", [TOK, 128], BF16) for h in range(12)]
        g.KTg_l = [dint(f"KTg{h}", [4 * 192, TOK], BF16) for h in range(12)]
        g.Vg_l = [dint(f"Vg{h}", [4 * TOK, 128], BF16) for h in range(12)]
        g.kt_own = lambda h: g.KT_own_l[h]
        g.v_own = lambda h: g.V_own_l[h]
        g.kt_g = lambda h, r: g.KTg_l[h][r * 192:(r + 1) * 192, :]
        g.v_g = lambda h, r: g.Vg_l[h][r * TOK:(r + 1) * TOK, :]

    with contextlib.ExitStack() as st:
        kb = KB(nc, st)
        kb._dbufs = []
        g.kb = kb
        setup_globals(g)
        if do0:
            layer0(g)
            shared_kv(g)
            if mode == "L0":
                store_x(g, g.x1T)
        if mode == "fused":
            exchange_kv(g)
        if do1:
            layer1(g)
            store_x(g, g.yT)
        kb.barrier()
        g.n_inst = kb.n_inst
    return nc, g


def setup_globals(g):
    kb, nc = g.kb, g.nc
    g.x = kb.sb("x", [128, KC, TOK], F32)
    g.xb = [[kb.buf(f"x{kc}_{t}") for t in range(NTL)] for kc in range(KC)]
    g.par = kb.sb("par", [128, NPC], F32)
    g.par_b = kb.buf("par")
    g.idf = kb.sb("idf", [128, 128], F32)
    g.idb = kb.sb("idb", [128, 128], BF16)
    g.ones = kb.sb("ones", [128, 128], BF16)
    g.cst_b = kb.buf("cst")
    g.cs = kb.sb("cs", [64, 2, TOK], F32)
    g.cs_b = kb.buf("cs")
    g.pst = [kb.ps(f"ps{i}", [128, 512]) for i in range(8)]
    g.psb = [kb.buf(f"ps{i}") for i in range(8)]
    g.bank_i = 0
    g.bank_set = list(range(8))
    g.ffn_pending = []
    g.sq = [kb.sb(f"sq{i}", [128, 512], BF16) for i in range(6)]
    g.sq_b = [kb.buf(f"sq{i}") for i in range(6)]
    g.sq_i = 0
    g.rs = [kb.sb(f"rs{i}", [128, 512], F32) for i in range(3)]
    g.rs_b = [kb.buf(f"rs{i}") for i in range(3)]
    g.rs_i = 0
    g.fs = [kb.sb(f"fs{i}", [128, 514], F32) for i in range(3)]
    g.fs_b = [kb.buf(f"fs{i}") for i in range(3)]
    g.fs_i = 0

    xb_all = [b for row in g.xb for b in row]
    xv = g.xT.rearrange("(kc p) n -> p kc n", p=128)
    ldb = kb.buf("xload"); kb._dbufs.append(ldb)
    for t in range(NTL):
        kb.dma("sp", g.x[:, :, t * TL:(t + 1) * TL], xv[:, :, t * TL:(t + 1) * TL], ldb, [],
               [g.xb[kc][t] for kc in range(KC)])
    cb = kb.buf("cload"); kb._dbufs.append(cb)
    kb.dma("sp", g.par[:], g.params, cb, [], [g.par_b])
    kb.dma("sp", g.idf[:], g.ident, cb, [], [g.cst_b])
    kb.op("dve", [g.cst_b], [g.cst_b], nc.vector.tensor_copy, g.idb[:], g.idf[:])
    kb.op("dve", [], [g.cst_b], nc.vector.memset, g.ones[:], 1.0)
    rope_tables(g)


def bank(g):
    bs = g.bank_set
    for _ in range(len(bs)):
        i = bs[g.bank_i % len(bs)]
        g.bank_i += 1
        b = g.psb[i]
        if b.writer is None or b.readers:
            return g.pst[i], b
    raise RuntimeError("no free PSUM bank")


def alloc_ring(g, slot_el, nslot=2):
    kb = g.kb
    g.NSLOT = nslot
    g.SLOT = slot_el
    g.ring_gen = getattr(g, "ring_gen", 0) + 1
    g.wslot = [kb.sb(f"wslot{g.ring_gen}_{i}", [128, slot_el], BF16) for i in range(nslot)]
    g.wslot_b = [kb.buf(f"wslot{g.ring_gen}_{i}") for i in range(nslot)]
    for b in g.wslot_b:
        kb._dbufs.append(b)
    g.slot_i = 0


def scratch(g, kind):
    lst, bl, key = {"sq": (g.sq, g.sq_b, "sq_i"), "rs": (g.rs, g.rs_b, "rs_i"),
                    "fs": (g.fs, g.fs_b, "fs_i")}[kind]
    i = getattr(g, key)
    setattr(g, key, (i + 1) % len(lst))
    return lst[i], bl[i]


def load_w(g, pieces):
    kb = g.kb
    i = g.slot_i
    g.slot_i = (i + 1) % g.NSLOT
    t, b = g.wslot[i], g.wslot_b[i]
    views = []
    off = 0
    E = kb.E["pool"]
    kb._sync(E, [], [b])
    if b.dsem is None:
        b.dsem = kb.new_sem("d_" + b.name)
    if b.dcount:
        kb._wait(E, (b.dsem, b.dcount, None))
    for shape, src in pieces:
        n = int(np.prod(shape[1:]))
        v = t[:shape[0], off:off + n]
        if len(shape) == 3:
            v = v.rearrange("p (a b) -> p a b", a=shape[1])
        elif len(shape) == 4:
            v = v.rearrange("p (a b c) -> p a b c", a=shape[1], b=shape[2])
        ins = g.nc.gpsimd.dma_start(out=v, in_=src, max_dma_last_dim=4096)
        b.dcount += 16
        ins.then_inc(b.dsem, 16)
        kb.n_inst += 1
        views.append(v)
        off += n
    assert off <= g.SLOT
    kb._commit((b.dsem, b.dcount, None), [], [b])
    return views, b


def pcol(g, c, P=128):
    return g.par[:P, c:c + 1]


def rope_tables(g):
    kb, nc = g.kb, g.nc
    with contextlib.ExitStack() as st:
        old = kb.stack
        kb.stack = st
        posi = kb.sb("posi", [64, TOK], I32); pb = kb.buf("posi"); kb._dbufs.append(pb)
        ang = kb.sb("ang", [64, TOK], F32); ab = kb.buf("ang")
        t1 = kb.sb("rt1", [64, TOK], F32); t1b = kb.buf("rt1")
        ki = kb.sb("rki", [64, TOK], I32); kib = kb.buf("rki")
        kf = kb.sb("rkf", [64, TOK], F32); kfb = kb.buf("rkf")
        m = kb.sb("rm", [64, TOK], F32); mb = kb.buf("rm")
        kb.dma("sp", posi[:], g.pos.partition_broadcast(64).rearrange("p a n -> p (a n)"), pb, [], [pb])
        kb.op("dve", [pb], [ab], nc.vector.tensor_copy, ang[:], posi[:])
        kb.op("dve", [ab, g.par_b], [ab], nc.vector.tensor_scalar, out=ang[:], in0=ang[:],
              scalar1=pcol(g, PC_INVF, 64), scalar2=None, op0=ALU.mult)
        TWO_PI = 2.0 * np.pi
        for which, shift in ((1, 0.0), (0, np.pi / 2)):
            kb.op("dve", [ab], [t1b], nc.vector.tensor_scalar, out=t1[:], in0=ang[:],
                  scalar1=float(shift), scalar2=float(1.0 / TWO_PI), op0=ALU.add, op1=ALU.mult)
            kb.op("dve", [t1b], [kib], nc.vector.tensor_copy, ki[:], t1[:])
            kb.op("dve", [kib], [kfb], nc.vector.tensor_copy, kf[:], ki[:])
            kb.op("dve", [kfb, ab], [t1b], nc.vector.scalar_tensor_tensor, out=t1[:], in0=kf[:],
                  scalar=float(-TWO_PI), in1=ang[:], op0=ALU.mult, op1=ALU.add)
            if shift:
                kb.op("dve", [t1b], [t1b], nc.vector.tensor_scalar, out=t1[:], in0=t1[:],
                      scalar1=float(shift), scalar2=None, op0=ALU.add)
            kb.op("dve", [t1b], [mb], nc.vector.tensor_scalar, out=m[:], in0=t1[:],
                  scalar1=float(np.pi), scalar2=float(-TWO_PI), op0=ALU.is_gt, op1=ALU.mult)
            kb.op("dve", [mb, t1b], [t1b], nc.vector.tensor_tensor, out=t1[:], in0=t1[:], in1=m[:], op=ALU.add)
            kb.op("dve", [t1b], [mb], nc.vector.tensor_scalar, out=m[:], in0=t1[:],
                  scalar1=float(-np.pi), scalar2=float(TWO_PI), op0=ALU.is_lt, op1=ALU.mult)
            kb.op("dve", [mb, t1b], [t1b], nc.vector.tensor_tensor, out=t1[:], in0=t1[:], in1=m[:], op=ALU.add)
            kb.op("dve", [t1b], [t1b], nc.vector.tensor_scalar, out=t1[:], in0=t1[:],
                  scalar1=float(np.pi), scalar2=float(-np.pi), op0=ALU.min, op1=ALU.max)
            kb.op("act", [t1b], [g.cs_b], nc.scalar.activation, out=g.cs[:, which, :], in_=t1[:], func=AF.Sin)
        kb.op("dve", [g.cs_b, g.par_b], [g.cs_b], nc.vector.tensor_scalar, out=g.cs[:, 1, :], in0=g.cs[:, 1, :],
              scalar1=pcol(g, PC_SGN, 64), scalar2=None, op0=ALU.mult)
        kb.barrier()
        kb.stack = old


def sumsq_rstd(g, srcs, N, Dn, reads):
    kb, nc = g.kb, g.nc
    pt, pb = bank(g)
    n = len(srcs)
    for i, (ap, P) in enumerate(srcs):
        sq, sqb = scratch(g, "sq")
        kb.op("act", reads, [sqb], nc.scalar.activation, out=sq[:P, :N], in_=ap, func=AF.Square)
        kb.mm_open([sqb, g.cst_b], [pb] if i == 0 else [])
        ins = nc.tensor.matmul(pt[:, :N], g.ones[:P, :], sq[:P, :N], start=(i == 0), stop=(i == n - 1))
        kb.n_inst += 1
        kb.mm_close(ins, [sqb], [pb])
    rs, rsb = scratch(g, "rs")
    kb.op("act", [pb], [rsb], nc.scalar.activation, out=rs[:, :N], in_=pt[:, :N], func=AF.Sqrt,
          scale=float(1.0 / Dn), bias=float(EPS))
    kb.op("dve", [rsb], [rsb], nc.vector.reciprocal, rs[:, :N], rs[:, :N])
    return rs, rsb


def norm_x(g, gcol0, hT, hT_b, tiles=(0, 1), on_rstd=None):
    kb, nc = g.kb, g.nc
    for t in tiles:
        sl = slice(t * TL, (t + 1) * TL)
        rs, rsb = sumsq_rstd(g, [(g.x[:, kc, sl], 128) for kc in range(KC)], TL, D,
                             [g.xb[kc][t] for kc in range(KC)])
        if on_rstd is not None:
            on_rstd(t, rs, rsb)
        for kc in range(KC):
            kb.op("dve", [g.xb[kc][t], rsb, g.par_b], [hT_b[t]], nc.vector.scalar_tensor_tensor,
                  out=hT[:, kc, sl], in0=g.x[:, kc, sl], scalar=pcol(g, gcol0 + kc), in1=rs[:, :TL],
                  op0=ALU.mult, op1=ALU.mult)


def store_x(g, dst):
    kb = g.kb
    dv = dst.rearrange("(kc p) n -> p kc n", p=128)
    sb = kb.buf("xstore"); kb._dbufs.append(sb)
    for t in range(NTL):
        kb.dma("sp", dv[:, :, t * TL:(t + 1) * TL], g.x[:, :, t * TL:(t + 1) * TL], sb,
               [g.xb[kc][t] for kc in range(KC)], [])


def mem_kv(g, l, kmT, kmT_b, vm, vm_b):
    kb, nc = g.kb, g.nc
    with contextlib.ExitStack() as st:
        old = kb.stack
        kb.stack = st
        mt = kb.sb("mem_t", [128, 2, D], F32); mtb = kb.buf("mem_t"); kb._dbufs.append(mtb)
        gb = kb.sb("gmem_bc", [128, D], F32); gbb = kb.buf("gmem_bc"); kb._dbufs.append(gbb)
        mn = kb.sb("mem_n", [128, 2, D], BF16); mnb = kb.buf("mem_n")
        mnT = kb.sb("mem_nT", [128, KC, 256], BF16); mnTb = kb.buf("mem_nT")
        ssq = kb.sb("mem_ss", [128, 2], F32); ssb = kb.buf("mem_ss")
        kb.dma("sp", mt[:], g.mem.rearrange("(mb p) d -> p mb d", p=128), mtb, [], [mtb])
        kb.dma("sp", gb[:], g.g_mem[l:l + 1, :].partition_broadcast(128).rearrange("p a n -> p (a n)"),
               gbb, [], [gbb])
        for mb in range(2):
            kb.op("act", [mtb], [mnb, ssb], nc.scalar.activation, out=mn[:, mb, :], in_=mt[:, mb, :],
                  func=AF.Square, accum_out=ssq[:, mb:mb + 1])
        kb.op("act", [ssb], [ssb], nc.scalar.activation, out=ssq[:], in_=ssq[:], func=AF.Sqrt,
              scale=float(1.0 / D), bias=float(EPS))
        kb.op("dve", [ssb], [ssb], nc.vector.reciprocal, ssq[:], ssq[:])
        for mb in range(2):
            kb.op("dve", [mtb, ssb, gbb], [mnb], nc.vector.scalar_tensor_tensor, out=mn[:, mb, :],
                  in0=mt[:, mb, :], scalar=ssq[:, mb:mb + 1], in1=gb[:], op0=ALU.mult, op1=ALU.mult)
        for mb in range(2):
            for k4 in range(4):
                pt, pb = bank(g)
                ptb = pt[:].bitcast(BF16)
                kb.mm_open([mnb, g.cst_b], [pb])
                ins = None
                for q in range(4):
                    kc = k4 * 4 + q
                    ins = nc.tensor.transpose(ptb[:, q * 128:(q + 1) * 128], mn[:, mb, kc * 128:(kc + 1) * 128],
                                              g.idb[:])
                    kb.n_inst += 1
                kb.mm_close(ins, [mnb], [pb])
                kb.op("dve", [pb], [mnTb], nc.vector.tensor_copy,
                      mnT[:, k4 * 4:(k4 + 1) * 4, mb * 128:(mb + 1) * 128],
                      ptb[:, 0:512].rearrange("p (q n) -> p q n", q=4))
        wv = g.w_mem_kv[l].rearrange("(kc p) m -> p kc m", p=128)
        (wk,), wkb = load_w(g, [((128, KC, 512), wv[:, :, 0:512])])
        (wvv,), wvb = load_w(g, [((128, KC, 512), wv[:, :, 512:1024])])
        gk = PC_GMK0 if l == 0 else PC_GMK1
        for hh in range(4):
            pt, pb = bank(g)
            kb.mm(pb, pt[:, :256], [(wk[:, kc, hh * 128:(hh + 1) * 128], mnT[:, kc, :]) for kc in range(KC)],
                  [wkb, mnTb])
            rs, rsb = sumsq_rstd(g, [(pt[:, :256], 128)], 256, 128, [pb])
            kb.op("dve", [pb, rsb, g.par_b], [kmT_b], nc.vector.scalar_tensor_tensor, out=kmT[:, hh, :],
                  in0=pt[:, :256], scalar=pcol(g, gk), in1=rs[:, :256], op0=ALU.mult, op1=ALU.mult)
        for mb in range(2):
            pt, pb = bank(g)
            kb.mm(pb, pt[:, :], [(mnT[:, kc, mb * 128:(mb + 1) * 128], wvv[:, kc, :]) for kc in range(KC)],
                  [wvb, mnTb])
            kb.op("act", [pb], [vm_b], nc.scalar.copy, vm[:, mb, :], pt[:, :])
        kb.barrier()
        kb.stack = old


def mem_attention(g, wq, wqb, hT, hT_b, gq_col, kmT, kmT_b, vm, vm_b, mixT, mix_b):
    kb, nc = g.kb, g.nc
    sc = float(128 ** -0.5)
    qm = [kb.sb(f"qm{i}", [128, TL], BF16) for i in range(2)]
    qm_b = [kb.buf(f"qm{i}") for i in range(2)]
    qi = 0
    for t in range(NTL):
        sl = slice(t * TL, (t + 1) * TL)
        for hh in range(4):
            pt, pb = bank(g)
            kb.mm(pb, pt[:, :], [(wq[:, kc, hh * 128:(hh + 1) * 128], hT[:, kc, sl]) for kc in range(KC)],
                  [wqb, hT_b[t]])
            rs, rsb = sumsq_rstd(g, [(pt[:, :], 128)], TL, 128, [pb])
            q, qb = qm[qi % 2], qm_b[qi % 2]
            qi += 1
            kb.op("dve", [pb, rsb, g.par_b], [qb], nc.vector.scalar_tensor_tensor, out=q[:],
                  in0=pt[:, :], scalar=pcol(g, gq_col), in1=rs[:], op0=ALU.mult, op1=ALU.mult)
            po, pob = bank(g)
            pd, pdb = bank(g)
            for mb in range(2):
                ps_, psb_ = bank(g)
                kb.mm(psb_, ps_[:, :], [(kmT[:, hh, mb * 128:(mb + 1) * 128], q[:])], [kmT_b, qb])
                e, eb = scratch(g, "sq")
                kb.op("act", [psb_], [eb], nc.scalar.activation, out=e[:], in_=ps_[:, :], func=AF.Exp, scale=sc)
                kb.mm_open([eb, vm_b, g.cst_b], [pob, pdb] if mb == 0 else [])
                nc.tensor.matmul(po[:, :], vm[:, mb, hh * 128:(hh + 1) * 128], e[:], start=(mb == 0), stop=(mb == 1))
                ins = nc.tensor.matmul(pd[:, :], g.ones[:, :], e[:], start=(mb == 0), stop=(mb == 1))
                kb.n_inst += 2
                kb.mm_close(ins, [eb, vm_b], [pob, pdb])
            rd, rdb = scratch(g, "rs")
            kb.op("dve", [pdb], [rdb], nc.vector.reciprocal, rd[:], pd[:, :])
            kb.op("dve", [pob, rdb], [mix_b[t]], nc.vector.tensor_tensor, out=mixT[:, 12 + hh, sl], in0=po[:, :],
                  in1=rd[:], op=ALU.mult)


def out_proj(g, l, mixT, mix_b):
    kb, nc = g.kb, g.nc
    wv = g.w_out[l].rearrange("(kc p) m -> p kc m", p=128)
    for pc in range(4):
        (w,), wb = load_w(g, [((128, KC, 512), wv[:, :, pc * 512:(pc + 1) * 512])])
        for t in range(NTL):
            sl = slice(t * TL, (t + 1) * TL)
            for dq in range(4):
                dc = pc * 4 + dq
                pt, pb = bank(g)
                kb.mm(pb, pt[:, :], [(w[:, kc, dq * 128:(dq + 1) * 128], mixT[:, kc, sl]) for kc in range(KC)],
                      [wb, mix_b[t]])
                kb.op("dve", [pb, g.xb[dc][t]], [g.xb[dc][t]], nc.vector.tensor_tensor, out=g.x[:, dc, sl],
                      in0=g.x[:, dc, sl], in1=pt[:, :], op=ALU.add)


def ffn_drain(g, n=None):
    pend = g.ffn_pending
    k = len(pend) if n is None else min(n, len(pend))
    for _ in range(k):
        pend.pop(0)()


def ffn_group(g, wg, wu, wd, wb, hT, hT_b, gate_bc=None, gate_bcb=None, act=None, act_b=None):
    kb, nc = g.kb, g.nc
    for t in range(NTL):
        sl = slice(t * TL, (t + 1) * TL)
        for m in range(2):
            pg, pgb = bank(g)
            kb.mm(pgb, pg[:, :], [(wg[:, kc, m * 128:(m + 1) * 128], hT[:, kc, sl]) for kc in range(KC)],
                  [wb, hT_b[t]])
            sg, sgb = scratch(g, "sq")
            kb.op("act", [pgb], [sgb], nc.scalar.activation, out=sg[:], in_=pg[:, :], func=AF.Silu)
            ffn_drain(g, 4)
            pu, pub = bank(g)
            kb.mm(pub, pu[:, :], [(wu[:, kc, m * 128:(m + 1) * 128], hT[:, kc, sl]) for kc in range(KC)],
                  [wb, hT_b[t]])
            if gate_bc is None:
                kb.op("dve", [pub, sgb], [act_b[t][m]], nc.vector.tensor_tensor, out=act[t][:, m, :], in0=pu[:, :],
                      in1=sg[:], op=ALU.mult)
            else:
                ug, ugb = scratch(g, "fs")
                kb.op("dve", [pub, gate_bcb[t]], [ugb], nc.vector.tensor_tensor, out=ug[:, :TL], in0=pu[:, :],
                      in1=gate_bc[:, sl], op=ALU.mult)
                kb.op("dve", [ugb, sgb], [act_b[t][m]], nc.vector.tensor_tensor, out=act[t][:, m, :],
                      in0=ug[:, :TL], in1=sg[:], op=ALU.mult)
            ffn_drain(g, 4)
        ffn_drain(g)

        def down(dc, t=t, sl=sl, wd=wd, wb=wb):
            pt, pb = bank(g)
            kb.mm(pb, pt[:, :], [(wd[m][:, dc * 128:(dc + 1) * 128], act[t][:, m, :]) for m in range(2)],
                  [wb, act_b[t][0], act_b[t][1]])
            kb.op("dve", [pb, g.xb[dc][t]], [g.xb[dc][t]], nc.vector.tensor_tensor, out=g.x[:, dc, sl],
                  in0=g.x[:, dc, sl], in1=pt[:, :], op=ALU.add)

        for dc in range(KC):
            g.ffn_pending.append(lambda dc=dc, f=down: f(dc))


def ffn_weights(g, wgate, wup, wdown, gi):
    c0 = gi * 256
    gv = wgate.rearrange("(kc p) m -> p kc m", p=128)[:, :, c0:c0 + 256]
    uv = wup.rearrange("(kc p) m -> p kc m", p=128)[:, :, c0:c0 + 256]
    dv = wdown[c0:c0 + 256, :].rearrange("(m p) d -> p m d", p=128)
    (wg, wu, wd0, wd1), wb = load_w(g, [((128, KC, 256), gv), ((128, KC, 256), uv), ((128, D), dv[:, 0, :]),
                                          ((128, D), dv[:, 1, :])])
    return wg, wu, (wd0, wd1), wb


def layer0(g):
    kb, nc = g.kb, g.nc
    with contextlib.ExitStack() as st:
        old = kb.stack
        kb.stack = st
        alloc_ring(g, 8192)
        kmT = kb.sb("kmT", [128, 4, 256], BF16); kmT_b = kb.buf("kmT")
        vm = kb.sb("vm", [128, 2, 512], BF16); vm_b = kb.buf("vm")
        mem_kv(g, 0, kmT, kmT_b, vm, vm_b)
        hT = kb.sb("hT", [128, KC, TOK], BF16); hT_b = [kb.buf("hT0"), kb.buf("hT1")]
        mixT = kb.sb("mixT", [128, KC, TOK], BF16); mix_b = [kb.buf("mix0"), kb.buf("mix1")]
        xh = kb.sb("xh", [128, KC, 4], F32); xhb = kb.buf("xh"); kb._dbufs.append(xhb)
        hh_ = kb.sb("hh", [128, KC, 4], BF16); hhb = kb.buf("hh")
        zh = kb.sb("zh", [128, 8], F32); zhb = kb.buf("zh")
        kb.dma("sp", xh[:], g.xhT.rearrange("(kc p) n -> p kc n", p=128), xhb, [], [xhb])
        norm_x(g, PC_GMIX0, hT, hT_b)
        rs, rsb = sumsq_rstd(g, [(xh[:, kc, :], 128) for kc in range(KC)], 4, D, [xhb])
        for kc in range(KC):
            kb.op("dve", [xhb, rsb, g.par_b], [hhb], nc.vector.scalar_tensor_tensor, out=hh_[:, kc, :],
                  in0=xh[:, kc, :], scalar=pcol(g, PC_GMIX0 + kc), in1=rs[:, :4], op0=ALU.mult, op1=ALU.mult)
        wa = g.a_w_in.rearrange("(kc p) m -> p kc m", p=128)
        for j in range(12):
            w, wb = load_w(g, [((128, KC, 128), wa[:, :, s_ * 1536 + j * 128:s_ * 1536 + (j + 1) * 128])
                               for s_ in range(3)])
            ph, phb = bank(g)
            kb.mm(phb, ph[:, 0:4], [(w[0][:, kc, :], hh_[:, kc, :]) for kc in range(KC)], [wb, hhb])
            kb.mm(phb, ph[:, 4:8], [(w[2][:, kc, :], hh_[:, kc, :]) for kc in range(KC)], [wb, hhb])
            kb.op("act", [phb], [zhb], nc.scalar.copy, zh[:, 0:8], ph[:, 0:8])
            for t in range(NTL):
                sl = slice(t * TL, (t + 1) * TL)
                px, pxb = bank(g)
                kb.mm(pxb, px[:, :], [(w[0][:, kc, :], hT[:, kc, sl]) for kc in range(KC)], [wb, hT_b[t]])
                pc_, pcb = bank(g)
                kb.mm(pcb, pc_[:, :], [(w[2][:, kc, :], hT[:, kc, sl]) for kc in range(KC)], [wb, hT_b[t]])
                pg, pgb = bank(g)
                kb.mm(pgb, pg[:, :], [(w[1][:, kc, :], hT[:, kc, sl]) for kc in range(KC)], [wb, hT_b[t]])
                gc, gcb = scratch(g, "rs")
                kb.op("act", [pcb], [gcb], nc.scalar.copy, gc[:, :TL], pc_[:, :])
                z, zb = scratch(g, "fs")
                kb.op("dve", [pxb, gcb], [zb], nc.vector.tensor_tensor, out=z[:, 2:2 + TL], in0=px[:, :],
                      in1=gc[:, :TL], op=ALU.mult)
                kb.op("dve", [zhb, g.par_b, zb], [zb], nc.vector.scalar_tensor_tensor, out=z[:, 0:2],
                      in0=zh[:, 2 * t:2 * t + 2], scalar=pcol(g, PC_HALO + t), in1=zh[:, 4 + 2 * t:6 + 2 * t],
                      op0=ALU.mult, op1=ALU.mult)
                y, yb = scratch(g, "rs")
                cw = PC_CONV + j * 3
                kb.op("dve", [zb, g.par_b], [yb], nc.vector.tensor_scalar, out=y[:, :TL], in0=z[:, 0:TL],
                      scalar1=pcol(g, cw), scalar2=None, op0=ALU.mult)
                kb.op("dve", [zb, yb, g.par_b], [yb], nc.vector.scalar_tensor_tensor, out=y[:, :TL],
                      in0=z[:, 1:1 + TL], scalar=pcol(g, cw + 1), in1=y[:, :TL], op0=ALU.mult, op1=ALU.add)
                kb.op("dve", [zb, yb, g.par_b], [yb], nc.vector.scalar_tensor_tensor, out=y[:, :TL],
                      in0=z[:, 2:2 + TL], scalar=pcol(g, cw + 2), in1=y[:, :TL], op0=ALU.mult, op1=ALU.add)
                kb.op("dve", [pgb, yb], [mix_b[t]], nc.vector.tensor_tensor, out=mixT[:, j, sl], in0=pg[:, :],
                      in1=y[:, :TL], op=ALU.mult)
        (wq,), wqb = load_w(g, [((128, KC, 512),
                                  g.a_w_in.rearrange("(kc p) m -> p kc m", p=128)[:, :, 4608:5120])])
        mem_attention(g, wq, wqb, hT, hT_b, PC_GMQ0, kmT, kmT_b, vm, vm_b, mixT, mix_b)
        out_proj(g, 0, mixT, mix_b)
        kb.barrier()
        kb.stack = old
    with contextlib.ExitStack() as st:
        old = kb.stack
        kb.stack = st
        alloc_ring(g, 12288)
        hT = kb.sb("hT", [128, KC, TOK], BF16); hT_b = [kb.buf("hT0"), kb.buf("hT1")]
        norm_x(g, PC_GFFN0, hT, hT_b)
        act = [kb.sb(f"act{t}", [128, 2, TL], BF16) for t in range(NTL)]
        act_b = [[kb.buf(f"act{t}_{m}") for m in range(2)] for t in range(NTL)]
        for gi in range(DFF // 256):
            wg, wu, wd, wb = ffn_weights(g, g.ffn_gate, g.ffn_up, g.ffn_down, gi)
            ffn_group(g, wg, wu, wd, wb, hT, hT_b, act=act, act_b=act_b)
        ffn_drain(g)
        kb.barrier()
        kb.stack = old


def rope_AB(g, gcol, gswcol, AB, AB_b):
    kb, nc = g.kb, g.nc
    kb.op("dve", [g.cs_b, g.par_b], [AB_b], nc.vector.tensor_scalar, out=AB[:, 0, :], in0=g.cs[:, 0, :],
          scalar1=pcol(g, gcol, 64), scalar2=None, op0=ALU.mult)
    kb.op("dve", [g.cs_b, g.par_b], [AB_b], nc.vector.tensor_scalar, out=AB[:, 1, :], in0=g.cs[:, 1, :],
          scalar1=pcol(g, gswcol, 64), scalar2=None, op0=ALU.mult)


def shared_kv(g):
    kb, nc = g.kb, g.nc
    with contextlib.ExitStack() as st:
        old = kb.stack
        kb.stack = st
        hT = kb.sb("hT", [128, KC, TOK], BF16); hT_b = [kb.buf("hT0"), kb.buf("hT1")]
        ckv = kb.sb("ckv", [128, 2, TOK], BF16); ckv_b = [kb.buf("ckv0"), kb.buf("ckv1")]
        Rk = kb.sb("Rk", [64, TOK], F32); Rk_b = [kb.buf("Rk0"), kb.buf("Rk1")]
        sqpe = kb.sb("sqpe", [64, TOK], BF16); sqpe_b = [kb.buf("sqpe0"), kb.buf("sqpe1")]
        AB = kb.sb("ABk", [64, 2, TOK], F32); AB_b = kb.buf("ABk")
        kst = [kb.sb(f"kst{i}", [128, TOK], BF16) for i in range(2)]
        kst_b = [kb.buf(f"kst{i}") for i in range(2)]
        krst = [kb.sb(f"krst{i}", [64, TOK], BF16) for i in range(2)]
        krst_b = [kb.buf(f"krst{i}") for i in range(2)]
        vst = [kb.sb(f"vst{i}", [128, 512], BF16) for i in range(2)]
        vst_b = [kb.buf(f"vst{i}") for i in range(2)]
        for b in kst_b + krst_b + vst_b:
            kb._dbufs.append(b)
        alloc_ring(g, 8192)
        norm_x(g, PC_GKV, hT, hT_b)
        rope_AB(g, PC_GKN_R, PC_GKN_RS, AB, AB_b)
        wav = g.w_kv_a.rearrange("(kc p) m -> p kc m", p=128)
        (wa,), wab = load_w(g, [((128, KC, 320), wav)])
        wkbv = g.w_kv_b.rearrange("(kc p) m -> p kc m", p=128)
        (wb0, wb1), wbb = load_w(g, [((128, 3072), wkbv[:, 0, :]), ((128, 3072), wkbv[:, 1, :])])
        wbv = [wb0.rearrange("p (h c) -> p h c", h=12), wb1.rearrange("p (h c) -> p h c", h=12)]
        for t in range(NTL):
            sl = slice(t * TL, (t + 1) * TL)
            pl = []
            for c in range(2):
                pt, pb = bank(g)
                kb.mm(pb, pt[:, :], [(wa[:, kc, c * 128:(c + 1) * 128], hT[:, kc, sl]) for kc in range(KC)],
                      [wab, hT_b[t]])
                pl.append((pt, pb))
            rs, rsb = sumsq_rstd(g, [(pl[0][0][:, :], 128), (pl[1][0][:, :], 128)], TL, 256,
                                 [pl[0][1], pl[1][1]])
            for c in range(2):
                kb.op("dve", [pl[c][1], rsb, g.par_b], [ckv_b[t]], nc.vector.scalar_tensor_tensor,
                      out=ckv[:, c, sl], in0=pl[c][0][:, :], scalar=pcol(g, PC_GKVA + c), in1=rs[:],
                      op0=ALU.mult, op1=ALU.mult)
            pp, ppb = bank(g)
            kb.mm(ppb, pp[:64, :], [(wa[:, kc, 256:320], hT[:, kc, sl]) for kc in range(KC)], [wab, hT_b[t]])
            pq, pqb = bank(g)
            kb.mm_open([wab, hT_b[t]], [pqb])
            ins = None
            for kc in range(KC):
                nc.tensor.matmul(pq[0:32, :], wa[:, kc, 288:320], hT[:, kc, sl], start=(kc == 0), stop=(kc == KC - 1))
                ins = nc.tensor.matmul(pq[32:64, :], wa[:, kc, 256:288], hT[:, kc, sl], start=(kc == 0),
                                       stop=(kc == KC - 1))
                kb.n_inst += 2
            kb.mm_close(ins, [wab, hT_b[t]], [pqb])
            kb.op("act", [ppb], [sqpe_b[t]], nc.scalar.activation, out=sqpe[:, sl], in_=pp[:64, :], func=AF.Square)
            r1, r1b = scratch(g, "fs")
            kb.op("dve", [ppb, AB_b], [r1b], nc.vector.tensor_tensor, out=r1[:64, :TL], in0=pp[:64, :],
                  in1=AB[:, 0, sl], op=ALU.mult)
            kb.op("dve", [pqb, AB_b], [Rk_b[t]], nc.vector.tensor_tensor, out=Rk[:, sl], in0=pq[:64, :],
                  in1=AB[:, 1, sl], op=ALU.mult)
            kb.op("dve", [r1b, Rk_b[t]], [Rk_b[t]], nc.vector.tensor_tensor, out=Rk[:, sl], in0=Rk[:, sl],
                  in1=r1[:64, :TL], op=ALU.add)
        for h in range(12):
            si = h % 2
            for t in range(NTL):
                sl = slice(t * TL, (t + 1) * TL)
                pt, pb = bank(g)
                kb.mm(pb, pt[:, :], [(wbv[c][:, h, 0:128], ckv[:, c, sl]) for c in range(2)], [wbb, ckv_b[t]])
                ps_, psb_ = bank(g)
                sq, sqb = scratch(g, "sq")
                kb.op("act", [pb], [sqb], nc.scalar.activation, out=sq[:], in_=pt[:, :], func=AF.Square)
                kb.mm_open([sqb, sqpe_b[t], g.cst_b], [psb_])
                nc.tensor.matmul(ps_[:, :], g.ones[:, :], sq[:], start=True, stop=False)
                ins = nc.tensor.matmul(ps_[:, :], g.ones[:64, :], sqpe[:, sl], start=False, stop=True)
                kb.n_inst += 2
                kb.mm_close(ins, [sqb, sqpe_b[t]], [psb_])
                rs, rsb = scratch(g, "rs")
                kb.op("act", [psb_], [rsb], nc.scalar.activation, out=rs[:], in_=ps_[:, :], func=AF.Sqrt,
                      scale=float(1.0 / 192), bias=float(EPS))
                kb.op("dve", [rsb], [rsb], nc.vector.reciprocal, rs[:], rs[:])
                kb.op("dve", [pb, rsb, g.par_b], [kst_b[si]], nc.vector.scalar_tensor_tensor, out=kst[si][:, sl],
                      in0=pt[:, :], scalar=pcol(g, PC_GKN_N), in1=rs[:], op0=ALU.mult, op1=ALU.mult)
                kb.op("dve", [Rk_b[t], rsb], [krst_b[si]], nc.vector.tensor_tensor, out=krst[si][:, sl],
                      in0=Rk[:, sl], in1=rs[:64, :], op=ALU.mult)
            kb.dma("sp", g.kt_own(h)[0:128, :], kst[si][:], kst_b[si], [kst_b[si]], [])
            kb.dma("sp", g.kt_own(h)[128:192, :], krst[si][:], krst_b[si], [krst_b[si]], [])
        vi = 0
        for tb in range(TOK // 128):
            t = tb // 4
            for hg in range(3):
                pt, pb = bank(g)
                kb.mm(pb, pt[:, :], [(ckv[:, c, tb * 128:(tb + 1) * 128], wbv[c][:, hg * 4:(hg + 1) * 4, 128:256])
                                      for c in range(2)], [wbb, ckv_b[t]])
                s = vi % 2
                vi += 1
                kb.op("act", [pb], [vst_b[s]], nc.scalar.copy, vst[s][:], pt[:, :])
                for hq in range(4):
                    kb.dma("sp", g.v_own(hg * 4 + hq)[tb * 128:(tb + 1) * 128, :], vst[s][:, hq * 128:(hq + 1) * 128],
                           vst_b[s], [vst_b[s]], [])
        kb.barrier()
        kb.stack = old


def exchange_kv(g):
    kb, nc = g.kb, g.nc
    E = kb.E["pool"]
    kb.barrier()
    cs = kb.new_sem("cc")
    groups = [[0, 1, 2, 3], [4, 5, 6, 7]]
    n = 0
    for h in range(12):
        for src, dst in ((g.KT_own_l[h], g.KTg_l[h]), (g.V_own_l[h], g.Vg_l[h])):
            ins = nc.gpsimd.collective_compute("AllGather", ALU.bypass, replica_groups=groups,
                                               ins=[src.opt()], outs=[dst.opt()])
            ins.then_inc(cs)
            n += 1
    g.kv_tok = (cs, n, None)


def causal_attention(g, cqn, cqn_b, mixT, mix_b):
    kb, nc = g.kb, g.nc
    sc = float(192 ** -0.5)
    with contextlib.ExitStack() as st:
        old = kb.stack
        kb.stack = st
        NKV = 2
        kn = [kb.sb(f"kn{i}", [128, SEQ], BF16) for i in range(NKV)]
        kr = [kb.sb(f"kr{i}", [64, SEQ], BF16) for i in range(NKV)]
        vv = [kb.sb(f"vv{i}", [128, 32, 128], BF16) for i in range(NKV)]
        kv_b = [kb.buf(f"kv{i}") for i in range(NKV)]
        wqh = [kb.sb(f"wqh{i}", [128, 4, 192], BF16) for i in range(2)]
        wqh_b = [kb.buf(f"wqh{i}") for i in range(2)]
        qn = [kb.sb(f"qn{i}", [128, TOK], BF16) for i in range(2)]
        qr = [kb.sb(f"qr{i}", [64, TOK], BF16) for i in range(2)]
        q_b = [[kb.buf(f"q{i}_{t}") for t in range(NTL)] for i in range(2)]
        AB = kb.sb("ABq", [64, 2, TOK], F32); AB_b = kb.buf("ABq")
        qidx = kb.sb("qidx", [128, TOK], F32); qidx_b = kb.buf("qidx")
        for b in kv_b + wqh_b + [qidx_b]:
            kb._dbufs.append(b)
        kb.dma("sp", qidx[:], g.qidx.partition_broadcast(128).rearrange("p a n -> p (a n)"), qidx_b, [], [qidx_b])
        rope_AB(g, PC_GQN_R, PC_GQN_RS, AB, AB_b)
        wqv = g.w_q_b.rearrange("(kc p) m -> p kc m", p=128)
        g.bank_set = [4, 5, 6, 7]
        si = 0
        for h in range(12):
            par = h % 2
            b = kv_b[par]
            E = kb.E["sp"]
            if getattr(g, "kv_tok", None) is not None:
                kb._wait(E, g.kv_tok)
            kb._sync(E, [], [b])
            if b.dsem is None:
                b.dsem = kb.new_sem("d_" + b.name)
            if b.dcount:
                kb._wait(E, (b.dsem, b.dcount, None))
            for i in range(8):
                r, off = min(i, 7 - i), (512 if i >= 4 else 0)
                srcs = [(kn[par][:, i * 512:(i + 1) * 512], g.kt_g(h, r)[0:128, off:off + 512]),
                        (kr[par][:, i * 512:(i + 1) * 512], g.kt_g(h, r)[128:192, off:off + 512]),
                        (vv[par][:, i * 4:(i + 1) * 4, :],
                         g.v_g(h, r)[off:off + 512, :].rearrange("(kb p) d -> p kb d", p=128))]
                for o_, i_ in srcs:
                    ins = nc.sync.dma_start(out=o_, in_=i_)
                    b.dcount += 16
                    ins.then_inc(b.dsem, 16)
                    kb.n_inst += 1
            kb._commit((b.dsem, b.dcount, None), [], [b])
            kb.dma("pool", wqh[par][:], wqv[:, :, h * 192:(h + 1) * 192], wqh_b[par], [], [wqh_b[par]])
            w = wqh[par]
            for t in range(NTL):
                sl = slice(t * TL, (t + 1) * TL)
                pn, pnb = bank(g)
                kb.mm(pnb, pn[:, :], [(w[:, kc, 0:128], cqn[:, kc, sl]) for kc in range(4)], [wqh_b[par], cqn_b[t]])
                pr, prb = bank(g)
                kb.mm(prb, pr[:64, :], [(w[:, kc, 128:192], cqn[:, kc, sl]) for kc in range(4)], [wqh_b[par], cqn_b[t]])
                pq, pqb = bank(g)
                kb.mm_open([wqh_b[par], cqn_b[t]], [pqb])
                ins = None
                for kc in range(4):
                    nc.tensor.matmul(pq[0:32, :], w[:, kc, 160:192], cqn[:, kc, sl], start=(kc == 0), stop=(kc == 3))
                    ins = nc.tensor.matmul(pq[32:64, :], w[:, kc, 128:160], cqn[:, kc, sl], start=(kc == 0),
                                           stop=(kc == 3))
                    kb.n_inst += 2
                kb.mm_close(ins, [wqh_b[par], cqn_b[t]], [pqb])
                rs, rsb = sumsq_rstd(g, [(pn[:, :], 128), (pr[:64, :], 64)], TL, 192, [pnb, prb])
                kb.op("dve", [pnb, rsb, g.par_b], [q_b[par][t]], nc.vector.scalar_tensor_tensor, out=qn[par][:, sl],
                      in0=pn[:, :], scalar=pcol(g, PC_GQN_N), in1=rs[:], op0=ALU.mult, op1=ALU.mult)
                r1, r1b = scratch(g, "fs")
                kb.op("dve", [prb, AB_b], [r1b], nc.vector.tensor_tensor, out=r1[:64, :TL], in0=pr[:64, :],
                      in1=AB[:, 0, sl], op=ALU.mult)
                r2, r2b = scratch(g, "fs")
                kb.op("dve", [pqb, AB_b], [r2b], nc.vector.tensor_tensor, out=r2[:64, :TL], in0=pq[:64, :],
                      in1=AB[:, 1, sl], op=ALU.mult)
                kb.op("dve", [r1b, r2b], [r1b], nc.vector.tensor_tensor, out=r1[:64, :TL], in0=r1[:64, :TL],
                      in1=r2[:64, :TL], op=ALU.add)
                kb.op("dve", [r1b, rsb], [q_b[par][t]], nc.vector.tensor_tensor, out=qr[par][:, sl],
                      in0=r1[:64, :TL], in1=rs[:64, :], op=ALU.mult)
            for t in range(NTL):
                sl = slice(t * TL, (t + 1) * TL)
                nkb = 16 if t == 0 else 32
                po, pob = g.pst[2], g.psb[2]
                pd, pdb = g.pst[3], g.psb[3]
                def scores(kbi):
                    nonlocal si
                    S, Sb = g.pst[si % 2], g.psb[si % 2]
                    si += 1
                    ks = slice(kbi * 128, (kbi + 1) * 128)
                    kb.mm(Sb, S[:, :], [(kn[par][:, ks], qn[par][:, sl]), (kr[par][:, ks], qr[par][:, sl])],
                          [kv_b[par], q_b[par][t]])
                    return S, Sb

                pend = [scores(0), scores(1)]
                for kbi in range(nkb):
                    S, Sb = pend.pop(0)
                    e, eb = scratch(g, "sq")
                    kb.op("act", [Sb], [eb], nc.scalar.activation, out=e[:], in_=S[:, :], func=AF.Exp, scale=sc)
                    if kbi + 2 < nkb:
                        pend.append(scores(kbi + 2))
                    if t == 0 or kbi >= 16:
                        em, emb = scratch(g, "sq")
                        kb.op("dve", [qidx_b, g.par_b, eb], [emb], nc.vector.scalar_tensor_tensor, out=em[:],
                              in0=qidx[:, sl], scalar=pcol(g, PC_KIDX + kbi), in1=e[:], op0=ALU.is_ge, op1=ALU.mult)
                        e, eb = em, emb
                    kb.mm_open([eb, kv_b[par], g.cst_b], [pob, pdb] if kbi == 0 else [])
                    nc.tensor.matmul(po[:, :], vv[par][:, kbi, :], e[:], start=(kbi == 0), stop=(kbi == nkb - 1))
                    ins = nc.tensor.matmul(pd[:, :], g.ones[:, :], e[:], start=(kbi == 0), stop=(kbi == nkb - 1))
                    kb.n_inst += 2
                    kb.mm_close(ins, [eb, kv_b[par]], [pob, pdb])
                rd, rdb = scratch(g, "rs")
                kb.op("dve", [pdb], [rdb], nc.vector.reciprocal, rd[:], pd[:, :])
                kb.op("dve", [pob, rdb], [mix_b[t]], nc.vector.tensor_tensor, out=mixT[:, h, sl], in0=po[:, :],
                      in1=rd[:], op=ALU.mult)
        g.bank_set = list(range(8))
        kb.barrier()
        kb.stack = old


def moe(g):
    kb, nc = g.kb, g.nc
    with contextlib.ExitStack() as st:
        old = kb.stack
        kb.stack = st
        alloc_ring(g, 12288)
        hT = kb.sb("hT", [128, KC, TOK], BF16); hT_b = [kb.buf("hT0"), kb.buf("hT1")]
        selsb = kb.sb("selsb", [8, NEXP * 128], F32); sel_b = kb.buf("selsb"); kb._dbufs.append(sel_b)
        wr = kb.sb("wr", [128, KC, NEXP], F32); wr_b = kb.buf("wr"); kb._dbufs.append(wr_b)
        rtok = kb.sb("rtok", [128, 8], F32); rtok_b = kb.buf("rtok")
        lg = kb.sb("lg", [128, 8, NEXP], F32); lg_b = kb.buf("lg")
        t8 = kb.sb("t8", [128, 8, 8], F32); t8_b = kb.buf("t8")
        wts = kb.sb("wts", [128, 4, 8], F32); wts_b = kb.buf("wts")
        m1 = kb.sb("m1", [128, NEXP], F32); m1_b = kb.buf("m1")
        gates = kb.sb("gates", [128, 8, NEXP], F32); gates_b = kb.buf("gates")
        gT = kb.sb("gT", [8, TOK], F32); gT_b = kb.buf("gT")
        G = [kb.sb(f"G{i}", [128, TOK], F32) for i in range(2)]
        G_b = [[kb.buf(f"G{i}_{t}") for t in range(NTL)] for i in range(2)]
        act = [kb.sb(f"act{t}", [128, 2, TL], BF16) for t in range(NTL)]
        act_b = [[kb.buf(f"act{t}_{m}") for m in range(2)] for t in range(NTL)]
        kb.dma("sp", selsb[:], g.sel, sel_b, [], [sel_b])
        kb.dma("sp", wr[:], g.w_router.rearrange("(kc p) e -> p kc e", p=128), wr_b, [], [wr_b])
        for kc in range(KC):
            kb.op("dve", [wr_b, g.par_b], [wr_b], nc.vector.tensor_scalar, out=wr[:, kc, :], in0=wr[:, kc, :],
                  scalar1=pcol(g, PC_GFFN1 + kc), scalar2=None, op0=ALU.mult)

        def on_rstd(t, rs, rsb):
            for q in range(4):
                pt, pb = bank(g)
                kb.mm_open([rsb, g.cst_b], [pb])
                ins = nc.tensor.transpose(pt[:, 0:128], rs[:, q * 128:(q + 1) * 128], g.idf[:])
                kb.n_inst += 1
                kb.mm_close(ins, [rsb], [pb])
                kb.op("dve", [pb], [rtok_b], nc.vector.tensor_copy, rtok[:, t * 4 + q:t * 4 + q + 1], pt[:, 0:1])

        norm_x(g, PC_GFFN1, hT, hT_b, on_rstd=on_rstd)
        for tb in range(8):
            t = tb // 4
            pt, pb = bank(g)
            kb.mm(pb, pt[:, 0:NEXP], [(g.x[:, kc, tb * 128:(tb + 1) * 128], wr[:, kc, :]) for kc in range(KC)],
                  [wr_b] + [g.xb[kc][t] for kc in range(KC)])
            kb.op("dve", [pb, rtok_b], [lg_b], nc.vector.tensor_scalar, out=lg[:, tb, :], in0=pt[:, 0:NEXP],
                  scalar1=rtok[:, tb:tb + 1], scalar2=None, op0=ALU.mult)
            kb.op("dve", [lg_b], [t8_b], nc.vector.max, out=t8[:, tb, :], in_=lg[:, tb, :])
        kb.op("dve", [t8_b], [wts_b], nc.vector.tensor_tensor, out=wts[:, 0, :], in0=t8[:, :, 1], in1=t8[:, :, 0],
              op=ALU.subtract)
        kb.op("act", [wts_b], [wts_b], nc.scalar.activation, out=wts[:, 1, :], in_=wts[:, 0, :], func=AF.Exp)
        kb.op("dve", [wts_b], [wts_b], nc.vector.tensor_scalar, out=wts[:, 2, :], in0=wts[:, 1, :], scalar1=1.0,
              scalar2=None, op0=ALU.add)
        kb.op("dve", [wts_b], [wts_b], nc.vector.reciprocal, wts[:, 2, :], wts[:, 2, :])
        kb.op("dve", [wts_b], [wts_b], nc.vector.tensor_tensor, out=wts[:, 3, :], in0=wts[:, 1, :], in1=wts[:, 2, :],
              op=ALU.mult)
        for tb in range(8):
            kb.op("dve", [lg_b, t8_b, wts_b], [m1_b], nc.vector.tensor_scalar, out=m1[:], in0=lg[:, tb, :],
                  scalar1=t8[:, tb, 0:1], scalar2=wts[:, 2, tb:tb + 1], op0=ALU.is_equal, op1=ALU.mult)
            kb.op("dve", [lg_b, t8_b, wts_b], [gates_b], nc.vector.tensor_scalar, out=gates[:, tb, :], in0=lg[:, tb, :],
                  scalar1=t8[:, tb, 1:2], scalar2=wts[:, 3, tb:tb + 1], op0=ALU.is_equal, op1=ALU.mult)
            kb.op("dve", [m1_b, gates_b], [gates_b], nc.vector.tensor_tensor, out=gates[:, tb, :], in0=gates[:, tb, :],
                  in1=m1[:], op=ALU.add)
        for t in range(NTL):
            pt, pb = bank(g)
            kb.mm_open([gates_b, g.cst_b], [pb])
            ins = None
            for q in range(4):
                ins = nc.tensor.transpose(pt[0:NEXP, q * 128:(q + 1) * 128], gates[:, t * 4 + q, :], g.idf[:])
                kb.n_inst += 1
            kb.mm_close(ins, [gates_b], [pb])
            kb.op("act", [pb], [gT_b], nc.scalar.copy, gT[:, t * TL:(t + 1) * TL], pt[0:NEXP, :])
        for e in range(NEXP):
            gi_ = e % 2
            for t in range(NTL):
                sl = slice(t * TL, (t + 1) * TL)
                pt, pb = bank(g)
                kb.mm(pb, pt[:, :], [(selsb[:, e * 128:(e + 1) * 128], gT[:, sl])], [sel_b, gT_b])
                kb.op("act", [pb], [G_b[gi_][t]], nc.scalar.copy, G[gi_][:, sl], pt[:, :])
            for gi in range(DFF // 256):
                wg, wu, wd, wb = ffn_weights(g, g.moe_gate[e], g.moe_up[e], g.moe_down[e], gi)
                ffn_group(g, wg, wu, wd, wb, hT, hT_b, gate_bc=G[gi_], gate_bcb=G_b[gi_], act=act, act_b=act_b)
        ffn_drain(g)
        kb.barrier()
        kb.stack = old


def layer1(g):
    kb, nc = g.kb, g.nc
    with contextlib.ExitStack() as st0:
        old0 = kb.stack
        kb.stack = st0
        kmT = kb.sb("kmT", [128, 4, 256], BF16); kmT_b = kb.buf("kmT")
        vm = kb.sb("vm", [128, 2, 512], BF16); vm_b = kb.buf("vm")
        with contextlib.ExitStack() as st:
            kb.stack = st
            alloc_ring(g, 8192)
            mem_kv(g, 1, kmT, kmT_b, vm, vm_b)
            kb.stack = st0
        mixT = kb.sb("mixT", [128, KC, TOK], BF16); mix_b = [kb.buf("mix0"), kb.buf("mix1")]
        cqn = kb.sb("cqn", [128, 4, TOK], BF16); cqn_b = [kb.buf("cqn0"), kb.buf("cqn1")]
        with contextlib.ExitStack() as st:
            kb.stack = st
            alloc_ring(g, 8192)
            hT = kb.sb("hT", [128, KC, TOK], BF16); hT_b = [kb.buf("hT0"), kb.buf("hT1")]
            norm_x(g, PC_GMIX1, hT, hT_b)
            bw = g.b_w_in.rearrange("(kc p) m -> p kc m", p=128)
            (wqa,), wqab = load_w(g, [((128, KC, 512), bw[:, :, 0:512])])
            for t in range(NTL):
                sl = slice(t * TL, (t + 1) * TL)
                pl = []
                for c in range(4):
                    pt, pb = bank(g)
                    kb.mm(pb, pt[:, :], [(wqa[:, kc, c * 128:(c + 1) * 128], hT[:, kc, sl]) for kc in range(KC)],
                          [wqab, hT_b[t]])
                    pl.append((pt, pb))
                rs, rsb = sumsq_rstd(g, [(p[0][:, :], 128) for p in pl], TL, 512, [p[1] for p in pl])
                for c in range(4):
                    kb.op("dve", [pl[c][1], rsb, g.par_b], [cqn_b[t]], nc.vector.scalar_tensor_tensor,
                          out=cqn[:, c, sl], in0=pl[c][0][:, :], scalar=pcol(g, PC_GQA + c), in1=rs[:],
                          op0=ALU.mult, op1=ALU.mult)
            (wqm,), wqmb = load_w(g, [((128, KC, 512), bw[:, :, 512:1024])])
            mem_attention(g, wqm, wqmb, hT, hT_b, PC_GMQ1, kmT, kmT_b, vm, vm_b, mixT, mix_b)
            kb.barrier()
            kb.stack = st0
        causal_attention(g, cqn, cqn_b, mixT, mix_b)
        with contextlib.ExitStack() as st:
            kb.stack = st
            alloc_ring(g, 8192)
            out_proj(g, 1, mixT, mix_b)
            kb.barrier()
            kb.stack = st0
        kb.stack = old0
    moe(g)


def _core_tokens(core):
    b, c = core // 4, core % 4
    iA, iB = c, 7 - c
    idx = np.concatenate([np.arange(512 * iA, 512 * iA + 512), np.arange(512 * iB, 512 * iB + 512)])
    return b, (iA, iB), idx


def _chunk(v):
    return np.ascontiguousarray(v.reshape(-1, 128).T)


def _params(inp, core):
    b, (iA, iB), idx = _core_tokens(core)
    P = np.zeros((128, NPC), np.float32)
    P[:, PC_GMIX0:PC_GMIX0 + 16] = _chunk(inp["g_mix"][0])
    P[:, PC_GFFN0:PC_GFFN0 + 16] = _chunk(inp["g_ffn"][0])
    P[:, PC_GKV:PC_GKV + 16] = _chunk(inp["g_kv"])
    P[:, PC_GMIX1:PC_GMIX1 + 16] = _chunk(inp["g_mix"][1])
    P[:, PC_GFFN1:PC_GFFN1 + 16] = _chunk(inp["g_ffn"][1])
    cw = inp["a_conv_w"][0]
    P[:, PC_CONV:PC_CONV + 36] = cw.reshape(3, 12, 128).transpose(2, 1, 0).reshape(128, 36)
    P[:, PC_GMQ0] = inp["g_mq"][0]
    P[:, PC_GMK0] = inp["g_mk"][0]
    P[:, PC_GMQ1] = inp["g_mq"][1]
    P[:, PC_GMK1] = inp["g_mk"][1]
    P[:, PC_GQA:PC_GQA + 4] = _chunk(inp["b_g_q_a"][0])
    P[:, PC_GKVA:PC_GKVA + 2] = _chunk(inp["g_kv_a"])
    gq, gk = inp["b_g_qn"][0], inp["g_kn"]
    P[:, PC_GQN_N] = gq[:128]
    P[:, PC_GKN_N] = gk[:128]
    P[:64, PC_GQN_R] = gq[128:]
    P[:64, PC_GQN_RS] = np.concatenate([gq[160:], gq[128:160]])
    P[:64, PC_GKN_R] = gk[128:]
    P[:64, PC_GKN_RS] = np.concatenate([gk[160:], gk[128:160]])
    P[:32, PC_SGN] = -1.0
    P[32:64, PC_SGN] = 1.0
    invf = (1.0 / (10000.0 ** (np.arange(0, 64, 2, dtype=np.float32) / np.float32(64)))).astype(np.float32)
    P[:64, PC_INVF] = np.concatenate([invf, invf])
    P[:, PC_HALO] = 0.0 if iA == 0 else 1.0
    P[:, PC_HALO + 1] = 1.0
    P[:, PC_KIDX:PC_KIDX + 32] = (np.arange(32)[None, :] * 128 + np.arange(128)[:, None]).astype(np.float32)
    return P


def _consts():
    ident = np.eye(128, dtype=np.float32)
    sel = np.zeros((8, NEXP * 128), np.float32)
    for e in range(NEXP):
        sel[e, e * 128:(e + 1) * 128] = 1.0
    return ident, sel


def host_inputs(inp, core, mode):
    b, (iA, iB), idx = _core_tokens(core)
    ident, sel = _consts()
    m = {
        "params": _params(inp, core), "ident": ident, "sel": sel,
        "mem": np.ascontiguousarray(inp["mem"][b]), "g_mem": inp["g_mem"],
        "w_mem_kv": inp["w_mem_kv"], "w_out": inp["w_out"],
        "pos": np.ascontiguousarray(inp["positions"][b, idx][None, :]).astype(np.int32),
    }
    if mode in ("L0", "fused"):
        m["xT"] = np.ascontiguousarray(inp["x"][b, idx, :].T)
        xh = np.zeros((4, D), np.float32)
        for t, i in enumerate((iA, iB)):
            if i > 0:
                xh[2 * t:2 * t + 2] = inp["x"][b, 512 * i - 2:512 * i, :]
        m["xhT"] = np.ascontiguousarray(xh.T)
        m["a_w_in"] = inp["a_w_in"][0]
        m["w_kv_a"] = inp["w_kv_a"]
        m["w_kv_b"] = inp["w_kv_b"]
        m["ffn_gate"] = inp["ffn_w_gate"][0]
        m["ffn_up"] = inp["ffn_w_up"][0]
        m["ffn_down"] = inp["ffn_w_down"][0]
    if mode in ("L1", "fused"):
        m["qidx"] = idx[None, :].astype(np.float32)
        m["b_w_in"] = inp["b_w_in"][0]
        m["w_q_b"] = inp["b_w_q_b"][0]
        m["w_router"] = inp["moe_w_router"][0]
        m["moe_gate"] = inp["moe_w_gate"][0]
        m["moe_up"] = inp["moe_w_up"][0]
        m["moe_down"] = inp["moe_w_down"][0]
    return m


_PROGS = {}


def _prog(mode):
    if mode not in _PROGS:
        _PROGS[mode] = build(mode)[0]
    return _PROGS[mode]


FUSED = True


def kernel(**inputs):
    inp = {k: np.asarray(v) for k, v in inputs.items()}
    cores = list(range(NCORE))
    if FUSED:
        maps = [host_inputs(inp, c, "fused") for c in cores]
        res = run_bass_kernel_spmd(_prog("fused"), maps, core_ids=cores).results
    else:
        maps0 = [host_inputs(inp, c, "L0") for c in cores]
        r0 = run_bass_kernel_spmd(_prog("L0"), maps0, core_ids=cores).results
        maps1 = []
        for c in cores:
            b = c // 4
            m = host_inputs(inp, c, "L1")
            m["xT"] = np.asarray(r0[c]["x1T"])
            m["KTg"] = np.concatenate([np.asarray(r0[4 * b + r]["KT_own"]) for r in range(4)], axis=0)
            m["Vg"] = np.concatenate([np.asarray(r0[4 * b + r]["V_own"]) for r in range(4)], axis=0)
            maps1.append(m)
        res = run_bass_kernel_spmd(_prog("L1"), maps1, core_ids=cores).results
    out = np.zeros((NB, SEQ, D), np.float32)
    for c in cores:
        b, _, idx = _core_tokens(c)
        out[b, idx, :] = np.asarray(res[c]["yT"]).T
    return out
```

```python
import contextlib
import numpy as np
import concourse.bass as bass
import concourse.mybir as mybir
from concourse.bass_utils import run_bass_kernel_spmd

F32 = mybir.dt.float32
BF16 = mybir.dt.bfloat16
I32 = mybir.dt.int32
AF = mybir.ActivationFunctionType
ALU = mybir.AluOpType


class Buf:
    __slots__ = ("name", "writer", "readers", "dsem", "dcount")

    def __init__(self, name):
        self.name = name
        self.writer = None
        self.readers = []
        self.dsem = None
        self.dcount = 0


class Eng:
    def __init__(self, name, h, sem):
        self.name = name
        self.h = h
        self.sem = sem
        self.cnt = 0
        self.known = {}


class KB:
    def __init__(self, nc, stack):
        self.nc = nc
        self.stack = stack
        self.gstack = stack
        self.nsem = 0
        self.E = {}
        for name, h in (("pe", nc.tensor), ("act", nc.scalar), ("dve", nc.vector),
                        ("pool", nc.gpsimd), ("sp", nc.sync)):
            self.E[name] = Eng(name, h, self.new_sem("e_" + name))
        self.nbuf = 0
        self.n_inst = 0

    def new_sem(self, name):
        self.nsem += 1
        return self.gstack.enter_context(self.nc.semaphore(f"{name}_{self.nsem}"))

    def buf(self, name=None):
        self.nbuf += 1
        return Buf(name or f"b{self.nbuf}")

    def sb(self, name, shape, dt):
        self.nbuf += 1
        return self.stack.enter_context(self.nc.sbuf_tensor(f"{name}_{self.nbuf}", list(shape), dt))

    def ps(self, name, shape, dt=F32):
        return self.stack.enter_context(self.nc.psum_tensor(name, list(shape), dt))

    def _wait(self, E, tok):
        sem, val, src = tok
        if src is E and E.name == "pe":
            return
        key = id(sem)
        if E.known.get(key, 0) >= val:
            return
        E.h.wait_ge(sem, val)
        E.known[key] = val

    def _sync(self, E, reads, writes):
        for b in reads:
            if b.writer is not None:
                self._wait(E, b.writer)
        for b in writes:
            if b.writer is not None and b.writer[2] is not E:
                self._wait(E, b.writer)
            for r in b.readers:
                if r[2] is not E:
                    self._wait(E, r)

    def _commit(self, tok, reads, writes):
        for b in reads:
            b.readers.append(tok)
            if len(b.readers) > 64:
                best = {}
                for t in b.readers:
                    k = id(t[0])
                    if k not in best or best[k][1] < t[1]:
                        best[k] = t
                b.readers = list(best.values())
        for b in writes:
            b.writer = tok
            b.readers = []

    def op(self, eng, reads, writes, fn, *a, **kw):
        E = self.E[eng]
        self._sync(E, reads, writes)
        ins = fn(*a, **kw)
        E.cnt += 1
        ins.then_inc(E.sem, 1)
        self.n_inst += 1
        self._commit((E.sem, E.cnt, E), reads, writes)
        return ins

    def mm(self, out_buf, out_ap, pairs, reads, transpose=False):
        E = self.E["pe"]
        self._sync(E, reads, [out_buf])
        n = len(pairs)
        ins = None
        for i, (l, r) in enumerate(pairs):
            ins = self.nc.tensor.matmul(out_ap, l, r, start=(i == 0), stop=(i == n - 1))
            self.n_inst += 1
        E.cnt += 1
        ins.then_inc(E.sem, 1)
        self._commit((E.sem, E.cnt, E), reads, [out_buf])

    def mm_open(self, reads, writes):
        E = self.E["pe"]
        self._sync(E, reads, writes)

    def mm_close(self, ins, reads, writes):
        E = self.E["pe"]
        E.cnt += 1
        ins.then_inc(E.sem, 1)
        self._commit((E.sem, E.cnt, E), reads, writes)

    def dma(self, q, out_ap, in_ap, sbuf_buf, reads, writes, **kw):
        E = self.E[q]
        b = sbuf_buf
        if b.dsem is None:
            b.dsem = self.new_sem("d_" + b.name)
        self._sync(E, reads, writes)
        if b.dcount:
            self._wait(E, (b.dsem, b.dcount, None))
        ins = E.h.dma_start(out=out_ap, in_=in_ap, **kw)
        b.dcount += 16
        ins.then_inc(b.dsem, 16)
        self.n_inst += 1
        tok = (b.dsem, b.dcount, None)
        self._commit(tok, reads, writes)
        return tok

    def wait_tok(self, eng, tok):
        self._wait(self.E[eng], tok)


    def barrier(self):
        toks = [(E.sem, E.cnt, E) for E in self.E.values() if E.cnt > 0]
        toks += [(b.dsem, b.dcount, None) for b in self._dbufs if b.dcount > 0]
        for E in self.E.values():
            for t in toks:
                if t[2] is E:
                    continue
                self._wait(E, t)


D = 2048
SEQ = 4096
NB = 2
NCORE = 8
TOK = 1024
TL = 512
NTL = 2
KC = 16
DFF = 7168
NEXP = 8
EPS = 1e-6
NPC = 168

PC_GMIX0, PC_GFFN0, PC_GKV, PC_GMIX1, PC_GFFN1 = 0, 16, 32, 48, 64
PC_CONV = 80
PC_GMQ0, PC_GMK0, PC_GMQ1, PC_GMK1 = 116, 117, 118, 119
PC_GQA = 120
PC_GKVA = 124
PC_GQN_N, PC_GKN_N = 126, 127
PC_GQN_R, PC_GQN_RS, PC_GKN_R, PC_GKN_RS, PC_SGN, PC_INVF = 128, 129, 130, 131, 132, 133
PC_HALO = 134
PC_KIDX = 136


class Ctx:
    pass


def build(mode):
    nc = bass.Bass("TRN2", target_bir_lowering=False)
    do0 = mode in ("L0", "fused")
    do1 = mode in ("L1", "fused")

    def din(name, shape, dt=F32):
        return nc.dram_tensor(name, list(shape), dt, kind="ExternalInput").ap()

    def dout(name, shape, dt=F32):
        return nc.dram_tensor(name, list(shape), dt, kind="ExternalOutput").ap()

    def dint(name, shape, dt=F32):
        return nc.dram_tensor(name, list(shape), dt, kind="Internal").ap()

    g = Ctx()
    g.nc = nc
    g.xT = din("xT", [D, TOK])
    g.params = din("params", [128, NPC])
    g.ident = din("ident", [128, 128])
    g.sel = din("sel", [8, NEXP * 128])
    g.mem = din("mem", [256, D])
    g.g_mem = din("g_mem", [2, D])
    g.w_mem_kv = din("w_mem_kv", [2, D, 1024])
    g.w_out = din("w_out", [2, D, D])
    if do0:
        g.xhT = din("xhT", [D, 4])
        g.pos = din("pos", [1, TOK], I32)
        g.a_w_in = din("a_w_in", [D, 5120])
        g.w_kv_a = din("w_kv_a", [D, 320])
        g.w_kv_b = din("w_kv_b", [256, 3072])
        g.ffn_gate = din("ffn_gate", [D, DFF])
        g.ffn_up = din("ffn_up", [D, DFF])
        g.ffn_down = din("ffn_down", [DFF, D])
    if do1:
        if not do0:
            g.pos = din("pos", [1, TOK], I32)
        g.qidx = din("qidx", [1, TOK])
        g.b_w_in = din("b_w_in", [D, 1024])
        g.w_q_b = din("w_q_b", [512, 2304])
        g.w_router = din("w_router", [D, NEXP])
        g.moe_gate = din("moe_gate", [NEXP, D, DFF])
        g.moe_up = din("moe_up", [NEXP, D, DFF])
        g.moe_down = din("moe_down", [NEXP, DFF, D])
        g.yT = dout("yT", [D, TOK])
    if mode == "L0":
        g.x1T = dout("x1T", [D, TOK])
        KT_own = dout("KT_own", [2304, TOK], BF16)
        V_own = dout("V_own", [TOK, 1536], BF16)
        g.kt_own = lambda h: KT_own[h * 192:(h + 1) * 192, :]
        g.v_own = lambda h: V_own[:, h * 128:(h + 1) * 128]
    elif mode == "L1":
        KTg = din("KTg", [4 * 2304, TOK], BF16)
        Vg = din("Vg", [4 * TOK, 1536], BF16)
        g.kt_g = lambda h, r: KTg[r * 2304 + h * 192:r * 2304 + (h + 1) * 192, :]
        g.v_g = lambda h, r: Vg[r * TOK:(r + 1) * TOK, h * 128:(h + 1) * 128]
    else:
        g.KT_own_l = [dint(f"KT_own{h}", [192, TOK], BF16) for h in range(12)]
        g.V_own_l = [dint(f"V_own{h}", [TOK, 128], BF16) for h in range(12)]
        g.KTg_l = [dint(f"KTg{h}", [4 * 192, TOK], BF16) for h in range(12)]
        g.Vg_l = [dint(f"Vg{h}", [4 * TOK, 128], BF16) for h in range(12)]
        g.kt_own = lambda h: g.KT_own_l[h]
        g.v_own = lambda h: g.V_own_l[h]
        g.kt_g = lambda h, r: g.KTg_l[h][r * 192:(r + 1) * 192, :]
        g.v_g = lambda h, r: g.Vg_l[h][r * TOK:(r + 1) * TOK, :]

    with contextlib.ExitStack() as st:
        kb = KB(nc, st)
        kb._dbufs = []
        g.kb = kb
        g.fused = (mode == "fused")
        g.cc_sem = kb.new_sem("cc")
        g.cc_n = 0
        g.kv_tok_h = {}
        setup_globals(g)
        if do0:
            layer0(g)
            shared_kv(g)
            if mode == "L0":
                store_x(g, g.x1T)
        if mode == "fused":
            exchange_kv(g)
        if do1:
            layer1(g)
            store_x(g, g.yT)
        kb.barrier()
        g.n_inst = kb.n_inst
    return nc, g


def setup_globals(g):
    kb, nc = g.kb, g.nc
    g.x = kb.sb("x", [128, KC, TOK], F32)
    g.xb = [[kb.buf(f"x{kc}_{t}") for t in range(NTL)] for kc in range(KC)]
    g.par = kb.sb("par", [128, NPC], F32)
    g.par_b = kb.buf("par")
    g.idf = kb.sb("idf", [128, 128], F32)
    g.idb = kb.sb("idb", [128, 128], BF16)
    g.ones = kb.sb("ones", [128, 128], BF16)
    g.cst_b = kb.buf("cst")
    g.cs = kb.sb("cs", [64, 2, TOK], F32)
    g.cs_b = kb.buf("cs")
    g.pst = [kb.ps(f"ps{i}", [128, 512]) for i in range(8)]
    g.psb = [kb.buf(f"ps{i}") for i in range(8)]
    g.bank_i = 0
    g.bank_set = list(range(8))
    g.ffn_pending = []
    g.sq = [kb.sb(f"sq{i}", [128, 512], BF16) for i in range(6)]
    g.sq_b = [kb.buf(f"sq{i}") for i in range(6)]
    g.sq_i = 0
    g.rs = [kb.sb(f"rs{i}", [128, 512], F32) for i in range(3)]
    g.rs_b = [kb.buf(f"rs{i}") for i in range(3)]
    g.rs_i = 0
    g.fs = [kb.sb(f"fs{i}", [128, 514], F32) for i in range(3)]
    g.fs_b = [kb.buf(f"fs{i}") for i in range(3)]
    g.fs_i = 0

    xb_all = [b for row in g.xb for b in row]
    xv = g.xT.rearrange("(kc p) n -> p kc n", p=128)
    ldb = kb.buf("xload"); kb._dbufs.append(ldb)
    for t in range(NTL):
        kb.dma("sp", g.x[:, :, t * TL:(t + 1) * TL], xv[:, :, t * TL:(t + 1) * TL], ldb, [],
               [g.xb[kc][t] for kc in range(KC)])
    cb = kb.buf("cload"); kb._dbufs.append(cb)
    kb.dma("sp", g.par[:], g.params, cb, [], [g.par_b])
    kb.dma("sp", g.idf[:], g.ident, cb, [], [g.cst_b])
    kb.op("dve", [g.cst_b], [g.cst_b], nc.vector.tensor_copy, g.idb[:], g.idf[:])
    kb.op("dve", [], [g.cst_b], nc.vector.memset, g.ones[:], 1.0)
    rope_tables(g)


def bank(g):
    bs = g.bank_set
    for _ in range(len(bs)):
        i = bs[g.bank_i % len(bs)]
        g.bank_i += 1
        b = g.psb[i]
        if b.writer is None or b.readers:
            return g.pst[i], b
    raise RuntimeError("no free PSUM bank")


def alloc_ring(g, slot_el, nslot=2):
    kb = g.kb
    g.NSLOT = nslot
    g.SLOT = slot_el
    g.ring_gen = getattr(g, "ring_gen", 0) + 1
    g.wslot = [kb.sb(f"wslot{g.ring_gen}_{i}", [128, slot_el], BF16) for i in range(nslot)]
    g.wslot_b = [kb.buf(f"wslot{g.ring_gen}_{i}") for i in range(nslot)]
    for b in g.wslot_b:
        kb._dbufs.append(b)
    g.slot_i = 0


def scratch(g, kind):
    lst, bl, key = {"sq": (g.sq, g.sq_b, "sq_i"), "rs": (g.rs, g.rs_b, "rs_i"),
                    "fs": (g.fs, g.fs_b, "fs_i")}[kind]
    i = getattr(g, key)
    setattr(g, key, (i + 1) % len(lst))
    return lst[i], bl[i]


def load_w(g, pieces):
    kb = g.kb
    i = g.slot_i
    g.slot_i = (i + 1) % g.NSLOT
    t, b = g.wslot[i], g.wslot_b[i]
    views = []
    off = 0
    E = kb.E["pool"]
    kb._sync(E, [], [b])
    if b.dsem is None:
        b.dsem = kb.new_sem("d_" + b.name)
    if b.dcount:
        kb._wait(E, (b.dsem, b.dcount, None))
    for shape, src in pieces:
        n = int(np.prod(shape[1:]))
        v = t[:shape[0], off:off + n]
        if len(shape) == 3:
            v = v.rearrange("p (a b) -> p a b", a=shape[1])
        elif len(shape) == 4:
            v = v.rearrange("p (a b c) -> p a b c", a=shape[1], b=shape[2])
        ins = g.nc.gpsimd.dma_start(out=v, in_=src, max_dma_last_dim=4096)
        b.dcount += 16
        ins.then_inc(b.dsem, 16)
        kb.n_inst += 1
        views.append(v)
        off += n
    assert off <= g.SLOT
    kb._commit((b.dsem, b.dcount, None), [], [b])
    return views, b


def pcol(g, c, P=128):
    return g.par[:P, c:c + 1]


def rope_tables(g):
    kb, nc = g.kb, g.nc
    with contextlib.ExitStack() as st:
        old = kb.stack
        kb.stack = st
        posi = kb.sb("posi", [64, TOK], I32); pb = kb.buf("posi"); kb._dbufs.append(pb)
        ang = kb.sb("ang", [64, TOK], F32); ab = kb.buf("ang")
        t1 = kb.sb("rt1", [64, TOK], F32); t1b = kb.buf("rt1")
        ki = kb.sb("rki", [64, TOK], I32); kib = kb.buf("rki")
        kf = kb.sb("rkf", [64, TOK], F32); kfb = kb.buf("rkf")
        m = kb.sb("rm", [64, TOK], F32); mb = kb.buf("rm")
        kb.dma("sp", posi[:], g.pos.partition_broadcast(64).rearrange("p a n -> p (a n)"), pb, [], [pb])
        kb.op("dve", [pb], [ab], nc.vector.tensor_copy, ang[:], posi[:])
        kb.op("dve", [ab, g.par_b], [ab], nc.vector.tensor_scalar, out=ang[:], in0=ang[:],
              scalar1=pcol(g, PC_INVF, 64), scalar2=None, op0=ALU.mult)
        TWO_PI = 2.0 * np.pi
        for which, shift in ((1, 0.0), (0, np.pi / 2)):
            kb.op("dve", [ab], [t1b], nc.vector.tensor_scalar, out=t1[:], in0=ang[:],
                  scalar1=float(shift), scalar2=float(1.0 / TWO_PI), op0=ALU.add, op1=ALU.mult)
            kb.op("dve", [t1b], [kib], nc.vector.tensor_copy, ki[:], t1[:])
            kb.op("dve", [kib], [kfb], nc.vector.tensor_copy, kf[:], ki[:])
            kb.op("dve", [kfb, ab], [t1b], nc.vector.scalar_tensor_tensor, out=t1[:], in0=kf[:],
                  scalar=float(-TWO_PI), in1=ang[:], op0=ALU.mult, op1=ALU.add)
            if shift:
                kb.op("dve", [t1b], [t1b], nc.vector.tensor_scalar, out=t1[:], in0=t1[:],
                      scalar1=float(shift), scalar2=None, op0=ALU.add)
            kb.op("dve", [t1b], [mb], nc.vector.tensor_scalar, out=m[:], in0=t1[:],
                  scalar1=float(np.pi), scalar2=float(-TWO_PI), op0=ALU.is_gt, op1=ALU.mult)
            kb.op("dve", [mb, t1b], [t1b], nc.vector.tensor_tensor, out=t1[:], in0=t1[:], in1=m[:], op=ALU.add)
            kb.op("dve", [t1b], [mb], nc.vector.tensor_scalar, out=m[:], in0=t1[:],
                  scalar1=float(-np.pi), scalar2=float(TWO_PI), op0=ALU.is_lt, op1=ALU.mult)
            kb.op("dve", [mb, t1b], [t1b], nc.vector.tensor_tensor, out=t1[:], in0=t1[:], in1=m[:], op=ALU.add)
            kb.op("dve", [t1b], [t1b], nc.vector.tensor_scalar, out=t1[:], in0=t1[:],
                  scalar1=float(np.pi), scalar2=float(-np.pi), op0=ALU.min, op1=ALU.max)
            kb.op("act", [t1b], [g.cs_b], nc.scalar.activation, out=g.cs[:, which, :], in_=t1[:], func=AF.Sin)
        kb.op("dve", [g.cs_b, g.par_b], [g.cs_b], nc.vector.tensor_scalar, out=g.cs[:, 1, :], in0=g.cs[:, 1, :],
              scalar1=pcol(g, PC_SGN, 64), scalar2=None, op0=ALU.mult)
        kb.barrier()
        kb.stack = old


def sumsq_rstd(g, srcs, N, Dn, reads):
    kb, nc = g.kb, g.nc
    pt, pb = bank(g)
    n = len(srcs)
    for i, (ap, P) in enumerate(srcs):
        sq, sqb = scratch(g, "sq")
        kb.op("act", reads, [sqb], nc.scalar.activation, out=sq[:P, :N], in_=ap, func=AF.Square)
        kb.mm_open([sqb, g.cst_b], [pb] if i == 0 else [])
        ins = nc.tensor.matmul(pt[:, :N], g.ones[:P, :], sq[:P, :N], start=(i == 0), stop=(i == n - 1))
        kb.n_inst += 1
        kb.mm_close(ins, [sqb], [pb])
    rs, rsb = scratch(g, "rs")
    kb.op("act", [pb], [rsb], nc.scalar.activation, out=rs[:, :N], in_=pt[:, :N], func=AF.Sqrt,
          scale=float(1.0 / Dn), bias=float(EPS))
    kb.op("dve", [rsb], [rsb], nc.vector.reciprocal, rs[:, :N], rs[:, :N])
    return rs, rsb


def norm_x(g, gcol0, hT, hT_b, tiles=(0, 1), on_rstd=None):
    kb, nc = g.kb, g.nc
    for t in tiles:
        sl = slice(t * TL, (t + 1) * TL)
        rs, rsb = sumsq_rstd(g, [(g.x[:, kc, sl], 128) for kc in range(KC)], TL, D,
                             [g.xb[kc][t] for kc in range(KC)])
        if on_rstd is not None:
            on_rstd(t, rs, rsb)
        for kc in range(KC):
            kb.op("dve", [g.xb[kc][t], rsb, g.par_b], [hT_b[t]], nc.vector.scalar_tensor_tensor,
                  out=hT[:, kc, sl], in0=g.x[:, kc, sl], scalar=pcol(g, gcol0 + kc), in1=rs[:, :TL],
                  op0=ALU.mult, op1=ALU.mult)


def store_x(g, dst):
    kb = g.kb
    dv = dst.rearrange("(kc p) n -> p kc n", p=128)
    sb = kb.buf("xstore"); kb._dbufs.append(sb)
    for t in range(NTL):
        kb.dma("sp", dv[:, :, t * TL:(t + 1) * TL], g.x[:, :, t * TL:(t + 1) * TL], sb,
               [g.xb[kc][t] for kc in range(KC)], [])


def mem_kv(g, l, kmT, kmT_b, vm, vm_b):
    kb, nc = g.kb, g.nc
    with contextlib.ExitStack() as st:
        old = kb.stack
        kb.stack = st
        mt = kb.sb("mem_t", [128, 2, D], F32); mtb = kb.buf("mem_t"); kb._dbufs.append(mtb)
        gb = kb.sb("gmem_bc", [128, D], F32); gbb = kb.buf("gmem_bc"); kb._dbufs.append(gbb)
        mn = kb.sb("mem_n", [128, 2, D], BF16); mnb = kb.buf("mem_n")
        mnT = kb.sb("mem_nT", [128, KC, 256], BF16); mnTb = kb.buf("mem_nT")
        ssq = kb.sb("mem_ss", [128, 2], F32); ssb = kb.buf("mem_ss")
        kb.dma("sp", mt[:], g.mem.rearrange("(mb p) d -> p mb d", p=128), mtb, [], [mtb])
        kb.dma("sp", gb[:], g.g_mem[l:l + 1, :].partition_broadcast(128).rearrange("p a n -> p (a n)"),
               gbb, [], [gbb])
        for mb in range(2):
            kb.op("act", [mtb], [mnb, ssb], nc.scalar.activation, out=mn[:, mb, :], in_=mt[:, mb, :],
                  func=AF.Square, accum_out=ssq[:, mb:mb + 1])
        kb.op("act", [ssb], [ssb], nc.scalar.activation, out=ssq[:], in_=ssq[:], func=AF.Sqrt,
              scale=float(1.0 / D), bias=float(EPS))
        kb.op("dve", [ssb], [ssb], nc.vector.reciprocal, ssq[:], ssq[:])
        for mb in range(2):
            kb.op("dve", [mtb, ssb, gbb], [mnb], nc.vector.scalar_tensor_tensor, out=mn[:, mb, :],
                  in0=mt[:, mb, :], scalar=ssq[:, mb:mb + 1], in1=gb[:], op0=ALU.mult, op1=ALU.mult)
        for mb in range(2):
            for k4 in range(4):
                pt, pb = bank(g)
                ptb = pt[:].bitcast(BF16)
                kb.mm_open([mnb, g.cst_b], [pb])
                ins = None
                for q in range(4):
                    kc = k4 * 4 + q
                    ins = nc.tensor.transpose(ptb[:, q * 128:(q + 1) * 128], mn[:, mb, kc * 128:(kc + 1) * 128],
                                              g.idb[:])
                    kb.n_inst += 1
                kb.mm_close(ins, [mnb], [pb])
                kb.op("dve", [pb], [mnTb], nc.vector.tensor_copy,
                      mnT[:, k4 * 4:(k4 + 1) * 4, mb * 128:(mb + 1) * 128],
                      ptb[:, 0:512].rearrange("p (q n) -> p q n", q=4))
        wv = g.w_mem_kv[l].rearrange("(kc p) m -> p kc m", p=128)
        (wk,), wkb = load_w(g, [((128, KC, 512), wv[:, :, 0:512])])
        (wvv,), wvb = load_w(g, [((128, KC, 512), wv[:, :, 512:1024])])
        gk = PC_GMK0 if l == 0 else PC_GMK1
        for hh in range(4):
            pt, pb = bank(g)
            kb.mm(pb, pt[:, :256], [(wk[:, kc, hh * 128:(hh + 1) * 128], mnT[:, kc, :]) for kc in range(KC)],
                  [wkb, mnTb])
            rs, rsb = sumsq_rstd(g, [(pt[:, :256], 128)], 256, 128, [pb])
            kb.op("dve", [pb, rsb, g.par_b], [kmT_b], nc.vector.scalar_tensor_tensor, out=kmT[:, hh, :],
                  in0=pt[:, :256], scalar=pcol(g, gk), in1=rs[:, :256], op0=ALU.mult, op1=ALU.mult)
        for mb in range(2):
            pt, pb = bank(g)
            kb.mm(pb, pt[:, :], [(mnT[:, kc, mb * 128:(mb + 1) * 128], wvv[:, kc, :]) for kc in range(KC)],
                  [wvb, mnTb])
            kb.op("act", [pb], [vm_b], nc.scalar.copy, vm[:, mb, :], pt[:, :])
        kb.barrier()
        kb.stack = old


def mem_attention(g, wq, wqb, hT, hT_b, gq_col, kmT, kmT_b, vm, vm_b, mixT, mix_b):
    kb, nc = g.kb, g.nc
    sc = float(128 ** -0.5)
    qm = [kb.sb(f"qm{i}", [128, TL], BF16) for i in range(2)]
    qm_b = [kb.buf(f"qm{i}") for i in range(2)]
    qi = 0
    for t in range(NTL):
        sl = slice(t * TL, (t + 1) * TL)
        for hh in range(4):
            pt, pb = bank(g)
            kb.mm(pb, pt[:, :], [(wq[:, kc, hh * 128:(hh + 1) * 128], hT[:, kc, sl]) for kc in range(KC)],
                  [wqb, hT_b[t]])
            rs, rsb = sumsq_rstd(g, [(pt[:, :], 128)], TL, 128, [pb])
            q, qb = qm[qi % 2], qm_b[qi % 2]
            qi += 1
            kb.op("dve", [pb, rsb, g.par_b], [qb], nc.vector.scalar_tensor_tensor, out=q[:],
                  in0=pt[:, :], scalar=pcol(g, gq_col), in1=rs[:], op0=ALU.mult, op1=ALU.mult)
            po, pob = bank(g)
            pd, pdb = bank(g)
            for mb in range(2):
                ps_, psb_ = bank(g)
                kb.mm(psb_, ps_[:, :], [(kmT[:, hh, mb * 128:(mb + 1) * 128], q[:])], [kmT_b, qb])
                e, eb = scratch(g, "sq")
                kb.op("act", [psb_], [eb], nc.scalar.activation, out=e[:], in_=ps_[:, :], func=AF.Exp, scale=sc)
                kb.mm_open([eb, vm_b, g.cst_b], [pob, pdb] if mb == 0 else [])
                nc.tensor.matmul(po[:, :], vm[:, mb, hh * 128:(hh + 1) * 128], e[:], start=(mb == 0), stop=(mb == 1))
                ins = nc.tensor.matmul(pd[:, :], g.ones[:, :], e[:], start=(mb == 0), stop=(mb == 1))
                kb.n_inst += 2
                kb.mm_close(ins, [eb, vm_b], [pob, pdb])
            rd, rdb = scratch(g, "rs")
            kb.op("dve", [pdb], [rdb], nc.vector.reciprocal, rd[:], pd[:, :])
            kb.op("dve", [pob, rdb], [mix_b[t]], nc.vector.tensor_tensor, out=mixT[:, 12 + hh, sl], in0=po[:, :],
                  in1=rd[:], op=ALU.mult)


def out_proj(g, l, mixT, mix_b):
    kb, nc = g.kb, g.nc
    wv = g.w_out[l].rearrange("(kc p) m -> p kc m", p=128)
    for pc in range(4):
        (w,), wb = load_w(g, [((128, KC, 512), wv[:, :, pc * 512:(pc + 1) * 512])])
        for t in range(NTL):
            sl = slice(t * TL, (t + 1) * TL)
            for dq in range(4):
                dc = pc * 4 + dq
                pt, pb = bank(g)
                kb.mm(pb, pt[:, :], [(w[:, kc, dq * 128:(dq + 1) * 128], mixT[:, kc, sl]) for kc in range(KC)],
                      [wb, mix_b[t]])
                kb.op("dve", [pb, g.xb[dc][t]], [g.xb[dc][t]], nc.vector.tensor_tensor, out=g.x[:, dc, sl],
                      in0=g.x[:, dc, sl], in1=pt[:, :], op=ALU.add)


def ffn_drain(g, n=None):
    pend = g.ffn_pending
    k = len(pend) if n is None else min(n, len(pend))
    for _ in range(k):
        pend.pop(0)()


def ffn_group(g, wg, wu, wd, wb, hT, hT_b, gate_bc=None, gate_bcb=None, act=None, act_b=None):
    kb, nc = g.kb, g.nc
    for t in range(NTL):
        sl = slice(t * TL, (t + 1) * TL)
        for m in range(2):
            pg, pgb = bank(g)
            kb.mm(pgb, pg[:, :], [(wg[:, kc, m * 128:(m + 1) * 128], hT[:, kc, sl]) for kc in range(KC)],
                  [wb, hT_b[t]])
            sg, sgb = scratch(g, "sq")
            kb.op("act", [pgb], [sgb], nc.scalar.activation, out=sg[:], in_=pg[:, :], func=AF.Silu)
            ffn_drain(g, 4)
            pu, pub = bank(g)
            kb.mm(pub, pu[:, :], [(wu[:, kc, m * 128:(m + 1) * 128], hT[:, kc, sl]) for kc in range(KC)],
                  [wb, hT_b[t]])
            if gate_bc is None:
                kb.op("dve", [pub, sgb], [act_b[t][m]], nc.vector.tensor_tensor, out=act[t][:, m, :], in0=pu[:, :],
                      in1=sg[:], op=ALU.mult)
            else:
                ug, ugb = scratch(g, "fs")
                kb.op("dve", [pub, gate_bcb[t]], [ugb], nc.vector.tensor_tensor, out=ug[:, :TL], in0=pu[:, :],
                      in1=gate_bc[:, sl], op=ALU.mult)
                kb.op("dve", [ugb, sgb], [act_b[t][m]], nc.vector.tensor_tensor, out=act[t][:, m, :],
                      in0=ug[:, :TL], in1=sg[:], op=ALU.mult)
            ffn_drain(g, 4)
        ffn_drain(g)

        def down(dc, t=t, sl=sl, wd=wd, wb=wb):
            pt, pb = bank(g)
            kb.mm(pb, pt[:, :], [(wd[m][:, dc * 128:(dc + 1) * 128], act[t][:, m, :]) for m in range(2)],
                  [wb, act_b[t][0], act_b[t][1]])
            kb.op("dve", [pb, g.xb[dc][t]], [g.xb[dc][t]], nc.vector.tensor_tensor, out=g.x[:, dc, sl],
                  in0=g.x[:, dc, sl], in1=pt[:, :], op=ALU.add)

        for dc in range(KC):
            g.ffn_pending.append(lambda dc=dc, f=down: f(dc))


def ffn_weights(g, wgate, wup, wdown, gi):
    c0 = gi * 256
    gv = wgate.rearrange("(kc p) m -> p kc m", p=128)[:, :, c0:c0 + 256]
    uv = wup.rearrange("(kc p) m -> p kc m", p=128)[:, :, c0:c0 + 256]
    dv = wdown[c0:c0 + 256, :].rearrange("(m p) d -> p m d", p=128)
    (wg, wu, wd0, wd1), wb = load_w(g, [((128, KC, 256), gv), ((128, KC, 256), uv), ((128, D), dv[:, 0, :]),
                                          ((128, D), dv[:, 1, :])])
    return wg, wu, (wd0, wd1), wb


def layer0(g):
    kb, nc = g.kb, g.nc
    with contextlib.ExitStack() as st:
        old = kb.stack
        kb.stack = st
        alloc_ring(g, 8192)
        kmT = kb.sb("kmT", [128, 4, 256], BF16); kmT_b = kb.buf("kmT")
        vm = kb.sb("vm", [128, 2, 512], BF16); vm_b = kb.buf("vm")
        mem_kv(g, 0, kmT, kmT_b, vm, vm_b)
        hT = kb.sb("hT", [128, KC, TOK], BF16); hT_b = [kb.buf("hT0"), kb.buf("hT1")]
        mixT = kb.sb("mixT", [128, KC, TOK], BF16); mix_b = [kb.buf("mix0"), kb.buf("mix1")]
        xh = kb.sb("xh", [128, KC, 4], F32); xhb = kb.buf("xh"); kb._dbufs.append(xhb)
        hh_ = kb.sb("hh", [128, KC, 4], BF16); hhb = kb.buf("hh")
        zh = kb.sb("zh", [128, 8], F32); zhb = kb.buf("zh")
        kb.dma("sp", xh[:], g.xhT.rearrange("(kc p) n -> p kc n", p=128), xhb, [], [xhb])
        norm_x(g, PC_GMIX0, hT, hT_b)
        rs, rsb = sumsq_rstd(g, [(xh[:, kc, :], 128) for kc in range(KC)], 4, D, [xhb])
        for kc in range(KC):
            kb.op("dve", [xhb, rsb, g.par_b], [hhb], nc.vector.scalar_tensor_tensor, out=hh_[:, kc, :],
                  in0=xh[:, kc, :], scalar=pcol(g, PC_GMIX0 + kc), in1=rs[:, :4], op0=ALU.mult, op1=ALU.mult)
        wa = g.a_w_in.rearrange("(kc p) m -> p kc m", p=128)
        for j in range(12):
            w, wb = load_w(g, [((128, KC, 128), wa[:, :, s_ * 1536 + j * 128:s_ * 1536 + (j + 1) * 128])
                               for s_ in range(3)])
            ph, phb = bank(g)
            kb.mm(phb, ph[:, 0:4], [(w[0][:, kc, :], hh_[:, kc, :]) for kc in range(KC)], [wb, hhb])
            kb.mm(phb, ph[:, 4:8], [(w[2][:, kc, :], hh_[:, kc, :]) for kc in range(KC)], [wb, hhb])
            kb.op("act", [phb], [zhb], nc.scalar.copy, zh[:, 0:8], ph[:, 0:8])
            for t in range(NTL):
                sl = slice(t * TL, (t + 1) * TL)
                px, pxb = bank(g)
                kb.mm(pxb, px[:, :], [(w[0][:, kc, :], hT[:, kc, sl]) for kc in range(KC)], [wb, hT_b[t]])
                pc_, pcb = bank(g)
                kb.mm(pcb, pc_[:, :], [(w[2][:, kc, :], hT[:, kc, sl]) for kc in range(KC)], [wb, hT_b[t]])
                pg, pgb = bank(g)
                kb.mm(pgb, pg[:, :], [(w[1][:, kc, :], hT[:, kc, sl]) for kc in range(KC)], [wb, hT_b[t]])
                gc, gcb = scratch(g, "rs")
                kb.op("act", [pcb], [gcb], nc.scalar.copy, gc[:, :TL], pc_[:, :])
                z, zb = scratch(g, "fs")
                kb.op("dve", [pxb, gcb], [zb], nc.vector.tensor_tensor, out=z[:, 2:2 + TL], in0=px[:, :],
                      in1=gc[:, :TL], op=ALU.mult)
                kb.op("dve", [zhb, g.par_b, zb], [zb], nc.vector.scalar_tensor_tensor, out=z[:, 0:2],
                      in0=zh[:, 2 * t:2 * t + 2], scalar=pcol(g, PC_HALO + t), in1=zh[:, 4 + 2 * t:6 + 2 * t],
                      op0=ALU.mult, op1=ALU.mult)
                y, yb = scratch(g, "rs")
                cw = PC_CONV + j * 3
                kb.op("dve", [zb, g.par_b], [yb], nc.vector.tensor_scalar, out=y[:, :TL], in0=z[:, 0:TL],
                      scalar1=pcol(g, cw), scalar2=None, op0=ALU.mult)
                kb.op("dve", [zb, yb, g.par_b], [yb], nc.vector.scalar_tensor_tensor, out=y[:, :TL],
                      in0=z[:, 1:1 + TL], scalar=pcol(g, cw + 1), in1=y[:, :TL], op0=ALU.mult, op1=ALU.add)
                kb.op("dve", [zb, yb, g.par_b], [yb], nc.vector.scalar_tensor_tensor, out=y[:, :TL],
                      in0=z[:, 2:2 + TL], scalar=pcol(g, cw + 2), in1=y[:, :TL], op0=ALU.mult, op1=ALU.add)
                kb.op("dve", [pgb, yb], [mix_b[t]], nc.vector.tensor_tensor, out=mixT[:, j, sl], in0=pg[:, :],
                      in1=y[:, :TL], op=ALU.mult)
        (wq,), wqb = load_w(g, [((128, KC, 512),
                                  g.a_w_in.rearrange("(kc p) m -> p kc m", p=128)[:, :, 4608:5120])])
        mem_attention(g, wq, wqb, hT, hT_b, PC_GMQ0, kmT, kmT_b, vm, vm_b, mixT, mix_b)
        out_proj(g, 0, mixT, mix_b)
        kb.barrier()
        kb.stack = old
    with contextlib.ExitStack() as st:
        old = kb.stack
        kb.stack = st
        alloc_ring(g, 12288)
        hT = kb.sb("hT", [128, KC, TOK], BF16); hT_b = [kb.buf("hT0"), kb.buf("hT1")]
        norm_x(g, PC_GFFN0, hT, hT_b)
        act = [kb.sb(f"act{t}", [128, 2, TL], BF16) for t in range(NTL)]
        act_b = [[kb.buf(f"act{t}_{m}") for m in range(2)] for t in range(NTL)]
        for gi in range(DFF // 256):
            wg, wu, wd, wb = ffn_weights(g, g.ffn_gate, g.ffn_up, g.ffn_down, gi)
            ffn_group(g, wg, wu, wd, wb, hT, hT_b, act=act, act_b=act_b)
        ffn_drain(g)
        kb.barrier()
        kb.stack = old


def rope_AB(g, gcol, gswcol, AB, AB_b):
    kb, nc = g.kb, g.nc
    kb.op("dve", [g.cs_b, g.par_b], [AB_b], nc.vector.tensor_scalar, out=AB[:, 0, :], in0=g.cs[:, 0, :],
          scalar1=pcol(g, gcol, 64), scalar2=None, op0=ALU.mult)
    kb.op("dve", [g.cs_b, g.par_b], [AB_b], nc.vector.tensor_scalar, out=AB[:, 1, :], in0=g.cs[:, 1, :],
          scalar1=pcol(g, gswcol, 64), scalar2=None, op0=ALU.mult)


def shared_kv(g):
    kb, nc = g.kb, g.nc
    with contextlib.ExitStack() as st:
        old = kb.stack
        kb.stack = st
        hT = kb.sb("hT", [128, KC, TOK], BF16); hT_b = [kb.buf("hT0"), kb.buf("hT1")]
        ckv = kb.sb("ckv", [128, 2, TOK], BF16); ckv_b = [kb.buf("ckv0"), kb.buf("ckv1")]
        Rk = kb.sb("Rk", [64, TOK], F32); Rk_b = [kb.buf("Rk0"), kb.buf("Rk1")]
        sqpe = kb.sb("sqpe", [64, TOK], BF16); sqpe_b = [kb.buf("sqpe0"), kb.buf("sqpe1")]
        AB = kb.sb("ABk", [64, 2, TOK], F32); AB_b = kb.buf("ABk")
        kst = [kb.sb(f"kst{i}", [128, TOK], BF16) for i in range(2)]
        kst_b = [kb.buf(f"kst{i}") for i in range(2)]
        krst = [kb.sb(f"krst{i}", [64, TOK], BF16) for i in range(2)]
        krst_b = [kb.buf(f"krst{i}") for i in range(2)]
        vst = [kb.sb(f"vst{i}", [128, 512], BF16) for i in range(2)]
        vst_b = [kb.buf(f"vst{i}") for i in range(2)]
        for b in kst_b + krst_b + vst_b:
            kb._dbufs.append(b)
        alloc_ring(g, 8192)
        norm_x(g, PC_GKV, hT, hT_b)
        rope_AB(g, PC_GKN_R, PC_GKN_RS, AB, AB_b)
        wav = g.w_kv_a.rearrange("(kc p) m -> p kc m", p=128)
        (wa,), wab = load_w(g, [((128, KC, 320), wav)])
        wkbv = g.w_kv_b.rearrange("(kc p) m -> p kc m", p=128)
        (wb0, wb1), wbb = load_w(g, [((128, 3072), wkbv[:, 0, :]), ((128, 3072), wkbv[:, 1, :])])
        wbv = [wb0.rearrange("p (h c) -> p h c", h=12), wb1.rearrange("p (h c) -> p h c", h=12)]
        for t in range(NTL):
            sl = slice(t * TL, (t + 1) * TL)
            pl = []
            for c in range(2):
                pt, pb = bank(g)
                kb.mm(pb, pt[:, :], [(wa[:, kc, c * 128:(c + 1) * 128], hT[:, kc, sl]) for kc in range(KC)],
                      [wab, hT_b[t]])
                pl.append((pt, pb))
            rs, rsb = sumsq_rstd(g, [(pl[0][0][:, :], 128), (pl[1][0][:, :], 128)], TL, 256,
                                 [pl[0][1], pl[1][1]])
            for c in range(2):
                kb.op("dve", [pl[c][1], rsb, g.par_b], [ckv_b[t]], nc.vector.scalar_tensor_tensor,
                      out=ckv[:, c, sl], in0=pl[c][0][:, :], scalar=pcol(g, PC_GKVA + c), in1=rs[:],
                      op0=ALU.mult, op1=ALU.mult)
            pp, ppb = bank(g)
            kb.mm(ppb, pp[:64, :], [(wa[:, kc, 256:320], hT[:, kc, sl]) for kc in range(KC)], [wab, hT_b[t]])
            pq, pqb = bank(g)
            kb.mm_open([wab, hT_b[t]], [pqb])
            ins = None
            for kc in range(KC):
                nc.tensor.matmul(pq[0:32, :], wa[:, kc, 288:320], hT[:, kc, sl], start=(kc == 0), stop=(kc == KC - 1))
                ins = nc.tensor.matmul(pq[32:64, :], wa[:, kc, 256:288], hT[:, kc, sl], start=(kc == 0),
                                       stop=(kc == KC - 1))
                kb.n_inst += 2
            kb.mm_close(ins, [wab, hT_b[t]], [pqb])
            kb.op("act", [ppb], [sqpe_b[t]], nc.scalar.activation, out=sqpe[:, sl], in_=pp[:64, :], func=AF.Square)
            r1, r1b = scratch(g, "fs")
            kb.op("dve", [ppb, AB_b], [r1b], nc.vector.tensor_tensor, out=r1[:64, :TL], in0=pp[:64, :],
                  in1=AB[:, 0, sl], op=ALU.mult)
            kb.op("dve", [pqb, AB_b], [Rk_b[t]], nc.vector.tensor_tensor, out=Rk[:, sl], in0=pq[:64, :],
                  in1=AB[:, 1, sl], op=ALU.mult)
            kb.op("dve", [r1b, Rk_b[t]], [Rk_b[t]], nc.vector.tensor_tensor, out=Rk[:, sl], in0=Rk[:, sl],
                  in1=r1[:64, :TL], op=ALU.add)
        vi = 0
        vtok = [None, None]
        for tb in range(TOK // 128):
            t = tb // 4
            for hg in range(3):
                pt, pb = bank(g)
                kb.mm(pb, pt[:, :], [(ckv[:, c, tb * 128:(tb + 1) * 128], wbv[c][:, hg * 4:(hg + 1) * 4, 128:256])
                                      for c in range(2)], [wbb, ckv_b[t]])
                s = vi % 2
                vi += 1
                kb.op("act", [pb], [vst_b[s]], nc.scalar.copy, vst[s][:], pt[:, :])
                for hq in range(4):
                    vtok[s] = kb.dma("sp", g.v_own(hg * 4 + hq)[tb * 128:(tb + 1) * 128, :],
                                     vst[s][:, hq * 128:(hq + 1) * 128], vst_b[s], [vst_b[s]], [])
        if g.fused:
            for tk in vtok:
                kb._wait(kb.E["pool"], tk)
            for h in range(12):
                gather(g, g.V_own_l[h], g.Vg_l[h])
        for h in range(12):
            si = h % 2
            for t in range(NTL):
                sl = slice(t * TL, (t + 1) * TL)
                pt, pb = bank(g)
                kb.mm(pb, pt[:, :], [(wbv[c][:, h, 0:128], ckv[:, c, sl]) for c in range(2)], [wbb, ckv_b[t]])
                ps_, psb_ = bank(g)
                sq, sqb = scratch(g, "sq")
                kb.op("act", [pb], [sqb], nc.scalar.activation, out=sq[:], in_=pt[:, :], func=AF.Square)
                kb.mm_open([sqb, sqpe_b[t], g.cst_b], [psb_])
                nc.tensor.matmul(ps_[:, :], g.ones[:, :], sq[:], start=True, stop=False)
                ins = nc.tensor.matmul(ps_[:, :], g.ones[:64, :], sqpe[:, sl], start=False, stop=True)
                kb.n_inst += 2
                kb.mm_close(ins, [sqb, sqpe_b[t]], [psb_])
                rs, rsb = scratch(g, "rs")
                kb.op("act", [psb_], [rsb], nc.scalar.activation, out=rs[:], in_=ps_[:, :], func=AF.Sqrt,
                      scale=float(1.0 / 192), bias=float(EPS))
                kb.op("dve", [rsb], [rsb], nc.vector.reciprocal, rs[:], rs[:])
                kb.op("dve", [pb, rsb, g.par_b], [kst_b[si]], nc.vector.scalar_tensor_tensor, out=kst[si][:, sl],
                      in0=pt[:, :], scalar=pcol(g, PC_GKN_N), in1=rs[:], op0=ALU.mult, op1=ALU.mult)
                kb.op("dve", [Rk_b[t], rsb], [krst_b[si]], nc.vector.tensor_tensor, out=krst[si][:, sl],
                      in0=Rk[:, sl], in1=rs[:64, :], op=ALU.mult)
            tk1 = kb.dma("sp", g.kt_own(h)[0:128, :], kst[si][:], kst_b[si], [kst_b[si]], [])
            tk2 = kb.dma("sp", g.kt_own(h)[128:192, :], krst[si][:], krst_b[si], [krst_b[si]], [])
            if g.fused:
                kb._wait(kb.E["pool"], tk1)
                kb._wait(kb.E["pool"], tk2)
                gather(g, g.KT_own_l[h], g.KTg_l[h])
                g.kv_tok_h[h] = (g.cc_sem, g.cc_n, None)
        kb.barrier()
        kb.stack = old


def gather(g, src, dst):
    ins = g.nc.gpsimd.collective_compute("AllGather", ALU.bypass, replica_groups=[[0, 1, 2, 3], [4, 5, 6, 7]],
                                         ins=[src.opt()], outs=[dst.opt()])
    ins.then_inc(g.cc_sem)
    g.cc_n += 1


def exchange_kv(g):
    pass


def causal_attention(g, cqn, cqn_b, mixT, mix_b):
    kb, nc = g.kb, g.nc
    sc = float(192 ** -0.5)
    with contextlib.ExitStack() as st:
        old = kb.stack
        kb.stack = st
        NKV = 2
        kn = [kb.sb(f"kn{i}", [128, SEQ], BF16) for i in range(NKV)]
        kr = [kb.sb(f"kr{i}", [64, SEQ], BF16) for i in range(NKV)]
        vv = [kb.sb(f"vv{i}", [128, 32, 128], BF16) for i in range(NKV)]
        kv_b = [kb.buf(f"kv{i}") for i in range(NKV)]
        wqh = [kb.sb(f"wqh{i}", [128, 4, 192], BF16) for i in range(2)]
        wqh_b = [kb.buf(f"wqh{i}") for i in range(2)]
        qn = [kb.sb(f"qn{i}", [128, TOK], BF16) for i in range(2)]
        qr = [kb.sb(f"qr{i}", [64, TOK], BF16) for i in range(2)]
        q_b = [[kb.buf(f"q{i}_{t}") for t in range(NTL)] for i in range(2)]
        AB = kb.sb("ABq", [64, 2, TOK], F32); AB_b = kb.buf("ABq")
        qidx = kb.sb("qidx", [128, TOK], F32); qidx_b = kb.buf("qidx")
        for b in kv_b + wqh_b + [qidx_b]:
            kb._dbufs.append(b)
        kb.dma("sp", qidx[:], g.qidx.partition_broadcast(128).rearrange("p a n -> p (a n)"), qidx_b, [], [qidx_b])
        rope_AB(g, PC_GQN_R, PC_GQN_RS, AB, AB_b)
        wqv = g.w_q_b.rearrange("(kc p) m -> p kc m", p=128)
        g.bank_set = [4, 5, 6, 7]
        si = 0
        def load_kv(h):
            par = h % 2
            b = kv_b[par]
            E = kb.E["sp"]
            if g.fused:
                kb._wait(E, g.kv_tok_h[h])
            kb._sync(E, [], [b])
            if b.dsem is None:
                b.dsem = kb.new_sem("d_" + b.name)
            if b.dcount:
                kb._wait(E, (b.dsem, b.dcount, None))
            for i in range(8):
                r, off = min(i, 7 - i), (512 if i >= 4 else 0)
                srcs = [(kn[par][:, i * 512:(i + 1) * 512], g.kt_g(h, r)[0:128, off:off + 512]),
                        (kr[par][:, i * 512:(i + 1) * 512], g.kt_g(h, r)[128:192, off:off + 512]),
                        (vv[par][:, i * 4:(i + 1) * 4, :],
                         g.v_g(h, r)[off:off + 512, :].rearrange("(kb p) d -> p kb d", p=128))]
                for o_, i_ in srcs:
                    ins = nc.sync.dma_start(out=o_, in_=i_)
                    b.dcount += 16
                    ins.then_inc(b.dsem, 16)
                    kb.n_inst += 1
            kb._commit((b.dsem, b.dcount, None), [], [b])
            kb.dma("pool", wqh[par][:], wqv[:, :, h * 192:(h + 1) * 192], wqh_b[par], [], [wqh_b[par]])
            w = wqh[par]

        def q_head(h):
            par = h % 2
            w = wqh[par]
            for t in range(NTL):
                sl = slice(t * TL, (t + 1) * TL)
                pn, pnb = bank(g)
                kb.mm(pnb, pn[:, :], [(w[:, kc, 0:128], cqn[:, kc, sl]) for kc in range(4)], [wqh_b[par], cqn_b[t]])
                pr, prb = bank(g)
                kb.mm(prb, pr[:64, :], [(w[:, kc, 128:192], cqn[:, kc, sl]) for kc in range(4)], [wqh_b[par], cqn_b[t]])
                pq, pqb = bank(g)
                kb.mm_open([wqh_b[par], cqn_b[t]], [pqb])
                ins = None
                for kc in range(4):
                    nc.tensor.matmul(pq[0:32, :], w[:, kc, 160:192], cqn[:, kc, sl], start=(kc == 0), stop=(kc == 3))
                    ins = nc.tensor.matmul(pq[32:64, :], w[:, kc, 128:160], cqn[:, kc, sl], start=(kc == 0),
                                           stop=(kc == 3))
                    kb.n_inst += 2
                kb.mm_close(ins, [wqh_b[par], cqn_b[t]], [pqb])
                rs, rsb = sumsq_rstd(g, [(pn[:, :], 128), (pr[:64, :], 64)], TL, 192, [pnb, prb])
                kb.op("dve", [pnb, rsb, g.par_b], [q_b[par][t]], nc.vector.scalar_tensor_tensor, out=qn[par][:, sl],
                      in0=pn[:, :], scalar=pcol(g, PC_GQN_N), in1=rs[:], op0=ALU.mult, op1=ALU.mult)
                r1, r1b = scratch(g, "fs")
                kb.op("dve", [prb, AB_b], [r1b], nc.vector.tensor_tensor, out=r1[:64, :TL], in0=pr[:64, :],
                      in1=AB[:, 0, sl], op=ALU.mult)
                r2, r2b = scratch(g, "fs")
                kb.op("dve", [pqb, AB_b], [r2b], nc.vector.tensor_tensor, out=r2[:64, :TL], in0=pq[:64, :],
                      in1=AB[:, 1, sl], op=ALU.mult)
                kb.op("dve", [r1b, r2b], [r1b], nc.vector.tensor_tensor, out=r1[:64, :TL], in0=r1[:64, :TL],
                      in1=r2[:64, :TL], op=ALU.add)
                kb.op("dve", [r1b, rsb], [q_b[par][t]], nc.vector.tensor_tensor, out=qr[par][:, sl],
                      in0=r1[:64, :TL], in1=rs[:64, :], op=ALU.mult)

        def attn_head(h):
            nonlocal si
            par = h % 2
            for t in range(NTL):
                sl = slice(t * TL, (t + 1) * TL)
                nkb = 16 if t == 0 else 32
                po, pob = g.pst[2], g.psb[2]
                pd, pdb = g.pst[3], g.psb[3]
                def scores(kbi):
                    nonlocal si
                    S, Sb = g.pst[si % 2], g.psb[si % 2]
                    si += 1
                    ks = slice(kbi * 128, (kbi + 1) * 128)
                    kb.mm(Sb, S[:, :], [(kn[par][:, ks], qn[par][:, sl]), (kr[par][:, ks], qr[par][:, sl])],
                          [kv_b[par], q_b[par][t]])
                    return S, Sb

                pend = [scores(0), scores(1)]
                for kbi in range(nkb):
                    S, Sb = pend.pop(0)
                    e, eb = scratch(g, "sq")
                    kb.op("act", [Sb], [eb], nc.scalar.activation, out=e[:], in_=S[:, :], func=AF.Exp, scale=sc)
                    if kbi + 2 < nkb:
                        pend.append(scores(kbi + 2))
                    if t == 0 or kbi >= 16:
                        em, emb = scratch(g, "sq")
                        kb.op("dve", [qidx_b, g.par_b, eb], [emb], nc.vector.scalar_tensor_tensor, out=em[:],
                              in0=qidx[:, sl], scalar=pcol(g, PC_KIDX + kbi), in1=e[:], op0=ALU.is_ge, op1=ALU.mult)
                        e, eb = em, emb
                    kb.mm_open([eb, kv_b[par], g.cst_b], [pob, pdb] if kbi == 0 else [])
                    nc.tensor.matmul(po[:, :], vv[par][:, kbi, :], e[:], start=(kbi == 0), stop=(kbi == nkb - 1))
                    ins = nc.tensor.matmul(pd[:, :], g.ones[:, :], e[:], start=(kbi == 0), stop=(kbi == nkb - 1))
                    kb.n_inst += 2
                    kb.mm_close(ins, [eb, kv_b[par]], [pob, pdb])
                rd, rdb = scratch(g, "rs")
                kb.op("dve", [pdb], [rdb], nc.vector.reciprocal, rd[:], pd[:, :])
                kb.op("dve", [pob, rdb], [mix_b[t]], nc.vector.tensor_tensor, out=mixT[:, h, sl], in0=po[:, :],
                      in1=rd[:], op=ALU.mult)
        load_kv(0)
        q_head(0)
        for h in range(12):
            if h + 1 < 12:
                load_kv(h + 1)
                q_head(h + 1)
            attn_head(h)
        g.bank_set = list(range(8))
        kb.barrier()
        kb.stack = old


def moe(g):
    kb, nc = g.kb, g.nc
    with contextlib.ExitStack() as st:
        old = kb.stack
        kb.stack = st
        alloc_ring(g, 12288)
        hT = kb.sb("hT", [128, KC, TOK], BF16); hT_b = [kb.buf("hT0"), kb.buf("hT1")]
        selsb = kb.sb("selsb", [8, NEXP * 128], F32); sel_b = kb.buf("selsb"); kb._dbufs.append(sel_b)
        wr = kb.sb("wr", [128, KC, NEXP], F32); wr_b = kb.buf("wr"); kb._dbufs.append(wr_b)
        rtok = kb.sb("rtok", [128, 8], F32); rtok_b = kb.buf("rtok")
        lg = kb.sb("lg", [128, 8, NEXP], F32); lg_b = kb.buf("lg")
        t8 = kb.sb("t8", [128, 8, 8], F32); t8_b = kb.buf("t8")
        wts = kb.sb("wts", [128, 4, 8], F32); wts_b = kb.buf("wts")
        m1 = kb.sb("m1", [128, NEXP], F32); m1_b = kb.buf("m1")
        gates = kb.sb("gates", [128, 8, NEXP], F32); gates_b = kb.buf("gates")
        gT = kb.sb("gT", [8, TOK], F32); gT_b = kb.buf("gT")
        G = [kb.sb(f"G{i}", [128, TOK], F32) for i in range(2)]
        G_b = [[kb.buf(f"G{i}_{t}") for t in range(NTL)] for i in range(2)]
        act = [kb.sb(f"act{t}", [128, 2, TL], BF16) for t in range(NTL)]
        act_b = [[kb.buf(f"act{t}_{m}") for m in range(2)] for t in range(NTL)]
        kb.dma("sp", selsb[:], g.sel, sel_b, [], [sel_b])
        kb.dma("sp", wr[:], g.w_router.rearrange("(kc p) e -> p kc e", p=128), wr_b, [], [wr_b])
        for kc in range(KC):
            kb.op("dve", [wr_b, g.par_b], [wr_b], nc.vector.tensor_scalar, out=wr[:, kc, :], in0=wr[:, kc, :],
                  scalar1=pcol(g, PC_GFFN1 + kc), scalar2=None, op0=ALU.mult)

        def on_rstd(t, rs, rsb):
            for q in range(4):
                pt, pb = bank(g)
                kb.mm_open([rsb, g.cst_b], [pb])
                ins = nc.tensor.transpose(pt[:, 0:128], rs[:, q * 128:(q + 1) * 128], g.idf[:])
                kb.n_inst += 1
                kb.mm_close(ins, [rsb], [pb])
                kb.op("dve", [pb], [rtok_b], nc.vector.tensor_copy, rtok[:, t * 4 + q:t * 4 + q + 1], pt[:, 0:1])

        norm_x(g, PC_GFFN1, hT, hT_b, on_rstd=on_rstd)
        for tb in range(8):
            t = tb // 4
            pt, pb = bank(g)
            kb.mm(pb, pt[:, 0:NEXP], [(g.x[:, kc, tb * 128:(tb + 1) * 128], wr[:, kc, :]) for kc in range(KC)],
                  [wr_b] + [g.xb[kc][t] for kc in range(KC)])
            kb.op("dve", [pb, rtok_b], [lg_b], nc.vector.tensor_scalar, out=lg[:, tb, :], in0=pt[:, 0:NEXP],
                  scalar1=rtok[:, tb:tb + 1], scalar2=None, op0=ALU.mult)
            kb.op("dve", [lg_b], [t8_b], nc.vector.max, out=t8[:, tb, :], in_=lg[:, tb, :])
        kb.op("dve", [t8_b], [wts_b], nc.vector.tensor_tensor, out=wts[:, 0, :], in0=t8[:, :, 1], in1=t8[:, :, 0],
              op=ALU.subtract)
        kb.op("act", [wts_b], [wts_b], nc.scalar.activation, out=wts[:, 1, :], in_=wts[:, 0, :], func=AF.Exp)
        kb.op("dve", [wts_b], [wts_b], nc.vector.tensor_scalar, out=wts[:, 2, :], in0=wts[:, 1, :], scalar1=1.0,
              scalar2=None, op0=ALU.add)
        kb.op("dve", [wts_b], [wts_b], nc.vector.reciprocal, wts[:, 2, :], wts[:, 2, :])
        kb.op("dve", [wts_b], [wts_b], nc.vector.tensor_tensor, out=wts[:, 3, :], in0=wts[:, 1, :], in1=wts[:, 2, :],
              op=ALU.mult)
        for tb in range(8):
            kb.op("dve", [lg_b, t8_b, wts_b], [m1_b], nc.vector.tensor_scalar, out=m1[:], in0=lg[:, tb, :],
                  scalar1=t8[:, tb, 0:1], scalar2=wts[:, 2, tb:tb + 1], op0=ALU.is_equal, op1=ALU.mult)
            kb.op("dve", [lg_b, t8_b, wts_b], [gates_b], nc.vector.tensor_scalar, out=gates[:, tb, :], in0=lg[:, tb, :],
                  scalar1=t8[:, tb, 1:2], scalar2=wts[:, 3, tb:tb + 1], op0=ALU.is_equal, op1=ALU.mult)
            kb.op("dve", [m1_b, gates_b], [gates_b], nc.vector.tensor_tensor, out=gates[:, tb, :], in0=gates[:, tb, :],
                  in1=m1[:], op=ALU.add)
        for t in range(NTL):
            pt, pb = bank(g)
            kb.mm_open([gates_b, g.cst_b], [pb])
            ins = None
            for q in range(4):
                ins = nc.tensor.transpose(pt[0:NEXP, q * 128:(q + 1) * 128], gates[:, t * 4 + q, :], g.idf[:])
                kb.n_inst += 1
            kb.mm_close(ins, [gates_b], [pb])
            kb.op("act", [pb], [gT_b], nc.scalar.copy, gT[:, t * TL:(t + 1) * TL], pt[0:NEXP, :])
        for e in range(NEXP):
            gi_ = e % 2
            for t in range(NTL):
                sl = slice(t * TL, (t + 1) * TL)
                pt, pb = bank(g)
                kb.mm(pb, pt[:, :], [(selsb[:, e * 128:(e + 1) * 128], gT[:, sl])], [sel_b, gT_b])
                kb.op("act", [pb], [G_b[gi_][t]], nc.scalar.copy, G[gi_][:, sl], pt[:, :])
            for gi in range(DFF // 256):
                wg, wu, wd, wb = ffn_weights(g, g.moe_gate[e], g.moe_up[e], g.moe_down[e], gi)
                ffn_group(g, wg, wu, wd, wb, hT, hT_b, gate_bc=G[gi_], gate_bcb=G_b[gi_], act=act, act_b=act_b)
        ffn_drain(g)
        kb.barrier()
        kb.stack = old


def layer1(g):
    kb, nc = g.kb, g.nc
    with contextlib.ExitStack() as st0:
        old0 = kb.stack
        kb.stack = st0
        kmT = kb.sb("kmT", [128, 4, 256], BF16); kmT_b = kb.buf("kmT")
        vm = kb.sb("vm", [128, 2, 512], BF16); vm_b = kb.buf("vm")
        with contextlib.ExitStack() as st:
            kb.stack = st
            alloc_ring(g, 8192)
            mem_kv(g, 1, kmT, kmT_b, vm, vm_b)
            kb.stack = st0
        mixT = kb.sb("mixT", [128, KC, TOK], BF16); mix_b = [kb.buf("mix0"), kb.buf("mix1")]
        cqn = kb.sb("cqn", [128, 4, TOK], BF16); cqn_b = [kb.buf("cqn0"), kb.buf("cqn1")]
        with contextlib.ExitStack() as st:
            kb.stack = st
            alloc_ring(g, 8192)
            hT = kb.sb("hT", [128, KC, TOK], BF16); hT_b = [kb.buf("hT0"), kb.buf("hT1")]
            norm_x(g, PC_GMIX1, hT, hT_b)
            bw = g.b_w_in.rearrange("(kc p) m -> p kc m", p=128)
            (wqa,), wqab = load_w(g, [((128, KC, 512), bw[:, :, 0:512])])
            for t in range(NTL):
                sl = slice(t * TL, (t + 1) * TL)
                pl = []
                for c in range(4):
                    pt, pb = bank(g)
                    kb.mm(pb, pt[:, :], [(wqa[:, kc, c * 128:(c + 1) * 128], hT[:, kc, sl]) for kc in range(KC)],
                          [wqab, hT_b[t]])
                    pl.append((pt, pb))
                rs, rsb = sumsq_rstd(g, [(p[0][:, :], 128) for p in pl], TL, 512, [p[1] for p in pl])
                for c in range(4):
                    kb.op("dve", [pl[c][1], rsb, g.par_b], [cqn_b[t]], nc.vector.scalar_tensor_tensor,
                          out=cqn[:, c, sl], in0=pl[c][0][:, :], scalar=pcol(g, PC_GQA + c), in1=rs[:],
                          op0=ALU.mult, op1=ALU.mult)
            (wqm,), wqmb = load_w(g, [((128, KC, 512), bw[:, :, 512:1024])])
            mem_attention(g, wqm, wqmb, hT, hT_b, PC_GMQ1, kmT, kmT_b, vm, vm_b, mixT, mix_b)
            kb.barrier()
            kb.stack = st0
        causal_attention(g, cqn, cqn_b, mixT, mix_b)
        with contextlib.ExitStack() as st:
            kb.stack = st
            alloc_ring(g, 8192)
            out_proj(g, 1, mixT, mix_b)
            kb.barrier()
            kb.stack = st0
        kb.stack = old0
    moe(g)


def _core_tokens(core):
    b, c = core // 4, core % 4
    iA, iB = c, 7 - c
    idx = np.concatenate([np.arange(512 * iA, 512 * iA + 512), np.arange(512 * iB, 512 * iB + 512)])
    return b, (iA, iB), idx


def _chunk(v):
    return np.ascontiguousarray(v.reshape(-1, 128).T)


def _params(inp, core):
    b, (iA, iB), idx = _core_tokens(core)
    P = np.zeros((128, NPC), np.float32)
    P[:, PC_GMIX0:PC_GMIX0 + 16] = _chunk(inp["g_mix"][0])
    P[:, PC_GFFN0:PC_GFFN0 + 16] = _chunk(inp["g_ffn"][0])
    P[:, PC_GKV:PC_GKV + 16] = _chunk(inp["g_kv"])
    P[:, PC_GMIX1:PC_GMIX1 + 16] = _chunk(inp["g_mix"][1])
    P[:, PC_GFFN1:PC_GFFN1 + 16] = _chunk(inp["g_ffn"][1])
    cw = inp["a_conv_w"][0]
    P[:, PC_CONV:PC_CONV + 36] = cw.reshape(3, 12, 128).transpose(2, 1, 0).reshape(128, 36)
    P[:, PC_GMQ0] = inp["g_mq"][0]
    P[:, PC_GMK0] = inp["g_mk"][0]
    P[:, PC_GMQ1] = inp["g_mq"][1]
    P[:, PC_GMK1] = inp["g_mk"][1]
    P[:, PC_GQA:PC_GQA + 4] = _chunk(inp["b_g_q_a"][0])
    P[:, PC_GKVA:PC_GKVA + 2] = _chunk(inp["g_kv_a"])
    gq, gk = inp["b_g_qn"][0], inp["g_kn"]
    P[:, PC_GQN_N] = gq[:128]
    P[:, PC_GKN_N] = gk[:128]
    P[:64, PC_GQN_R] = gq[128:]
    P[:64, PC_GQN_RS] = np.concatenate([gq[160:], gq[128:160]])
    P[:64, PC_GKN_R] = gk[128:]
    P[:64, PC_GKN_RS] = np.concatenate([gk[160:], gk[128:160]])
    P[:32, PC_SGN] = -1.0
    P[32:64, PC_SGN] = 1.0
    invf = (1.0 / (10000.0 ** (np.arange(0, 64, 2, dtype=np.float32) / np.float32(64)))).astype(np.float32)
    P[:64, PC_INVF] = np.concatenate([invf, invf])
    P[:, PC_HALO] = 0.0 if iA == 0 else 1.0
    P[:, PC_HALO + 1] = 1.0
    P[:, PC_KIDX:PC_KIDX + 32] = (np.arange(32)[None, :] * 128 + np.arange(128)[:, None]).astype(np.float32)
    return P


def _consts():
    ident = np.eye(128, dtype=np.float32)
    sel = np.zeros((8, NEXP * 128), np.float32)
    for e in range(NEXP):
        sel[e, e * 128:(e + 1) * 128] = 1.0
    return ident, sel


def host_inputs(inp, core, mode):
    b, (iA, iB), idx = _core_tokens(core)
    ident, sel = _consts()
    m = {
        "params": _params(inp, core), "ident": ident, "sel": sel,
        "mem": np.ascontiguousarray(inp["mem"][b]), "g_mem": inp["g_mem"],
        "w_mem_kv": inp["w_mem_kv"], "w_out": inp["w_out"],
        "pos": np.ascontiguousarray(inp["positions"][b, idx][None, :]).astype(np.int32),
    }
    if mode in ("L0", "fused"):
        m["xT"] = np.ascontiguousarray(inp["x"][b, idx, :].T)
        xh = np.zeros((4, D), np.float32)
        for t, i in enumerate((iA, iB)):
            if i > 0:
                xh[2 * t:2 * t + 2] = inp["x"][b, 512 * i - 2:512 * i, :]
        m["xhT"] = np.ascontiguousarray(xh.T)
        m["a_w_in"] = inp["a_w_in"][0]
        m["w_kv_a"] = inp["w_kv_a"]
        m["w_kv_b"] = inp["w_kv_b"]
        m["ffn_gate"] = inp["ffn_w_gate"][0]
        m["ffn_up"] = inp["ffn_w_up"][0]
        m["ffn_down"] = inp["ffn_w_down"][0]
    if mode in ("L1", "fused"):
        m["qidx"] = idx[None, :].astype(np.float32)
        m["b_w_in"] = inp["b_w_in"][0]
        m["w_q_b"] = inp["b_w_q_b"][0]
        m["w_router"] = inp["moe_w_router"][0]
        m["moe_gate"] = inp["moe_w_gate"][0]
        m["moe_up"] = inp["moe_w_up"][0]
        m["moe_down"] = inp["moe_w_down"][0]
    return m


_PROGS = {}


def _prog(mode):
    if mode not in _PROGS:
        _PROGS[mode] = build(mode)[0]
    return _PROGS[mode]


FUSED = True


def kernel(**inputs):
    inp = {k: np.asarray(v) for k, v in inputs.items()}
    cores = list(range(NCORE))
    if FUSED:
        maps = [host_inputs(inp, c, "fused") for c in cores]
        res = run_bass_kernel_spmd(_prog("fused"), maps, core_ids=cores).results
    else:
        maps0 = [host_inputs(inp, c, "L0") for c in cores]
        r0 = run_bass_kernel_spmd(_prog("L0"), maps0, core_ids=cores).results
        maps1 = []
        for c in cores:
            b = c // 4
            m = host_inputs(inp, c, "L1")
            m["xT"] = np.asarray(r0[c]["x1T"])
            m["KTg"] = np.concatenate([np.asarray(r0[4 * b + r]["KT_own"]) for r in range(4)], axis=0)
            m["Vg"] = np.concatenate([np.asarray(r0[4 * b + r]["V_own"]) for r in range(4)], axis=0)
            maps1.append(m)
        res = run_bass_kernel_spmd(_prog("L1"), maps1, core_ids=cores).results
    out = np.zeros((NB, SEQ, D), np.float32)
    for c in cores:
        b, _, idx = _core_tokens(c)
        out[b, idx, :] = np.asarray(res[c]["yT"]).T
    return out
```
